# Optimizing a Trainium2 kernel written in Bass

```python
import math
import jax, jax.numpy as jnp
from jax import lax
import numpy as np

D_MODEL = 2048
BATCH = 4
SEQ = 2048
DEPTH = 1

GRID_W = 64
CTX_LEN = 256
EPS = 1e-6
NEG_INF = -1e30
D_INNER = 2 * D_MODEL
SSM_HEADDIM = 64
SSM_HEADS = D_INNER // SSM_HEADDIM
SSM_GROUPS = 8
D_STATE = 128
D_CONV = 5
CHUNK = 128
HEAD_DIM = 128
ATTN_HEADS = D_MODEL // HEAD_DIM
KV_HEADS = 4
Q_PER_KV = ATTN_HEADS // KV_HEADS
WINDOW = 128
ATTN_BLOCK = 128
ROPE_BASE = 10000.0
ROPE_PAIRS = HEAD_DIM // 4
PEER_HEADS = 8
N_KEYS = 128
N_EXPERTS = N_KEYS * N_KEYS
PEER_KEY_DIM = 256
KEY_HALF = PEER_KEY_DIM // 2
PEER_TOPK = 16
TOKEN_BLOCK = 128
K_W = KV_HEADS * HEAD_DIM
GN_W = SSM_GROUPS * D_STATE
XBC_W = D_INNER + 2 * GN_W
DT_W = 2 * SSM_HEADS
Q_W = ATTN_HEADS * HEAD_DIM
Z_W = D_INNER
GATE_W = 2 * D_MODEL
CTX_COLS = 2 * K_W + XBC_W + DT_W
TOTAL_COLS = CTX_COLS + Q_W + Z_W + GATE_W

kernel_name = "hybrid_ssd_swa_peer_dit_block"


def rms_norm(t, g):
    tf = t.astype(jnp.float32)
    tf = tf * lax.rsqrt(jnp.mean(tf * tf, axis=-1, keepdims=True) + EPS)
    return tf.astype(t.dtype) * g


def modulate(t, shift, scale):
    return t * (1.0 + scale) + shift


def axial_rope_tables(rows):
    row = jnp.repeat(jnp.arange(rows), GRID_W).astype(jnp.float32)
    col = jnp.tile(jnp.arange(GRID_W), rows).astype(jnp.float32)
    freqs = ROPE_BASE ** (-jnp.arange(ROPE_PAIRS, dtype=jnp.float32) / ROPE_PAIRS)
    ar = row[:, None] * freqs
    ac = col[:, None] * freqs
    ang = jnp.concatenate([ar, ar, ac, ac], axis=-1)
    return jnp.cos(ang), jnp.sin(ang)


def rope2d(t, cos, sin):
    shp = (1, cos.shape[0]) + (1,) * (t.ndim - 3) + (HEAD_DIM,)
    a1, a2, b1, b2 = jnp.split(t, 4, axis=-1)
    rot = jnp.concatenate([-a2, a1, -b2, b1], axis=-1)
    return t * cos.reshape(shp).astype(t.dtype) + rot * sin.reshape(shp).astype(t.dtype)


def softmax_with_sink(s, sink):
    col = jnp.broadcast_to(sink.astype(jnp.float32)[:, :, None, None], s.shape[:-1] + (1,))
    p = jax.nn.softmax(jnp.concatenate([s, col], axis=-1), axis=-1)
    return p[..., :-1]


def banded_window_attention(q, k, v, kc, vc, sink):
    b, S = q.shape[:2]
    nb = S // ATTN_BLOCK
    scale = HEAD_DIM ** -0.5
    qb = q.reshape(b, nb, ATTN_BLOCK, KV_HEADS, Q_PER_KV, HEAD_DIM)

    def windows(t):
        tp = jnp.pad(t, ((0, 0), (ATTN_BLOCK, ATTN_BLOCK), (0, 0), (0, 0)))
        tb = tp.reshape(b, nb + 2, ATTN_BLOCK, KV_HEADS, HEAD_DIM)
        return jnp.concatenate([tb[:, :-2], tb[:, 1:-1], tb[:, 2:]], axis=2)

    kw, vw = windows(k), windows(v)
    s_loc = jnp.einsum('bnqhrd,bnkhd->bnhrqk', qb, kw).astype(jnp.float32) * scale
    k_rel = jnp.arange(3 * ATTN_BLOCK) - ATTN_BLOCK
    q_rel = jnp.arange(ATTN_BLOCK)
    band = jnp.abs(k_rel[None, :] - q_rel[:, None]) <= WINDOW
    k_abs = jnp.arange(nb)[:, None] * ATTN_BLOCK + k_rel[None, :]
    valid = (k_abs >= 0) & (k_abs < S)
    mask = band[None] & valid[:, None, :]
    s_loc = jnp.where(mask[None, :, None, None], s_loc, NEG_INF)
    s_ctx = jnp.einsum('bnqhrd,bkhd->bnhrqk', qb, kc).astype(jnp.float32) * scale
    p = softmax_with_sink(jnp.concatenate([s_loc, s_ctx], axis=-1), sink)
    p_loc = p[..., :3 * ATTN_BLOCK].astype(v.dtype)
    p_ctx = p[..., 3 * ATTN_BLOCK:].astype(v.dtype)
    out = jnp.einsum('bnhrqk,bnkhd->bnqhrd', p_loc, vw) + jnp.einsum('bnhrqk,bkhd->bnqhrd', p_ctx, vc)
    return out.reshape(b, S, Q_W)


def context_attention(qc, kc, vc, sink):
    b, C = qc.shape[:2]
    s = jnp.einsum('bqhrd,bkhd->bhrqk', qc, kc).astype(jnp.float32) * (HEAD_DIM ** -0.5)
    p = softmax_with_sink(s, sink).astype(vc.dtype)
    return jnp.einsum('bhrqk,bkhd->bqhrd', p, vc).reshape(b, C, Q_W)


def dwconv_silu(t, w, bias):
    ch = t.shape[-1]
    out = lax.conv_general_dilated(t, w[:, None, :].astype(t.dtype), window_strides=(1,),
                                   padding=[(D_CONV // 2, D_CONV // 2)],
                                   dimension_numbers=('NWC', 'WIO', 'NWC'), feature_group_count=ch)
    return jax.nn.silu(out + bias)


def ssd_chunked(xh, dt, A, Bm, Cm, h0, return_y):
    b, L, H, P = xh.shape
    G, N = Bm.shape[-2:]
    R = H // G
    nc = L // CHUNK
    xr = xh.astype(jnp.float32).reshape(b, nc, CHUNK, G, R, P)
    dtr = dt.reshape(b, nc, CHUNK, G, R)
    Br = Bm.astype(jnp.float32).reshape(b, nc, CHUNK, G, N)
    Cr = Cm.astype(jnp.float32).reshape(b, nc, CHUNK, G, N)
    acs = jnp.cumsum(dtr * A.reshape(G, R), axis=2)
    a_last = acs[:, :, -1]
    wx = dtr[..., None] * xr
    wx_end = jnp.exp(a_last[:, :, None] - acs)[..., None] * wx
    states = jnp.einsum('bcsgn,bcsgrp->bcgrpn', Br, wx_end)

    def step(h, inp):
        s_c, al = inp
        return jnp.exp(al)[..., None, None] * h + s_c, h

    h_final, h_prev = lax.scan(step, h0.astype(jnp.float32).reshape(b, G, R, P, N),
                               (jnp.moveaxis(states, 1, 0), jnp.moveaxis(a_last, 1, 0)))
    h_final = h_final.reshape(b, H, P, N)
    if not return_y:
        return None, h_final
    h_prev = jnp.moveaxis(h_prev, 0, 1)
    y_off = jnp.einsum('bclgn,bcgrpn->bclgrp', Cr, h_prev) * jnp.exp(acs)[..., None]
    causal = jnp.tril(jnp.ones((CHUNK, CHUNK), dtype=bool))
    seg = acs[:, :, :, None] - acs[:, :, None, :]
    decay = jnp.exp(jnp.where(causal[None, None, :, :, None, None], seg, -jnp.inf))
    CB = jnp.einsum('bclgn,bcsgn->bclsg', Cr, Br)
    y_diag = jnp.einsum('bclsgr,bcsgrp->bclgrp', CB[..., None] * decay, wx)
    return (y_diag + y_off).reshape(b, L, H, P), h_final


def ssm_bidir(xbc, dt_raw, dt_bias, a_log, h0_f, h0_b, return_y):
    b, L, _ = xbc.shape
    xh = xbc[..., :D_INNER].reshape(b, L, SSM_HEADS, SSM_HEADDIM)
    Bm = xbc[..., D_INNER:D_INNER + GN_W].reshape(b, L, SSM_GROUPS, D_STATE)
    Cm = xbc[..., D_INNER + GN_W:].reshape(b, L, SSM_GROUPS, D_STATE)
    A = -jnp.exp(a_log.astype(jnp.float32))
    dtf = dt_raw.astype(jnp.float32)
    dt_f = jax.nn.softplus(dtf[..., :SSM_HEADS] + dt_bias[0].astype(jnp.float32))
    dt_b = jax.nn.softplus(dtf[..., SSM_HEADS:] + dt_bias[1].astype(jnp.float32))
    fl = lambda t: jnp.flip(t, axis=1)
    y_f, h_f = ssd_chunked(xh, dt_f, A[0], Bm, Cm, h0_f, return_y)
    y_b, h_b = ssd_chunked(fl(xh), fl(dt_b), A[1], fl(Bm), fl(Cm), h0_b, return_y)
    y = y_f + fl(y_b) if return_y else None
    return y, xh, h_f, h_b


def merge_branches(y, xh, z, gates, attn, ssm_d, ssm_norm_g, w_br_ssm, w_br_attn, w_out):
    b, L = y.shape[:2]
    y = (y + ssm_d.astype(jnp.float32)[:, None] * xh.astype(jnp.float32)).reshape(b, L, D_INNER)
    y = rms_norm(y * jax.nn.silu(z.astype(jnp.float32)), ssm_norm_g).astype(z.dtype)
    g = jax.nn.sigmoid(gates)
    merged = g[..., :D_MODEL] * (y @ w_br_ssm) + g[..., D_MODEL:] * (attn @ w_br_attn)
    return merged @ w_out


def mixing_sublayer(u, uc, w_in, conv_w, conv_b, dt_bias, a_log, ssm_d, ssm_norm_g, attn_sink,
                    w_br_ssm, w_br_attn, w_out, cos, sin, ctx_out):
    b, S, _ = u.shape
    C = uc.shape[1]
    p = u @ w_in
    pc = uc @ (w_in if ctx_out else w_in[:, :CTX_COLS])

    def ctx_side(t):
        return (t[..., :K_W], t[..., K_W:2 * K_W], t[..., 2 * K_W:2 * K_W + XBC_W],
                t[..., 2 * K_W + XBC_W:CTX_COLS])

    def query_side(t):
        return (t[..., CTX_COLS:CTX_COLS + Q_W], t[..., CTX_COLS + Q_W:CTX_COLS + Q_W + Z_W],
                t[..., CTX_COLS + Q_W + Z_W:])

    k, v, xbc, dt = ctx_side(p)
    q, z, gates = query_side(p)
    kc, vc, xbcc, dtc = ctx_side(pc)
    sink = attn_sink.reshape(KV_HEADS, Q_PER_KV)
    q = rope2d(q.reshape(b, S, KV_HEADS, Q_PER_KV, HEAD_DIM), cos, sin)
    k = rope2d(k.reshape(b, S, KV_HEADS, HEAD_DIM), cos, sin)
    v = v.reshape(b, S, KV_HEADS, HEAD_DIM)
    kc = kc.reshape(b, C, KV_HEADS, HEAD_DIM)
    vc = vc.reshape(b, C, KV_HEADS, HEAD_DIM)
    attn = banded_window_attention(q, k, v, kc, vc, sink)
    xbc = dwconv_silu(xbc, conv_w, conv_b)
    xbcc = dwconv_silu(xbcc, conv_w, conv_b)
    h0 = jnp.zeros((b, SSM_HEADS, SSM_HEADDIM, D_STATE), jnp.float32)
    yc, xhc, hcf, hcb = ssm_bidir(xbcc, dtc, dt_bias, a_log, h0, h0, ctx_out)
    y, xh, _, _ = ssm_bidir(xbc, dt, dt_bias, a_log, hcf, hcb, True)
    out = merge_branches(y, xh, z, gates, attn, ssm_d, ssm_norm_g, w_br_ssm, w_br_attn, w_out)
    if not ctx_out:
        return out, None
    qc, zc, gatesc = query_side(pc)
    attnc = context_attention(qc.reshape(b, C, KV_HEADS, Q_PER_KV, HEAD_DIM), kc, vc, sink)
    outc = merge_branches(yc, xhc, zc, gatesc, attnc, ssm_d, ssm_norm_g, w_br_ssm, w_br_attn, w_out)
    return out, outc


def peer_ffn(u, wq, keys1, keys2, pu, pv):
    b, L, _ = u.shape
    q = (u @ wq).reshape(b, L, PEER_HEADS, 2, KEY_HALF)
    s1 = jnp.einsum('blhd,kd->blhk', q[..., 0, :], keys1).astype(jnp.float32)
    s2 = jnp.einsum('blhd,kd->blhk', q[..., 1, :], keys2).astype(jnp.float32)
    v1, i1 = lax.top_k(s1, PEER_TOPK)
    v2, i2 = lax.top_k(s2, PEER_TOPK)
    cand_s = (v1[..., :, None] + v2[..., None, :]).reshape(b, L, PEER_HEADS, PEER_TOPK * PEER_TOPK)
    cand_i = (i1[..., :, None] * N_KEYS + i2[..., None, :]).reshape(b, L, PEER_HEADS, PEER_TOPK * PEER_TOPK)
    top_s, pos = lax.top_k(cand_s, PEER_TOPK)
    expert_idx = jnp.take_along_axis(cand_i, pos, axis=-1)
    gate = jax.nn.softmax(top_s, axis=-1).astype(u.dtype)
    n_blk = (b * L) // TOKEN_BLOCK
    xs = u.reshape(n_blk, TOKEN_BLOCK, D_MODEL)
    idx = expert_idx.reshape(n_blk, TOKEN_BLOCK, PEER_HEADS * PEER_TOPK)
    gs = gate.reshape(n_blk, TOKEN_BLOCK, PEER_HEADS * PEER_TOPK)

    def block(args):
        xb, ib, gb = args
        act = jax.nn.gelu(jnp.einsum('td,ted->te', xb, pu[ib]), approximate=False)
        return jnp.einsum('te,ted->td', gb * act, pv[ib])

    return lax.map(block, (xs, idx, gs)).reshape(b, L, D_MODEL)


def setup_inputs(seed: int = 0) -> dict:
    key = jax.random.key(seed)
    ks = jax.random.split(key, 26)

    def nrm(k, shape, scale):
        return jax.random.normal(k, shape, jnp.float32) * scale

    dt0 = jnp.exp(jax.random.uniform(ks[10], (DEPTH, 2, SSM_HEADS), jnp.float32,
                                     minval=math.log(1e-3), maxval=math.log(1e-1)))
    return {
        "x": nrm(ks[0], (BATCH, SEQ, D_MODEL), 1.0),
        "c": nrm(ks[1], (BATCH, D_MODEL), 1.0),
        "ctx": nrm(ks[2], (BATCH, CTX_LEN, D_MODEL), 1.0),
        "c_ctx": nrm(ks[3], (D_MODEL,), 1.0),
        "ada_w": nrm(ks[4], (DEPTH, D_MODEL, 6 * D_MODEL), D_MODEL ** -0.5),
        "ada_b": nrm(ks[5], (DEPTH, 6 * D_MODEL), 0.02),
        "norm1_g": 1.0 + nrm(ks[6], (DEPTH, D_MODEL), 0.02),
        "w_in": nrm(ks[7], (DEPTH, D_MODEL, TOTAL_COLS), D_MODEL ** -0.5),
        "conv_w": nrm(ks[8], (DEPTH, D_CONV, XBC_W), D_CONV ** -0.5),
        "conv_b": nrm(ks[9], (DEPTH, XBC_W), 0.02),
        "dt_bias": dt0 + jnp.log(-jnp.expm1(-dt0)),
        "a_log": jnp.log(jax.random.uniform(ks[11], (DEPTH, 2, SSM_HEADS), jnp.float32, minval=1.0, maxval=16.0)),
        "ssm_d": 1.0 + nrm(ks[12], (DEPTH, SSM_HEADS), 0.02),
        "ssm_norm_g": 1.0 + nrm(ks[13], (DEPTH, D_INNER), 0.02),
        "attn_sink": nrm(ks[14], (DEPTH, ATTN_HEADS), 0.5),
        "w_branch_ssm": nrm(ks[15], (DEPTH, D_INNER, D_MODEL), D_INNER ** -0.5),
        "w_branch_attn": nrm(ks[16], (DEPTH, Q_W, D_MODEL), Q_W ** -0.5),
        "w_out": nrm(ks[17], (DEPTH, D_MODEL, D_MODEL), D_MODEL ** -0.5),
        "norm2_g": 1.0 + nrm(ks[18], (DEPTH, D_MODEL), 0.02),
        "peer_wq": nrm(ks[19], (DEPTH, D_MODEL, PEER_HEADS * PEER_KEY_DIM), D_MODEL ** -0.5),
        "peer_keys1": nrm(ks[20], (DEPTH, N_KEYS, KEY_HALF), KEY_HALF ** -0.5),
        "peer_keys2": nrm(ks[21], (DEPTH, N_KEYS, KEY_HALF), KEY_HALF ** -0.5),
        "peer_u": nrm(ks[22], (DEPTH, N_EXPERTS, D_MODEL), D_MODEL ** -0.5),
        "peer_v": nrm(ks[23], (DEPTH, N_EXPERTS, D_MODEL), PEER_HEADS ** -0.5),
        "final_norm_g": 1.0 + nrm(ks[24], (D_MODEL,), 0.02),
    }


def reference(x, c, ctx, c_ctx, ada_w, ada_b, norm1_g, w_in, conv_w, conv_b, dt_bias, a_log, ssm_d,
              ssm_norm_g, attn_sink, w_branch_ssm, w_branch_attn, w_out, norm2_g, peer_wq, peer_keys1,
              peer_keys2, peer_u, peer_v, final_norm_g):
    b, S, _ = x.shape
    ROWS = S // GRID_W
    cos, sin = axial_rope_tables(ROWS)
    silu_c = jax.nn.silu(c)
    silu_cc = jax.nn.silu(c_ctx)
    h, hc = x, ctx
    for l in range(DEPTH):
        ctx_out = l < DEPTH - 1
        mod = (silu_c @ ada_w[l] + ada_b[l]).reshape(b, 6, 1, D_MODEL)
        modc = (silu_cc @ ada_w[l] + ada_b[l]).reshape(6, D_MODEL)
        u = modulate(rms_norm(h, norm1_g[l]), mod[:, 0], mod[:, 1])
        uc = modulate(rms_norm(hc, norm1_g[l]), modc[0], modc[1])
        mix, mixc = mixing_sublayer(u, uc, w_in[l], conv_w[l], conv_b[l], dt_bias[l], a_log[l], ssm_d[l],
                                    ssm_norm_g[l], attn_sink[l], w_branch_ssm[l], w_branch_attn[l],
                                    w_out[l], cos, sin, ctx_out)
        h = h + mod[:, 2] * mix
        u = modulate(rms_norm(h, norm2_g[l]), mod[:, 3], mod[:, 4])
        h = h + mod[:, 5] * peer_ffn(u, peer_wq[l], peer_keys1[l], peer_keys2[l], peer_u[l], peer_v[l])
        if ctx_out:
            hc = hc + modc[2] * mixc
            uc = modulate(rms_norm(hc, norm2_g[l]), modc[3], modc[4])
            hc = hc + modc[5] * peer_ffn(uc, peer_wq[l], peer_keys1[l], peer_keys2[l], peer_u[l], peer_v[l])
    return rms_norm(h, final_norm_g)
```

```python
import math
from contextlib import ExitStack
import numpy as np
import concourse.bass as bass
import concourse.mybir as mybir
from concourse.bass_utils import run_bass_kernel_spmd

F32 = mybir.dt.float32
BF16 = mybir.dt.bfloat16
AF = mybir.ActivationFunctionType
ALU = mybir.AluOpType
AX = mybir.AxisListType

D = 2048
S = 2048
CTXL = 256
NTOK = S + CTXL
NT_ALL = 18
NT_OWN = 8
DI = 4096
NH = 64
C_K, C_V, C_XBC, C_DT, C_Q, C_Z, C_G = 0, 512, 1024, 7168, 7296, 9344, 13440
NE = 16384
EPS = 1e-6
SCALE = 128.0 ** -0.5

COMPUTE = ("pe", "act", "dve", "pool")
ALLENG = ("pe", "act", "dve", "pool", "sp")


class Prog:
    def __init__(self, nc, n_dma_sems=96):
        self.nc = nc
        self.lists = {e: [] for e in ALLENG}
        self.semnames = []
        self.semval = {}
        self.waited = {e: {} for e in ALLENG}
        self.res = {}
        for e in COMPUTE:
            self._newsem("E_" + e)
        self.n_dma_sems = n_dma_sems
        self.dma_keys = {}

    def _newsem(self, key):
        self.semnames.append(key)
        self.semval[key] = 0

    def _dma_sem(self, key):
        if key not in self.dma_keys:
            name = "D_%d" % len(self.dma_keys)
            assert len(self.dma_keys) < self.n_dma_sems, "too many dma sems"
            self.dma_keys[key] = name
            self._newsem(name)
        return self.dma_keys[key]

    def _deps(self, eng, reads, writes):
        evs = {}

        def add(ev):
            if ev is None:
                return
            s, v = ev
            if evs.get(s, 0) < v:
                evs[s] = v

        for r in reads:
            st = self.res.get(r)
            if st:
                add(st["w"])
        for w in writes:
            st = self.res.get(w)
            if st:
                add(st["w"])
                for s, v in st["r"].items():
                    add((s, v))
        out = []
        for s, v in evs.items():
            if eng == "pe" and s == "E_pe":
                continue
            if self.waited[eng].get(s, 0) >= v:
                continue
            self.waited[eng][s] = v
            out.append((s, v))
        return out

    def _commit(self, ev, reads, writes):
        s, v = ev
        for r in reads:
            st = self.res.setdefault(r, {"w": None, "r": {}})
            if st["r"].get(s, 0) < v:
                st["r"][s] = v
        for w in writes:
            self.res[w] = {"w": ev, "r": {}}

    def op(self, eng, fn, reads=(), writes=()):
        waits = self._deps(eng, reads, writes)
        s = "E_" + eng
        self.semval[s] += 1
        ev = (s, self.semval[s])
        self.lists[eng].append((waits, fn, s, 1))
        self._commit(ev, reads, writes)

    def dma(self, out, in_, reads=(), writes=(), key=None, queue="sp"):
        if key is None or key == "st":
            key = ("ld", writes[0]) if (writes and key is None) else ("st", reads[0])
        s = self._dma_sem(key)
        waits = self._deps(queue, reads, writes)
        self.semval[s] += 16
        ev = (s, self.semval[s])
        self.lists[queue].append((waits, lambda e: e.dma_start(out=out, in_=in_), s, 16))
        self._commit(ev, reads, writes)

    def barrier(self):
        for e in ALLENG:
            waits = []
            for s in self.semnames:
                v = self.semval[s]
                if v > 0 and self.waited[e].get(s, 0) < v:
                    if e == "pe" and s == "E_pe":
                        continue
                    self.waited[e][s] = v
                    waits.append((s, v))
            if waits:
                self.lists[e].append((waits, None, None, 0))
        self.res = {}

    def emit(self):
        nc = self.nc
        waits = [(s, self.semval[s]) for s in self.semnames if self.semval[s] > 0]
        self.lists["sp"].append((waits, None, None, 0))
        with ExitStack() as st:
            sems = {}
            for s in self.semnames:
                sems[s] = st.enter_context(nc.semaphore(s))
            block = st.enter_context(nc.Block())

            def mk(engname):
                def body(eng):
                    for waits, fn, s, inc in self.lists[engname]:
                        for (ws, wv) in waits:
                            eng.wait_ge(sems[ws], wv)
                        if fn is not None:
                            fn(eng).then_inc(sems[s], inc)
                return body

            block.tensor(mk("pe"))
            block.scalar(mk("act"))
            block.vector(mk("dve"))
            block.gpsimd(mk("pool"))
            block.sync(mk("sp"))


def build(debug=False):
    nc = bass.Bass("TRN2", target_bir_lowering=False)
    P = Prog(nc)
    skind = "ExternalOutput" if debug else "Internal"

    def din(name, shape, dt=F32):
        return nc.dram_tensor(name, shape, dt, kind="ExternalInput").ap()

    def dscr(name, shape, dt=F32):
        return nc.dram_tensor(name, shape, dt, kind=skind).ap()

    x_c = din("x_c", [S, D])
    ctx_c = din("ctx_c", [CTXL, D])
    c_fm = din("c_fm", [128, 16])
    cc_fm = din("cc_fm", [128, 16])
    ada_w = din("ada_w", [D, 6 * D])
    ada_b = din("ada_b", [1, 6 * D])
    g1_d = din("norm1_g", [1, D])
    g2_d = din("norm2_g", [1, D])
    gF_d = din("final_g", [1, D])
    w_in = din("w_in", [D, 17536])
    w_dt = din("w_dt", [D, 128])
    convw = din("convw_fm", [128, 48, 5])
    convb = din("convb_fm", [128, 48])
    dtb_d = din("dt_bias", [1, 128])
    alog_d = din("a_log", [1, 128])
    ssmd_d = din("ssm_d", [1, 64])
    gN_d = din("ssm_norm_g", [1, DI])
    sink_d = din("attn_sink", [1, 16])
    w_bs = din("w_bs", [DI, D])
    w_ba = din("w_ba", [D, D])
    w_o = din("w_o", [D, D])
    w_q = din("w_q", [D, D])
    keys1 = din("keys1", [128, 128])
    keys2 = din("keys2", [128, 128])
    pu = din("peer_u", [NE, D])
    pv = din("peer_v", [NE, D])
    ident_d = din("ident", [128, 128])
    triF_d = din("triF", [128, 128])
    triB_d = din("triB", [128, 128])
    cos_d = din("cos_t", [9 * 128, 2048])
    sin_d = din("sin_t", [9 * 128, 2048])
    out_d = nc.dram_tensor("out", [1024, D], F32, kind="ExternalOutput").ap()

    MOD = dscr("MOD_scr", [8, 128, D])
    KV = dscr("KV_scr", [NTOK, 1024])
    DTs = dscr("DT_scr", [NTOK, 128])
    Qs = dscr("Q_scr", [1024, 2048])
    Zs = dscr("Z_scr", [1024, DI])
    XBCT = dscr("XBCT_scr", [6144, NTOK])
    GTs = dscr("GT_scr", [4096, 1024])
    ATT = dscr("ATT_scr", [128, 16, 1024], BF16)
    XTM = dscr("XTM_scr", [NTOK, 5120], BF16)
    BCT = dscr("BCT_scr", [2048, NTOK], BF16)
    Y1 = dscr("Y1_scr", [1024, DI])
    Y2 = dscr("Y2_scr", [1024, DI])
    Hs = dscr("H_scr", [1024, D])
    GTP = dscr("GTP_scr", [NE, 1024], BF16)
    ACTs = dscr("ACT_scr", [NE, 1024], BF16)

    with ExitStack() as top:
        uid = {"n": 0}

        def sbt(st, name, shape, dt=F32):
            uid["n"] += 1
            return st.enter_context(nc.sbuf_tensor("s%d_%s" % (uid["n"], name), shape, dt))

        ps = [top.enter_context(nc.psum_tensor("ps%d" % i, [128, 512], F32)) for i in range(6)]
        psb = [top.enter_context(nc.psum_tensor("psb%d" % i, [128, 1024], BF16)) for i in range(2)]
        ident = sbt(top, "ident", [128, 128])
        identb = sbt(top, "identb", [128, 128], BF16)
        triF = sbt(top, "triF", [128, 128])
        triB = sbt(top, "triB", [128, 128])
        triFb = sbt(top, "triFb", [128, 128], BF16)
        triBb = sbt(top, "triBb", [128, 128], BF16)
        ones = sbt(top, "ones", [128, 128])
        onesb = sbt(top, "onesb", [128, 128], BF16)
        cst = sbt(top, "cst", [128, 4])
        P.dma(ident[:], ident_d, writes=["ident"])
        P.dma(triF[:], triF_d, writes=["triF"])
        P.dma(triB[:], triB_d, writes=["triB"])
        P.op("dve", lambda e: e.tensor_copy(identb[:], ident[:]), reads=["ident"], writes=["identb"])
        P.op("dve", lambda e: e.tensor_copy(triFb[:], triF[:]), reads=["triF"], writes=["triFb"])
        P.op("dve", lambda e: e.tensor_copy(triBb[:], triB[:]), reads=["triB"], writes=["triBb"])
        P.op("dve", lambda e: e.memset(ones[:], 1.0), writes=["ones"])
        P.op("dve", lambda e: e.memset(onesb[:], 1.0), writes=["onesb"])
        P.op("dve", lambda e: e.memset(cst[:, 0:1], 1.0), writes=["cst0"])
        P.op("dve", lambda e: e.memset(cst[:, 1:2], EPS), writes=["cst1"])
        P.op("dve", lambda e: e.memset(cst[:, 2:3], 0.0), writes=["cst2"])
        P.barrier()
        rr = {"n": 0}

        def evac_eng():
            rr["n"] += 1
            return "act" if rr["n"] % 2 == 0 else "dve"

        def copy_op(eng, out, in_, reads, writes):
            if eng == "act":
                P.op("act", lambda e: e.copy(out, in_), reads=reads, writes=writes)
            else:
                P.op(eng, lambda e: e.tensor_copy(out, in_), reads=reads, writes=writes)

        def load_w_block(wb, slot, Wd, KC, col0, ncols):
            for k0 in range(0, KC, 4):
                P.dma(wb[slot][:, k0:k0 + 4, 0:ncols],
                      Wd[k0 * 128:(k0 + 4) * 128, col0:col0 + ncols].rearrange("(k p) n -> p k n", p=128),
                      writes=["wb%d_%d" % (slot, k0)], key=("wb", slot), queue="pool")

        def norm_mod_T(st, src_rows, n_tiles, Atile_of, Stile_of, xT, tagp):
            xt = [sbt(st, tagp + "xt%d" % i, [128, D]) for i in range(2)]
            junk = sbt(st, tagp + "junk", [128, D])
            t1 = sbt(st, tagp + "t1", [128, D])
            ub = [sbt(st, tagp + "ub%d" % i, [128, D], BF16) for i in range(2)]
            ss = sbt(st, tagp + "ss", [128, 4])
            for t in range(n_tiles):
                sl = t % 2
                P.dma(xt[sl][:], src_rows(t), writes=[tagp + "xt%d" % sl], key=(tagp + "xt", sl))
                P.op("dve", lambda e: e.memset(ss[:, 0:1], 0.0), writes=[tagp + "ss0"])
                P.op("act", lambda e, sl=sl: e.activation(junk[:], xt[sl][:], AF.Square, accum_out=ss[:, 0:1]),
                     reads=[tagp + "xt%d" % sl, tagp + "ss0"], writes=[tagp + "junk", tagp + "ss0"])
                P.op("act", lambda e: e.activation(ss[:, 1:2], ss[:, 0:1], AF.Sqrt, bias=cst[:, 1:2], scale=1.0 / D),
                     reads=[tagp + "ss0"], writes=[tagp + "ss1"])
                P.op("dve", lambda e: e.reciprocal(ss[:, 2:3], ss[:, 1:2]), reads=[tagp + "ss1"], writes=[tagp + "ss2"])
                A, An = Atile_of(t)
                Sh, Sn = Stile_of(t)
                P.op("dve", lambda e, sl=sl, A=A: e.scalar_tensor_tensor(out=t1[:], in0=xt[sl][:], scalar=ss[:, 2:3], in1=A[:],
                                                                      op0=ALU.mult, op1=ALU.mult),
                     reads=[tagp + "xt%d" % sl, tagp + "ss2", An], writes=[tagp + "t1"])
                P.op("pool", lambda e, sl=sl, Sh=Sh: e.tensor_tensor(ub[sl][:], t1[:], Sh[:], ALU.add),
                     reads=[tagp + "t1", Sn], writes=[tagp + "ub%d" % sl])
                for half in range(2):
                    for j in range(8):
                        kc = half * 8 + j
                        P.op("pe", lambda e, sl=sl, kc=kc, half=half, j=j: e.transpose(
                            psb[half][:, j * 128:(j + 1) * 128], ub[sl][:, kc * 128:(kc + 1) * 128], identb[:]),
                            reads=[tagp + "ub%d" % sl, "identb"], writes=["psb%d" % half])
                    copy_op(evac_eng(), xT[:, half * 8:(half + 1) * 8, t * 128:(t + 1) * 128],
                            psb[half][:].rearrange("p (k t) -> p k t", k=8),
                            reads=["psb%d" % half], writes=["xT_" + tagp])

        def linear_T(xT, xTname, KC, wb, slot, ncols, tiles, consume):
            for t in tiles:
                b = rr["n"] % 4
                for kc in range(KC):
                    P.op("pe", lambda e, b=b, kc=kc, t=t: e.matmul(ps[b][:, 0:ncols], lhsT=xT[:, kc, t * 128:(t + 1) * 128],
                                                                  rhs=wb[slot][:, kc, 0:ncols], start=(kc == 0), stop=(kc == KC - 1)),
                         reads=[xTname] + ["wb%d_%d" % (slot, k0) for k0 in range(0, 16, 4)], writes=["ps%d" % b])
                consume(ps[b], "ps%d" % b, t)

        def linear_F(xT, xTname, KC, wb, slot, ncols, tokgroups, consume):
            for ch in range(ncols // 128):
                for (t0, nt) in tokgroups:
                    b = rr["n"] % 4
                    for kc in range(KC):
                        P.op("pe", lambda e, b=b, kc=kc, t0=t0, nt=nt, ch=ch: e.matmul(
                            ps[b][:, 0:nt], lhsT=wb[slot][:, kc, ch * 128:(ch + 1) * 128], rhs=xT[:, kc, t0:t0 + nt],
                            start=(kc == 0), stop=(kc == KC - 1)),
                            reads=[xTname] + ["wb%d_%d" % (slot, k0) for k0 in range(0, 16, 4)], writes=["ps%d" % b])
                    consume(ps[b], "ps%d" % b, ch, t0, nt)

        with ExitStack() as st:
            cs = sbt(st, "cs", [128, 2, 16])
            csb = sbt(st, "csb", [128, 2, 16, 128], BF16)
            adab = sbt(st, "adab", [128, 6 * D])
            g1 = sbt(st, "g1", [128, D])
            g2 = sbt(st, "g2", [128, D])
            aw = [sbt(st, "aw%d" % i, [128, 16, 512], BF16) for i in range(3)]
            res = [sbt(st, "ares%d" % i, [128, 512]) for i in range(4)]
            P.dma(cs[:, 0, :], c_fm, writes=["cs_a"], key="a0")
            P.dma(cs[:, 1, :], cc_fm, writes=["cs_b"], key="a0")
            P.dma(adab[:], ada_b.partition_broadcast(128), writes=["adab"])
            P.dma(g1[:], g1_d.partition_broadcast(128), writes=["g1"])
            P.dma(g2[:], g2_d.partition_broadcast(128), writes=["g2"])
            P.op("act", lambda e: e.activation(cs[:], cs[:], AF.Silu), reads=["cs_a", "cs_b"], writes=["cs"])
            P.op("dve", lambda e: e.tensor_copy(csb[:], cs[:].unsqueeze(3).to_broadcast([128, 2, 16, 128])),
                 reads=["cs"], writes=["csb"])
            ri = 0
            for j in range(6):
                for blk in range(4):
                    col0 = j * D + blk * 512
                    sl = (j * 4 + blk) % 3
                    for k0 in range(0, 16, 8):
                        P.dma(aw[sl][:, k0:k0 + 8, :],
                              ada_w[k0 * 128:(k0 + 8) * 128, col0:col0 + 512].rearrange("(k p) n -> p k n", p=128),
                              writes=["aw%d_%d" % (sl, k0)], key=("aw", sl), queue="pool")
                    for which in range(2 if j < 2 else 1):
                        b = ri % 4
                        r = res[ri % 4]
                        rn = "ares%d" % (ri % 4)
                        ri += 1
                        for kc in range(16):
                            P.op("pe", lambda e, b=b, kc=kc, sl=sl, which=which: e.matmul(
                                ps[b][:], lhsT=csb[:, which, kc, :], rhs=aw[sl][:, kc, :], start=(kc == 0), stop=(kc == 15)),
                                reads=["csb"] + ["aw%d_%d" % (sl, k0) for k0 in range(0, 16, 8)], writes=["ps%d" % b])
                        P.op("dve", lambda e, b=b, r=r, col0=col0: e.tensor_tensor(r[:], ps[b][:], adab[:, col0:col0 + 512], ALU.add),
                             reads=["ps%d" % b, "adab"], writes=[rn])
                        if j in (1, 4):
                            g = g1 if j == 1 else g2
                            P.op("dve", lambda e, r=r, g=g, blk=blk: e.scalar_tensor_tensor(
                                out=r[:], in0=r[:], scalar=1.0, in1=g[:, blk * 512:(blk + 1) * 512], op0=ALU.add, op1=ALU.mult),
                                reads=[rn, "g1", "g2"], writes=[rn])
                        if which == 0:
                            mi = {0: 1, 1: 0, 2: 2, 3: 4, 4: 3, 5: 5}[j]
                        else:
                            mi = {0: 7, 1: 6}[j]
                        P.dma(MOD[mi, :, blk * 512:(blk + 1) * 512], r[:], reads=[rn], writes=["MODd"], key="st")
        P.barrier()

        with ExitStack() as st:
            uT = sbt(st, "uT", [128, 16, NTOK], BF16)
            with ExitStack() as st1:
                A1 = sbt(st1, "A1", [128, D]); S1 = sbt(st1, "S1", [128, D])
                Ac = sbt(st1, "Ac", [128, D]); Sc = sbt(st1, "Sc", [128, D])
                P.dma(A1[:], MOD[0], writes=["A1"])
                P.dma(S1[:], MOD[1], writes=["S1"])
                P.dma(Ac[:], MOD[6], writes=["Ac"])
                P.dma(Sc[:], MOD[7], writes=["Sc"])
                norm_mod_T(st1,
                           lambda t: x_c[t * 128:(t + 1) * 128, :] if t < 16 else ctx_c[(t - 16) * 128:(t - 15) * 128, :],
                           NT_ALL,
                           lambda t: (A1, "A1") if t < 16 else (Ac, "Ac"),
                           lambda t: (S1, "S1") if t < 16 else (Sc, "Sc"),
                           uT, "n1")
            P.barrier()
            wb = [sbt(st, "wb%d" % i, [128, 16, 512], BF16) for i in range(2)]
            stg = [sbt(st, "stg%d" % i, [128, 512]) for i in range(4)]
            sti = {"n": 0}

            def store_T(dst_of):
                def consume(pst, psn, t):
                    i = sti["n"] % 4
                    sti["n"] += 1
                    dst = dst_of(t)
                    ncols = dst.shape[1]
                    copy_op(evac_eng(), stg[i][:, 0:ncols], pst[:, 0:ncols], reads=[psn], writes=["stg%d" % i])
                    P.dma(dst, stg[i][:, 0:ncols], reads=["stg%d" % i], key="st")
                return consume

            def store_F(dst_of):
                def consume(pst, psn, ch, t0, nt):
                    i = sti["n"] % 4
                    sti["n"] += 1
                    copy_op(evac_eng(), stg[i][:, 0:nt], pst[:, 0:nt], reads=[psn], writes=["stg%d" % i])
                    P.dma(dst_of(ch, t0, nt), stg[i][:, 0:nt], reads=["stg%d" % i], key="st")
                return consume

            ALLT = list(range(NT_ALL))
            OWNT = list(range(NT_OWN))
            KVT = list(range(9)) + [16, 17]
            ALLG = [(0, 512), (512, 512), (1024, 512), (1536, 512), (2048, 256)]
            OWNG = [(0, 512), (512, 512)]
            blocks = []
            blocks.append((w_in, C_K, 512, "T", KVT, store_T(lambda t: KV[t * 128:(t + 1) * 128, 0:512])))
            blocks.append((w_in, C_V, 512, "T", KVT, store_T(lambda t: KV[t * 128:(t + 1) * 128, 512:1024])))
            blocks.append((w_dt, 0, 128, "T", ALLT, store_T(lambda t: DTs[t * 128:(t + 1) * 128, :])))
            for bi in range(12):
                blocks.append((w_in, C_XBC + bi * 512, 512, "F", ALLG,
                               store_F(lambda ch, t0, nt, bi=bi: XBCT[bi * 512 + ch * 128: bi * 512 + (ch + 1) * 128, t0:t0 + nt])))
            for bi in range(4):
                blocks.append((w_in, C_Q + bi * 512, 512, "T", OWNT,
                               store_T(lambda t, bi=bi: Qs[t * 128:(t + 1) * 128, bi * 512:(bi + 1) * 512])))
            for bi in range(8):
                blocks.append((w_in, C_Z + bi * 512, 512, "T", OWNT,
                               store_T(lambda t, bi=bi: Zs[t * 128:(t + 1) * 128, bi * 512:(bi + 1) * 512])))
            for bi in range(8):
                blocks.append((w_in, C_G + bi * 512, 512, "F", OWNG,
                               store_F(lambda ch, t0, nt, bi=bi: GTs[bi * 512 + ch * 128: bi * 512 + (ch + 1) * 128, t0:t0 + nt])))
            for i, (Wd, col0, ncols, orient, toks, cons) in enumerate(blocks):
                sl = i % 2
                load_w_block(wb, sl, Wd, 16, col0, ncols)
                if orient == "T":
                    linear_T(uT, "xT_n1", 16, wb, sl, ncols, toks, cons)
                else:
                    linear_F(uT, "xT_n1", 16, wb, sl, ncols, toks, cons)
        P.barrier()

        with ExitStack() as st:
            kT = sbt(st, "kT", [128, 4, 11 * 128], BF16)
            Vt = sbt(st, "Vt", [128, 11, 512], BF16)
            qT = sbt(st, "qT", [128, 16, 1024], BF16)
            aT = sbt(st, "aT", [128, 16, 1024], BF16)
            esink = sbt(st, "esink", [128, 16])
            cosb = [sbt(st, "cosb%d" % i, [128, 2048]) for i in range(2)]
            sinb = [sbt(st, "sinb%d" % i, [128, 2048]) for i in range(2)]
            kin = [sbt(st, "kin%d" % i, [128, 1024]) for i in range(2)]
            qin = [sbt(st, "qin%d" % i, [128, 2048]) for i in range(2)]
            r1 = sbt(st, "r1", [128, 2048]); r2 = sbt(st, "r2", [128, 2048])
            rb = [sbt(st, "rb%d" % i, [128, 2048], BF16) for i in range(2)]
            P.dma(esink[:], sink_d.partition_broadcast(128), writes=["esink"])
            P.op("act", lambda e: e.activation(esink[:], esink[:], AF.Exp), reads=["esink"], writes=["esink"])

            def rope(src, nh, sl, out_bf, rn_src, rn_out):
                n = nh * 128
                v = lambda ap: ap.rearrange("p (g b c) -> p g b c", g=nh * 2, b=2, c=32)
                P.op("dve", lambda e: e.tensor_tensor(r1[:, 0:n], src, cosb[sl][:, 0:n], ALU.mult),
                     reads=[rn_src, "cosb%d" % sl], writes=["r1"])
                P.op("pool", lambda e: e.tensor_tensor(v(r2[:, 0:n])[:, :, 0, :], v(src)[:, :, 1, :], v(sinb[sl][:, 0:n])[:, :, 0, :], ALU.mult),
                     reads=[rn_src, "sinb%d" % sl], writes=["r2a"])
                P.op("pool", lambda e: e.tensor_tensor(v(r2[:, 0:n])[:, :, 1, :], v(src)[:, :, 0, :], v(sinb[sl][:, 0:n])[:, :, 1, :], ALU.mult),
                     reads=[rn_src, "sinb%d" % sl], writes=["r2b"])
                P.op("dve", lambda e: e.tensor_tensor(out_bf, r1[:, 0:n], r2[:, 0:n], ALU.add),
                     reads=["r1", "r2a", "r2b"], writes=[rn_out])

            for blk in range(11):
                sl = blk % 2
                row0 = blk * 128 if blk < 9 else S + (blk - 9) * 128
                P.dma(kin[sl][:], KV[row0:row0 + 128, :], writes=["kin%d" % sl], key=("kin", sl))
                copy_op("act", Vt[:, blk, :], kin[sl][:, 512:1024], reads=["kin%d" % sl], writes=["Vt"])
                if blk < 9:
                    P.dma(cosb[sl][:], cos_d[blk * 128:(blk + 1) * 128, :], writes=["cosb%d" % sl], key=None)
                    P.dma(sinb[sl][:], sin_d[blk * 128:(blk + 1) * 128, :], writes=["sinb%d" % sl], key=None)
                    rope(kin[sl][:, 0:512], 4, sl, rb[sl][:, 0:512], "kin%d" % sl, "rb%d" % sl)
                else:
                    copy_op("dve", rb[sl][:, 0:512], kin[sl][:, 0:512], reads=["kin%d" % sl], writes=["rb%d" % sl])
                for g in range(4):
                    P.op("pe", lambda e, sl=sl, g=g: e.transpose(psb[0][:, g * 128:(g + 1) * 128], rb[sl][:, g * 128:(g + 1) * 128], identb[:]),
                         reads=["rb%d" % sl, "identb"], writes=["psb0"])
                copy_op(evac_eng(), kT[:, :, blk * 128:(blk + 1) * 128], psb[0][:, 0:512].rearrange("p (g t) -> p g t", g=4),
                        reads=["psb0"], writes=["kT"])
            for t in range(8):
                sl = t % 2
                P.dma(qin[sl][:], Qs[t * 128:(t + 1) * 128, :], writes=["qin%d" % sl], key=("qin", sl))
                P.dma(cosb[sl][:], cos_d[t * 128:(t + 1) * 128, :], writes=["cosb%d" % sl], key=None)
                P.dma(sinb[sl][:], sin_d[t * 128:(t + 1) * 128, :], writes=["sinb%d" % sl], key=None)
                rope(qin[sl][:], 16, sl, rb[sl][:], "qin%d" % sl, "rb%d" % sl)
                for half in range(2):
                    for j in range(8):
                        hh = half * 8 + j
                        P.op("pe", lambda e, sl=sl, hh=hh, half=half, j=j: e.transpose(
                            psb[half][:, j * 128:(j + 1) * 128], rb[sl][:, hh * 128:(hh + 1) * 128], identb[:]),
                            reads=["rb%d" % sl, "identb"], writes=["psb%d" % half])
                    copy_op(evac_eng(), qT[:, half * 8:(half + 1) * 8, t * 128:(t + 1) * 128],
                            psb[half][:].rearrange("p (k t) -> p k t", k=8), reads=["psb%d" % half], writes=["qT"])
            pT = [sbt(st, "pT%d" % i, [128, 512], BF16) for i in range(4)]
            den = [sbt(st, "den%d" % i, [128, 512]) for i in range(2)]
            units = []
            for qb in range(8):
                for g in range(4):
                    kbs = []
                    if qb >= 1:
                        kbs.append((qb - 1, triBb, "triBb"))
                    kbs.append((qb, None, None))
                    kbs.append((qb + 1, triFb, "triFb"))
                    kbs.append((9, None, None))
                    kbs.append((10, None, None))
                    for ki, (kb, mask, mname) in enumerate(kbs):
                        units.append((qb, g, ki, len(kbs), kb, mask, mname))

            def stageS(u):
                qb, g, ki, n, kb, mask, mname = units[u]
                sb_ = u % 2
                pp = u % 4
                P.op("pe", lambda e: e.matmul(ps[sb_][:], lhsT=kT[:, g, kb * 128:(kb + 1) * 128],
                                              rhs=qT[:, 4 * g:4 * g + 4, qb * 128:(qb + 1) * 128], start=True, stop=True),
                     reads=["kT", "qT"], writes=["ps%d" % sb_])
                P.op("act", lambda e: e.activation(pT[pp][:], ps[sb_][:], AF.Exp, scale=SCALE),
                     reads=["ps%d" % sb_], writes=["pT%d" % pp])
                if mask is not None:
                    P.op("pool", lambda e: e.tensor_tensor(
                        pT[pp][:].rearrange("p (r q) -> p r q", r=4), pT[pp][:].rearrange("p (r q) -> p r q", r=4),
                        mask[:].unsqueeze(1).to_broadcast([128, 4, 128]), ALU.mult),
                        reads=["pT%d" % pp, mname], writes=["pT%d" % pp])

            def stageV(u):
                qb, g, ki, n, kb, mask, mname = units[u]
                pp = u % 4
                par = (qb * 4 + g) % 2
                pa, pb = (2, 3) if par == 0 else (4, 5)
                P.op("pe", lambda e: e.matmul(ps[pa][:], lhsT=Vt[:, kb, g * 128:(g + 1) * 128], rhs=pT[pp][:],
                                              start=(ki == 0), stop=(ki == n - 1)),
                     reads=["Vt", "pT%d" % pp], writes=["ps%d" % pa])
                P.op("pe", lambda e: e.matmul(ps[pb][:], lhsT=onesb[:], rhs=pT[pp][:], start=(ki == 0), stop=(ki == n - 1)),
                     reads=["onesb", "pT%d" % pp], writes=["ps%d" % pb])
                if ki == n - 1:
                    dn = den[par]
                    P.op("dve", lambda e: e.tensor_tensor(dn[:].rearrange("p (r q) -> p r q", r=4),
                                                          ps[pb][:].rearrange("p (r q) -> p r q", r=4),
                                                          esink[:, 4 * g:4 * g + 4].unsqueeze(2).to_broadcast([128, 4, 128]), ALU.add),
                         reads=["ps%d" % pb, "esink"], writes=["den%d" % par])
                    P.op("dve", lambda e: e.reciprocal(dn[:], dn[:]), reads=["den%d" % par], writes=["den%d" % par])
                    P.op("dve", lambda e: e.tensor_tensor(aT[:, 4 * g:4 * g + 4, qb * 128:(qb + 1) * 128],
                                                          ps[pa][:].rearrange("p (r q) -> p r q", r=4),
                                                          dn[:].rearrange("p (r q) -> p r q", r=4), ALU.mult),
                         reads=["ps%d" % pa, "den%d" % par], writes=["aT"])

            for i in range(len(units) + 2):
                if i < len(units):
                    stageS(i)
                if i >= 2:
                    stageV(i - 2)
            P.dma(ATT, aT[:], reads=["aT"], key="st")
        P.barrier()

        with ExitStack() as st:
            cw = sbt(st, "cw", [128, 48, 5]); cb = sbt(st, "cb", [128, 48])
            P.dma(cw[:], convw, writes=["cw"])
            P.dma(cb[:], convb, writes=["cb"])
            cin = [sbt(st, "cin%d" % i, [128, S + 4 + CTXL + 4], BF16) for i in range(2)]
            dg = [sbt(st, "dg%d" % i, [128, 5, 128], BF16) for i in range(2)]
            cout = sbt(st, "cout", [128, 8, NTOK], BF16)
            tstg = [sbt(st, "tstg%d" % i, [128, 1024], BF16) for i in range(2)]
            for i in range(2):
                P.op("pool", lambda e, i=i: e.memset(cin[i][:], 0.0), writes=["cin%d" % i])
            CG = [(0, 0, 512), (512, 512, 512), (1024, 1024, 512), (1536, 1536, 512), (S, S + 4, CTXL)]
            pc = 0
            for grp in range(6):
                for c8 in range(8):
                    cc = grp * 8 + c8
                    sl = cc % 2
                    P.dma(cin[sl][:, 2:2 + S], XBCT[cc * 128:(cc + 1) * 128, 0:S], writes=["cin%d_a" % sl], key=("cin", sl), queue="pool")
                    P.dma(cin[sl][:, S + 6:S + 6 + CTXL], XBCT[cc * 128:(cc + 1) * 128, S:NTOK], writes=["cin%d_b" % sl], key=("cin", sl), queue="pool")
                    for j in range(5):
                        P.op("dve", lambda e, sl=sl, cc=cc, j=j: e.tensor_scalar(dg[sl][:, j, :], identb[:], cw[:, cc, j:j + 1], None, op0=ALU.mult),
                             reads=["identb", "cw"], writes=["dg%d" % sl])
                    for (o0, i0, L) in CG:
                        bnk = pc % 4
                        pc += 1
                        for j in range(5):
                            P.op("pe", lambda e, sl=sl, j=j, i0=i0, L=L, bnk=bnk: e.matmul(
                                ps[bnk][:, 0:L], lhsT=dg[sl][:, j, :], rhs=cin[sl][:, i0 + j:i0 + j + L], start=(j == 0), stop=(j == 4)),
                                reads=["dg%d" % sl, "cin%d" % sl, "cin%d_a" % sl, "cin%d_b" % sl], writes=["ps%d" % bnk])
                        P.op("act", lambda e, cc=cc, c8=c8, o0=o0, L=L, bnk=bnk: e.activation(
                            cout[:, c8, o0:o0 + L], ps[bnk][:, 0:L], AF.Silu, bias=cb[:, cc:cc + 1]),
                            reads=["ps%d" % bnk, "cb"], writes=["cout"])
                if grp < 5:
                    for t in range(NT_ALL):
                        hb = t % 2
                        for c8 in range(8):
                            P.op("pe", lambda e, hb=hb, c8=c8, t=t: e.transpose(
                                psb[hb][:, c8 * 128:(c8 + 1) * 128], cout[:, c8, t * 128:(t + 1) * 128], identb[:]),
                                reads=["cout", "identb"], writes=["psb%d" % hb])
                        copy_op(evac_eng(), tstg[hb][:], psb[hb][:], reads=["psb%d" % hb], writes=["tstg%d" % hb])
                        P.dma(XTM[t * 128:(t + 1) * 128, grp * 1024:(grp + 1) * 1024], tstg[hb][:], reads=["tstg%d" % hb], key="st")
                if grp >= 4:
                    P.dma(BCT[(grp - 4) * 1024:(grp - 3) * 1024, :].rearrange("(c p) t -> p c t", p=128), cout[:], reads=["cout"], key="st2")
        P.barrier()

        with ExitStack() as st:
            hst = [sbt(st, "hst%d" % i, [128, DI]) for i in range(2)]
            hbf = sbt(st, "hbf", [128, DI], BF16)
            xtm = [sbt(st, "xtm%d" % i, [128, 5120], BF16) for i in range(2)]
            bct = [sbt(st, "bct%d" % i, [128, 16, 128], BF16) for i in range(2)]
            dtr = [sbt(st, "dtr%d" % i, [128, 128]) for i in range(2)]
            dtbb = sbt(st, "dtbb", [128, 128]); Ab = sbt(st, "Ab", [128, 128]); Dd = sbt(st, "Dd", [128, 64])
            dtL = [sbt(st, "dt%d" % i, [128, 64]) for i in range(2)]; aaL = [sbt(st, "aa%d" % i, [128, 64]) for i in range(2)]
            acsL = [sbt(st, "acs%d" % i, [128, 64]) for i in range(2)]; atotL = [sbt(st, "atot%d" % i, [128, 64]) for i in range(2)]
            wendL = [sbt(st, "wend%d" % i, [128, 64]) for i in range(2)]; eacsL = [sbt(st, "eacs%d" % i, [128, 64]) for i in range(2)]
            etotL = [sbt(st, "etot%d" % i, [128, 64]) for i in range(2)]
            wxL = [sbt(st, "wx%d" % i, [128, 128]) for i in range(2)]; wxeL = [sbt(st, "wxe%d" % i, [128, DI], BF16) for i in range(2)]
            TA = sbt(st, "TA", [128, 8, 128]); seg = sbt(st, "seg", [128, 8, 128])
            Mt2 = [sbt(st, "Mt%d" % i, [128, 8, 128], BF16) for i in range(2)]; CBa = sbt(st, "CBa", [128, 8, 128]); segL = [sbt(st, "segd%d" % i, [128, 8, 128]) for i in range(2)]
            yt = sbt(st, "yt", [128, DI]); ytmp = sbt(st, "ytmp", [128, 512])
            P.dma(dtbb[:], dtb_d.partition_broadcast(128), writes=["dtbb"])
            P.dma(Ab[:], alog_d.partition_broadcast(128), writes=["Ab"])
            P.dma(Dd[:], ssmd_d.partition_broadcast(128), writes=["Dd"])
            P.op("act", lambda e: e.activation(Ab[:], Ab[:], AF.Exp), reads=["Ab"], writes=["Ab"])
            P.op("dve", lambda e: e.tensor_scalar(Ab[:], Ab[:], -1.0, None, op0=ALU.mult), reads=["Ab"], writes=["Ab"])
            for i in range(2):
                P.op("pool", lambda e, i=i: e.memset(hst[i][:], 0.0), writes=["hst%d" % i])
            DIb = sbt(st, "DIb", [128, 64, 128], BF16)
            P.op("dve", lambda e: e.tensor_tensor(DIb[:], ident[:].unsqueeze(1).to_broadcast([128, 64, 128]),
                                                  Dd[:].unsqueeze(2).to_broadcast([128, 64, 128]), ALU.mult),
                 reads=["ident", "Dd"], writes=["DIb"])
            ldi = {"n": 0}

            def setup(tile, d, full, sl):
                tri = triF if d == 0 else triB
                trin = "triF" if d == 0 else "triB"
                h = hst[d]; hn = "hst%d" % d
                dt = dtL[sl]
                aa = aaL[sl]
                acs = acsL[sl]
                atot = atotL[sl]
                wend = wendL[sl]
                eacs = eacsL[sl]
                etot = etotL[sl]
                wx = wxL[sl]
                wxe = wxeL[sl]
                P.dma(xtm[sl][:], XTM[tile * 128:(tile + 1) * 128, :], writes=["xtm%d" % sl], key=None)
                yield
                P.dma(dtr[sl][:], DTs[tile * 128:(tile + 1) * 128, :], writes=["dtr%d" % sl], key=None)
                yield
                if full:
                    P.dma(bct[sl][:], BCT[:, tile * 128:(tile + 1) * 128].rearrange("(c p) t -> p c t", p=128),
                          writes=["bct%d" % sl], key=None)
                    yield
                P.op("dve", lambda e: e.tensor_tensor(dt[:], dtr[sl][:, d * 64:(d + 1) * 64], dtbb[:, d * 64:(d + 1) * 64], ALU.add),
                     reads=["dtr%d" % sl, "dtbb"], writes=["dt%d" % sl])
                yield
                P.op("act", lambda e: e.activation(dt[:], dt[:], AF.Exp), reads=["dt%d" % sl], writes=["dt%d" % sl])
                yield
                P.op("act", lambda e: e.activation(dt[:], dt[:], AF.Ln, bias=cst[:, 0:1]), reads=["dt%d" % sl, "cst0"], writes=["dt%d" % sl])
                yield
                P.op("dve", lambda e: e.tensor_tensor(aa[:], dt[:], Ab[:, d * 64:(d + 1) * 64], ALU.mult), reads=["dt%d" % sl, "Ab"], writes=["aa%d" % sl])
                yield
                P.op("pe", lambda e: e.matmul(ps[4][:, 0:64], lhsT=tri[:], rhs=aa[:], start=True, stop=True),
                     reads=[trin, "aa%d" % sl], writes=["ps4"])
                yield
                P.op("pe", lambda e: e.matmul(ps[4][:, 64:128], lhsT=ones[:], rhs=aa[:], start=True, stop=True),
                     reads=["ones", "aa%d" % sl], writes=["ps4"])
                yield
                P.op("dve", lambda e: e.tensor_copy(acs[:], ps[4][:, 0:64]), reads=["ps4"], writes=["acs%d" % sl])
                yield
                P.op("dve", lambda e: e.tensor_copy(atot[:], ps[4][:, 64:128]), reads=["ps4"], writes=["atot%d" % sl])
                yield
                P.op("dve", lambda e: e.tensor_tensor(wend[:], atot[:], acs[:], ALU.subtract), reads=["atot%d" % sl, "acs%d" % sl], writes=["wend%d" % sl])
                yield
                P.op("act", lambda e: e.activation(wend[:], wend[:], AF.Exp), reads=["wend%d" % sl], writes=["wend%d" % sl])
                yield
                P.op("act", lambda e: e.activation(etot[:], atot[:], AF.Exp), reads=["atot%d" % sl], writes=["etot%d" % sl])
                yield
                v3 = lambda ap: ap.rearrange("p (h q) -> p h q", h=64)
                bc = lambda ap: ap.unsqueeze(2).to_broadcast([128, 64, 64])
                P.op("dve", lambda e: e.tensor_tensor(wend[:], wend[:], dt[:], ALU.mult), reads=["wend%d" % sl, "dt%d" % sl], writes=["wend%d" % sl])
                yield
                P.op("dve", lambda e: e.tensor_tensor(v3(wxe[:]), v3(xtm[sl][:, 0:DI]), bc(wend[:]), ALU.mult),
                     reads=["xtm%d" % sl, "wend%d" % sl], writes=["wxe%d" % sl])
                yield
                if full:
                    P.op("act", lambda e: e.activation(wx[:, 0:64], dt[:], AF.Ln), reads=["dt%d" % sl], writes=["lndt%d" % sl])
                    yield
                    P.op("dve", lambda e: e.tensor_tensor(wx[:, 64:128], acs[:], wx[:, 0:64], ALU.subtract),
                         reads=["acs%d" % sl, "lndt%d" % sl], writes=["acs2%d" % sl])
                    yield
                if full:
                    P.op("act", lambda e: e.activation(eacs[:], acs[:], AF.Exp), reads=["acs%d" % sl], writes=["eacs%d" % sl])
                    yield

            def body(tile, d, full, sl, pump):
                tri = triF if d == 0 else triB
                trin = "triF" if d == 0 else "triB"
                h = hst[d]; hn = "hst%d" % d
                dt = dtL[sl]
                aa = aaL[sl]
                acs = acsL[sl]
                atot = atotL[sl]
                wend = wendL[sl]
                eacs = eacsL[sl]
                etot = etotL[sl]
                wx = wxL[sl]
                wxe = wxeL[sl]
                v3 = lambda ap: ap.rearrange("p (h q) -> p h q", h=64)
                bc = lambda ap: ap.unsqueeze(2).to_broadcast([128, 64, 64])
                if full:
                    P.op("dve", lambda e: e.tensor_copy(hbf[:], h[:]), reads=[hn], writes=["hbf"])
                    for hf in range(2):
                        for j in range(4):
                            g = 4 * hf + j
                            P.op("pe", lambda e, g=g, j=j: e.matmul(ps[5][:, j * 128:(j + 1) * 128], lhsT=bct[sl][:, g, :], rhs=bct[sl][:, 8 + g, :],
                                                                    start=True, stop=True), reads=["bct%d" % sl], writes=["ps5"])
                        P.op("dve", lambda e, hf=hf: e.tensor_tensor(CBa[:, 4 * hf:4 * hf + 4, :], ps[5][:].rearrange("p (j l) -> p j l", j=4),
                                                                     tri[:].unsqueeze(1).to_broadcast([128, 4, 128]), ALU.mult),
                             reads=["ps5", trin], writes=["CBa%d" % hf])

                    def s_TA(g):
                        for r in range(8):
                            hh = 8 * g + r
                            P.op("pe", lambda e, r=r, hh=hh: e.matmul(ps[r // 4][:, (r % 4) * 128:(r % 4 + 1) * 128],
                                                                       lhsT=aa[:, hh:hh + 1].to_broadcast([128, 128]), rhs=tri[:],
                                                                       start=True, stop=True),
                                 reads=[trin, "aa%d" % sl], writes=["ps%d" % (r // 4)])

                    def s_seg(g):
                        m = g % 2
                        for hf in range(2):
                            P.op("dve", lambda e, hf=hf: e.tensor_tensor(
                                segL[m][:, 4 * hf:4 * hf + 4, :], ps[hf][:].rearrange("p (r l) -> p r l", r=4),
                                wx[:, 64 + 8 * g + 4 * hf:64 + 8 * g + 4 * hf + 4].unsqueeze(2).to_broadcast([128, 4, 128]), ALU.subtract),
                                reads=["ps%d" % hf, "acs2%d" % sl], writes=["seg%d_%d" % (m, hf)])
                        P.op("act", lambda e: e.activation(segL[m][:], segL[m][:], AF.Exp),
                             reads=["seg%d_0" % m, "seg%d_1" % m], writes=["seg%d_0" % m, "seg%d_1" % m])

                    def s_M(g):
                        m = g % 2
                        P.op("dve", lambda e: e.scalar_tensor_tensor(out=Mt2[m][:], in0=segL[m][:], scalar=1e30,
                                                                     in1=CBa[:, g, :].unsqueeze(1).to_broadcast([128, 8, 128]),
                                                                     op0=ALU.min, op1=ALU.mult),
                             reads=["seg%d_0" % m, "seg%d_1" % m, "CBa%d" % (g // 4)], writes=["Mt%d" % m])
                        for r in range(8):
                            hh = 8 * g + r
                            P.op("pe", lambda e, r=r, hh=hh: e.matmul(ps[2][:, r * 64:(r + 1) * 64], lhsT=Mt2[m][:, r, :],
                                                                       rhs=xtm[sl][:, hh * 64:(hh + 1) * 64], start=True, stop=(d != 0)),
                                 reads=["Mt%d" % m, "xtm%d" % sl], writes=["ps2"])
                            if d == 0:
                                P.op("pe", lambda e, r=r, hh=hh: e.matmul(ps[2][:, r * 64:(r + 1) * 64], lhsT=DIb[:, hh, :],
                                                                           rhs=xtm[sl][:, hh * 64:(hh + 1) * 64], start=False, stop=True),
                                     reads=["DIb", "xtm%d" % sl], writes=["ps2"])
                        P.op("pe", lambda e: e.matmul(ps[3][:], lhsT=bct[sl][:, 8 + g, :], rhs=hbf[:, g * 512:(g + 1) * 512],
                                                      start=True, stop=True), reads=["bct%d" % sl, "hbf"], writes=["ps3"])

                    def s_Y(g):
                        P.op("dve", lambda e: e.tensor_tensor(ytmp[:].rearrange("p (r q) -> p r q", r=8),
                                                              ps[3][:].rearrange("p (r q) -> p r q", r=8),
                                                              eacs[:, 8 * g:8 * g + 8].unsqueeze(2).to_broadcast([128, 8, 64]), ALU.mult),
                             reads=["ps3", "eacs%d" % sl], writes=["ytmp"])
                        P.op("dve", lambda e: e.tensor_tensor(yt[:, g * 512:(g + 1) * 512], ps[2][:], ytmp[:], ALU.add),
                             reads=["ps2", "ytmp"], writes=["yt"])

                    for k in range(-2, 9):
                        if 0 <= k - 1 < 8:
                            s_Y(k - 1)
                        if 0 <= k + 1 < 8:
                            s_seg(k + 1)
                        if 0 <= k + 2 < 8:
                            s_TA(k + 2)
                        pump(1)
                        if 0 <= k < 8:
                            s_M(k)
                        pump(1)
                    P.dma((Y1 if d == 0 else Y2)[tile * 128:(tile + 1) * 128, :], yt[:], reads=["yt"], key="st")
                for half in range(2):
                    pump(4)
                    for g4 in range(4):
                        g = half * 4 + g4
                        P.op("pe", lambda e, g=g, g4=g4: e.matmul(ps[g4][:], lhsT=xtm[sl][:, DI + g * 128:DI + (g + 1) * 128],
                                                                  rhs=wxe[:, g * 512:(g + 1) * 512], start=True, stop=True),
                             reads=["xtm%d" % sl, "wxe%d" % sl], writes=["ps%d" % g4])
                    for g4 in range(4):
                        g = half * 4 + g4
                        eng = "dve" if g4 % 2 == 0 else "pool"
                        P.op("dve", lambda e, g=g: e.tensor_tensor(h[:, g * 512:(g + 1) * 512].rearrange("p (r q) -> p r q", r=8),
                                                                   h[:, g * 512:(g + 1) * 512].rearrange("p (r q) -> p r q", r=8),
                                                                   etot[:, 8 * g:8 * g + 8].unsqueeze(2).to_broadcast([128, 8, 64]), ALU.mult),
                             reads=[hn, "etot%d" % sl, "hbf"], writes=[hn])
                        P.op("dve", lambda e, g=g, g4=g4: e.tensor_tensor(h[:, g * 512:(g + 1) * 512], h[:, g * 512:(g + 1) * 512],
                                                                          ps[g4][:], ALU.add),
                             reads=[hn, "ps%d" % g4], writes=[hn])

            seq = ([(16, 0, False), (17, 0, False)] + [(t, 0, True) for t in range(8)] + [(17, 1, False), (16, 1, False)]
                   + [(t, 1, False) for t in range(15, 7, -1)] + [(t, 1, True) for t in range(7, -1, -1)])
            gens = [setup(sq[0], sq[1], sq[2], i % 2) for i, sq in enumerate(seq)]
            for _ in gens[0]:
                pass
            for i, sq in enumerate(seq):
                nxt = gens[i + 1] if i + 1 < len(seq) else None

                def pump(k, nxt=nxt):
                    if nxt is not None:
                        for _ in range(k):
                            next(nxt, None)
                body(sq[0], sq[1], sq[2], i % 2, pump)
                if nxt is not None:
                    for _ in nxt:
                        pass
        P.barrier()

        with ExitStack() as st:
            mT = sbt(st, "mT", [128, 16, 1024], BF16)
            with ExitStack() as st5:
                ynT = sbt(st5, "ynT", [128, 32, 1024], BF16)
                with ExitStack() as st5a:
                    y1L = [sbt(st5a, "y1%d" % i, [128, DI]) for i in range(2)]; y2L = [sbt(st5a, "y2%d" % i, [128, DI]) for i in range(2)]
                    zz = sbt(st5a, "zz", [128, DI])
                    gN = sbt(st5a, "gN", [128, DI]); ynb = sbt(st5a, "ynb", [128, DI], BF16)
                    ss5 = sbt(st5a, "ss5", [128, 4])
                    P.dma(gN[:], gN_d.partition_broadcast(128), writes=["gN"])
                    for t in range(8):
                        y1 = y1L[t % 2]; y2 = y2L[t % 2]; y1n = "y1%d" % (t % 2); y2n = "y2%d" % (t % 2)
                        P.dma(y1[:], Y1[t * 128:(t + 1) * 128, :], writes=[y1n])
                        P.dma(y2[:], Y2[t * 128:(t + 1) * 128, :], writes=[y2n])
                        P.dma(zz[:], Zs[t * 128:(t + 1) * 128, :], writes=["zz"])
                        P.op("dve", lambda e, y1=y1, y2=y2: e.tensor_tensor(y1[:], y1[:], y2[:], ALU.add), reads=[y1n, y2n], writes=[y1n])
                        P.op("act", lambda e: e.activation(zz[:], zz[:], AF.Silu), reads=["zz"], writes=["zz"])
                        P.op("dve", lambda e, y1=y1: e.tensor_tensor(y1[:], y1[:], zz[:], ALU.mult), reads=[y1n, "zz"], writes=[y1n])
                        P.op("dve", lambda e: e.memset(ss5[:, 0:1], 0.0), writes=["ss5a"])
                        P.op("act", lambda e, y1=y1, y2=y2: e.activation(y2[:], y1[:], AF.Square, accum_out=ss5[:, 0:1]),
                             reads=[y1n, "ss5a"], writes=[y2n, "ss5a"])
                        P.op("act", lambda e: e.activation(ss5[:, 1:2], ss5[:, 0:1], AF.Sqrt, bias=cst[:, 1:2], scale=1.0 / DI),
                             reads=["ss5a"], writes=["ss5b"])
                        P.op("dve", lambda e: e.reciprocal(ss5[:, 2:3], ss5[:, 1:2]), reads=["ss5b"], writes=["ss5c"])
                        P.op("dve", lambda e, y1=y1: e.scalar_tensor_tensor(out=ynb[:], in0=y1[:], scalar=ss5[:, 2:3], in1=gN[:],
                                                                     op0=ALU.mult, op1=ALU.mult),
                             reads=[y1n, "ss5c", "gN"], writes=["ynb"])
                        for q in range(4):
                            hb = q % 2
                            for j in range(8):
                                kc = q * 8 + j
                                P.op("pe", lambda e, hb=hb, j=j, kc=kc: e.transpose(
                                    psb[hb][:, j * 128:(j + 1) * 128], ynb[:, kc * 128:(kc + 1) * 128], identb[:]),
                                    reads=["ynb", "identb"], writes=["psb%d" % hb])
                            copy_op(evac_eng(), ynT[:, q * 8:(q + 1) * 8, t * 128:(t + 1) * 128],
                                    psb[hb][:].rearrange("p (k t) -> p k t", k=8), reads=["psb%d" % hb], writes=["ynT"])
                P.barrier()
                aTs = sbt(st5, "aTs", [128, 16, 1024], BF16)
                P.dma(aTs[:], ATT, writes=["aTs"])
                wbsL = [sbt(st5, "wbs%d" % i, [128, 32, 256], BF16) for i in range(2)]
                wbaL = [sbt(st5, "wba%d" % i, [128, 16, 256], BF16) for i in range(2)]
                gsL = [sbt(st5, "gs%d" % i, [128, 512]) for i in range(2)]; gaL = [sbt(st5, "ga%d" % i, [128, 512]) for i in range(2)]
                m1L = [sbt(st5, "m1%d" % i, [128, 512]) for i in range(2)]; m2L = [sbt(st5, "m2%d" % i, [128, 512]) for i in range(2)]
                for blk in range(8):
                    wbs = wbsL[blk % 2]; wba = wbaL[blk % 2]
                    wbsr = ["wbs%d_%d" % (blk % 2, k0) for k0 in range(0, 32, 4)]
                    wbar = ["wba%d_%d" % (blk % 2, k0) for k0 in range(0, 16, 4)]
                    for k0 in range(0, 32, 4):
                        P.dma(wbs[:, k0:k0 + 4, :], w_bs[k0 * 128:(k0 + 4) * 128, blk * 256:(blk + 1) * 256].rearrange("(k p) n -> p k n", p=128),
                              writes=["wbs%d_%d" % (blk % 2, k0)], key=("wbs", blk % 2), queue="pool")
                    for k0 in range(0, 16, 4):
                        P.dma(wba[:, k0:k0 + 4, :], w_ba[k0 * 128:(k0 + 4) * 128, blk * 256:(blk + 1) * 256].rearrange("(k p) n -> p k n", p=128),
                              writes=["wba%d_%d" % (blk % 2, k0)], key=("wba", blk % 2), queue="pool")
                    for ch in range(2):
                        cabs = blk * 2 + ch
                        for th in range(2):
                            t0 = th * 512
                            u5 = (cabs * 2 + th) % 2
                            gsu, gau, m1u, m2u = gsL[u5], gaL[u5], m1L[u5], m2L[u5]
                            pA, pB = 2 * u5, 2 * u5 + 1
                            P.dma(gsu[:], GTs[cabs * 128:(cabs + 1) * 128, t0:t0 + 512], writes=["gs%d" % u5])
                            P.dma(gau[:], GTs[2048 + cabs * 128:2048 + (cabs + 1) * 128, t0:t0 + 512], writes=["ga%d" % u5])
                            P.op("act", lambda e, gsu=gsu: e.activation(gsu[:], gsu[:], AF.Sigmoid), reads=["gs%d" % u5], writes=["gs%d" % u5])
                            P.op("act", lambda e, gau=gau: e.activation(gau[:], gau[:], AF.Sigmoid), reads=["ga%d" % u5], writes=["ga%d" % u5])
                            for kc in range(32):
                                P.op("pe", lambda e, kc=kc, ch=ch, t0=t0, wbs=wbs, pA=pA: e.matmul(ps[pA][:], lhsT=wbs[:, kc, ch * 128:(ch + 1) * 128],
                                                                                  rhs=ynT[:, kc, t0:t0 + 512], start=(kc == 0), stop=(kc == 31)),
                                     reads=wbsr + ["ynT"], writes=["ps%d" % pA])
                            for kc in range(16):
                                P.op("pe", lambda e, kc=kc, ch=ch, t0=t0, wba=wba, pB=pB: e.matmul(ps[pB][:], lhsT=wba[:, kc, ch * 128:(ch + 1) * 128],
                                                                                  rhs=aTs[:, kc, t0:t0 + 512], start=(kc == 0), stop=(kc == 15)),
                                     reads=wbar + ["aTs"], writes=["ps%d" % pB])
                            P.op("dve", lambda e, m1u=m1u, gsu=gsu, pA=pA: e.tensor_tensor(m1u[:], ps[pA][:], gsu[:], ALU.mult),
                                 reads=["ps%d" % pA, "gs%d" % u5], writes=["m1%d" % u5])
                            P.op("dve", lambda e, m2u=m2u, gau=gau, pB=pB: e.tensor_tensor(m2u[:], ps[pB][:], gau[:], ALU.mult),
                                 reads=["ps%d" % pB, "ga%d" % u5], writes=["m2%d" % u5])
                            P.op("pool", lambda e, cabs=cabs, t0=t0, m1u=m1u, m2u=m2u: e.tensor_tensor(mT[:, cabs, t0:t0 + 512], m1u[:], m2u[:], ALU.add),
                                 reads=["m1%d" % u5, "m2%d" % u5], writes=["mT"])
            P.barrier()
            wo = [sbt(st, "wo%d" % i, [128, 16, 512], BF16) for i in range(2)]
            mod2 = sbt(st, "mod2", [128, D])
            xo = [sbt(st, "xo%d" % i, [128, 512]) for i in range(2)]
            ho = [sbt(st, "ho%d" % i, [128, 512]) for i in range(2)]
            P.dma(mod2[:], MOD[2], writes=["mod2"])
            hi = 0
            for blk in range(4):
                sl = blk % 2
                load_w_block(wo, sl, w_o, 16, blk * 512, 512)
                for t in range(8):
                    i = hi % 2
                    hi += 1
                    P.dma(xo[i][:], x_c[t * 128:(t + 1) * 128, blk * 512:(blk + 1) * 512], writes=["xo%d" % i], key=("xo", i))
                    b = hi % 2
                    for kc in range(16):
                        P.op("pe", lambda e, b=b, kc=kc, t=t, sl=sl: e.matmul(ps[b][:], lhsT=mT[:, kc, t * 128:(t + 1) * 128],
                                                                              rhs=wo[sl][:, kc, :], start=(kc == 0), stop=(kc == 15)),
                             reads=["mT"] + ["wb%d_%d" % (sl, k0) for k0 in range(0, 16, 4)], writes=["ps%d" % b])
                    P.op("dve", lambda e, b=b, i=i, blk=blk: e.tensor_tensor(ho[i][:], ps[b][:], mod2[:, blk * 512:(blk + 1) * 512], ALU.mult),
                         reads=["ps%d" % b, "mod2"], writes=["ho%d" % i])
                    P.op("pool", lambda e, i=i: e.tensor_tensor(ho[i][:], ho[i][:], xo[i][:], ALU.add),
                         reads=["ho%d" % i, "xo%d" % i], writes=["ho%d" % i])
                    P.dma(Hs[t * 128:(t + 1) * 128, blk * 512:(blk + 1) * 512], ho[i][:], reads=["ho%d" % i], key="st")
        P.barrier()

        with ExitStack() as st:
            u2T = sbt(st, "u2T", [128, 16, 1024], BF16)
            with ExitStack() as st6:
                A2 = sbt(st6, "A2", [128, D]); S3 = sbt(st6, "S3", [128, D])
                P.dma(A2[:], MOD[3], writes=["A2"])
                P.dma(S3[:], MOD[4], writes=["S3"])
                norm_mod_T(st6, lambda t: Hs[t * 128:(t + 1) * 128, :], 8, lambda t: (A2, "A2"), lambda t: (S3, "S3"), u2T, "n2")
            P.barrier()
            with ExitStack() as st6:
                q2T = sbt(st6, "q2T", [128, 16, 1024], BF16)
                with ExitStack() as st6w:
                    wq = [sbt(st6w, "wq%d" % i, [128, 16, 512], BF16) for i in range(2)]
                    for blk in range(4):
                        sl = blk % 2
                        load_w_block(wq, sl, w_q, 16, blk * 512, 512)

                        def cons(pst, psn, ch, t0, nt, blk=blk):
                            copy_op(evac_eng(), q2T[:, blk * 4 + ch, t0:t0 + nt], pst[:, 0:nt], reads=[psn], writes=["q2T"])
                        linear_F(u2T, "xT_n2", 16, wq, sl, 512, [(0, 512), (512, 512)], cons)
                P.barrier()
                kin6 = sbt(st6, "kin6", [128, 2, 128])
                kT6 = sbt(st6, "kT6", [128, 2, 128], BF16)
                P.dma(kin6[:, 0, :], keys1, writes=["kin6_a"], key="s6k")
                P.dma(kin6[:, 1, :], keys2, writes=["kin6_b"], key="s6k")
                for i in range(2):
                    P.op("pe", lambda e, i=i: e.transpose(ps[4][:, i * 128:(i + 1) * 128], kin6[:, i, :], ident[:]),
                         reads=["kin6_a", "kin6_b", "ident"], writes=["ps4"])
                P.op("dve", lambda e: e.tensor_copy(kT6[:], ps[4][:, 0:256].rearrange("p (i k) -> p i k", i=2)),
                     reads=["ps4"], writes=["kT6"])
                sc = sbt(st6, "sc", [128, 16, 128]); tmp6 = sbt(st6, "tmp6", [128, 256])
                mx = sbt(st6, "mx", [128, 16, 16]); negm = sbt(st6, "negm", [128, 16])
                E = sbt(st6, "E", [128, 16, 128]); Et = sbt(st6, "Et", [128, 16, 16])
                cand = sbt(st6, "cand", [128, 8, 256]); ctop = sbt(st6, "ctop", [128, 8, 16])
                Zs6 = sbt(st6, "Zs6", [128, 8]); rZ = sbt(st6, "rZ", [128, 8])
                Pd = [sbt(st6, "Pd%d" % i, [128, 8, 128]) for i in range(6)]
                G2 = [sbt(st6, "G2%d" % i, [128, 4, 8, 128], BF16) for i in range(2)]
                Gsb = [sbt(st6, "Gsb%d" % i, [128, 8, 128], BF16) for i in range(2)]
                dgt = [sbt(st6, "dgt%d" % i, [128, 8, 128], BF16) for i in range(2)]
                negthr = [sbt(st6, "negthr%d" % i, [128, 8]) for i in range(2)]
                Gm = [sbt(st6, "Gm%d" % i, [128, 8, 8, 128], BF16) for i in range(2)]
                gst = [sbt(st6, "gst%d" % i, [128, 8, 128], BF16) for i in range(2)]
                E1n = sbt(st6, "E1n", [128, 8, 128]); Etn = sbt(st6, "Etn", [128, 8, 16]); ctop2 = sbt(st6, "ctop2", [128, 8, 16])
                thrg = sbt(st6, "thrg", [128, 8])
                gi = {"n": 0}
                pub = [sbt(st6, "pub%d" % i, [128, D], BF16) for i in range(2)]
                puT = [sbt(st6, "puT%d" % i, [128, 16, 128], BF16) for i in range(2)]
                actb = [sbt(st6, "actb%d" % i, [128, 1024], BF16) for i in range(2)]

                def phaseA(ch):
                    sl = ch % 2
                    P.dma(pub[sl][:], pu[ch * 128:(ch + 1) * 128, :], writes=["pub%d" % sl], key=("pub", sl), queue="pool")
                    for half in range(2):
                        for j in range(8):
                            kc = half * 8 + j
                            P.op("pe", lambda e, kc=kc, half=half, j=j: e.transpose(
                                psb[half][:, j * 128:(j + 1) * 128], pub[sl][:, kc * 128:(kc + 1) * 128], identb[:]),
                                reads=["pub%d" % sl, "identb"], writes=["psb%d" % half])
                        copy_op("act", puT[sl][:, half * 8:(half + 1) * 8, :], psb[half][:].rearrange("p (k t) -> p k t", k=8),
                                reads=["psb%d" % half], writes=["puT%d" % sl])
                    for th in range(2):
                        for kc in range(16):
                            P.op("pe", lambda e, kc=kc, th=th: e.matmul(ps[4 + th][:], lhsT=puT[sl][:, kc, :],
                                                                        rhs=u2T[:, kc, th * 512:(th + 1) * 512],
                                                                        start=(kc == 0), stop=(kc == 15)),
                                 reads=["puT%d" % sl, "xT_n2"], writes=["ps%d" % (4 + th)])
                        P.op("act", lambda e, th=th: e.activation(actb[sl][:, th * 512:(th + 1) * 512], ps[4 + th][:], AF.Gelu),
                             reads=["ps%d" % (4 + th)], writes=["actb%d_%d" % (sl, th)])
                    P.dma(ACTs[ch * 128:(ch + 1) * 128, :], actb[sl][:], reads=["actb%d_0" % sl, "actb%d_1" % sl], key=("st", "actb%d" % sl))

                def g_all():
                    for t in range(8):
                        for c in range(16):
                            P.op("pe", lambda e, c=c, t=t: e.matmul(ps[c // 4][:, (c % 4) * 128:(c % 4 + 1) * 128],
                                                                    lhsT=q2T[:, c, t * 128:(t + 1) * 128], rhs=kT6[:, c % 2, :],
                                                                    start=True, stop=True), reads=["q2T", "kT6"], writes=["ps%d" % (c // 4)])
                        for b4 in range(4):
                            copy_op(evac_eng(), sc[:, b4 * 4:(b4 + 1) * 4, :], ps[b4][:].rearrange("p (c k) -> p c k", c=4),
                                    reads=["ps%d" % b4], writes=["sc"])
                        for c in range(16):
                            P.op("dve", lambda e, c=c: e.max(out=mx[:, c, 0:8], in_=sc[:, c, :]), reads=["sc"], writes=["mx"])
                            P.op("dve", lambda e, c=c: e.match_replace(out=tmp6[:, 0:128], in_to_replace=mx[:, c, 0:8], in_values=sc[:, c, :],
                                                                       imm_value=-1e30), reads=["sc", "mx"], writes=["tmp6"])
                            P.op("dve", lambda e, c=c: e.max(out=mx[:, c, 8:16], in_=tmp6[:, 0:128]), reads=["tmp6"], writes=["mx"])
                        P.op("dve", lambda e: e.tensor_scalar(negm[:], mx[:, :, 0], -1.0, None, op0=ALU.mult), reads=["mx"], writes=["negm"])
                        for c in range(16):
                            P.op("act", lambda e, c=c: e.activation(E[:, c, :], sc[:, c, :], AF.Exp, bias=negm[:, c:c + 1]),
                                 reads=["sc", "negm"], writes=["E"])
                            P.op("act", lambda e, c=c: e.activation(Et[:, c, :], mx[:, c, :], AF.Exp, bias=negm[:, c:c + 1]),
                                 reads=["mx", "negm"], writes=["Et"])
                        Et4 = Et[:].rearrange("p (h two) a -> p h two a", two=2)
                        P.op("dve", lambda e, Et4=Et4: e.tensor_tensor(cand[:].rearrange("p h (a b) -> p h a b", a=16),
                                                                       Et4[:, :, 0, :].unsqueeze(3).to_broadcast([128, 8, 16, 16]),
                                                                       Et4[:, :, 1, :].unsqueeze(2).to_broadcast([128, 8, 16, 16]), ALU.mult),
                             reads=["Et"], writes=["cand"])
                        for hh in range(8):
                            P.op("dve", lambda e, hh=hh: e.max(out=ctop[:, hh, 0:8], in_=cand[:, hh, :]), reads=["cand"], writes=["ctop"])
                            P.op("dve", lambda e, hh=hh: e.match_replace(out=tmp6[:], in_to_replace=ctop[:, hh, 0:8], in_values=cand[:, hh, :],
                                                                         imm_value=-1e30), reads=["cand", "ctop"], writes=["tmp6"])
                            P.op("dve", lambda e, hh=hh: e.max(out=ctop[:, hh, 8:16], in_=tmp6[:]), reads=["tmp6"], writes=["ctop"])
                        P.op("dve", lambda e: e.reduce_sum(Zs6[:], ctop[:], axis=AX.X), reads=["ctop"], writes=["Zs6"])
                        P.op("dve", lambda e: e.reciprocal(rZ[:], Zs6[:]), reads=["Zs6"], writes=["rZ"])
                        E4 = E[:].rearrange("p (h two) k -> p h two k", two=2)
                        P.op("dve", lambda e, E4=E4: e.tensor_tensor(E1n[:], E4[:, :, 0, :], rZ[:].unsqueeze(2).to_broadcast([128, 8, 128]), ALU.mult),
                             reads=["E", "rZ"], writes=["E1n"])
                        P.op("dve", lambda e: e.tensor_tensor(thrg[:], ctop[:, :, 15], rZ[:], ALU.mult), reads=["ctop", "rZ"], writes=["thrg"])
                        P.op("dve", lambda e: e.tensor_scalar(thrg[:], thrg[:], 1.0 - 1e-6, None, op0=ALU.mult), reads=["thrg"], writes=["thrg"])
                        tp = t % 2
                        P.op("dve", lambda e, tp=tp: e.tensor_scalar(negthr[tp][:], thrg[:], -1.0, None, op0=ALU.mult),
                             reads=["thrg"], writes=["negthr%d" % tp])
                        P.op("dve", lambda e, tp=tp: e.tensor_tensor(dgt[tp][:], identb[:].unsqueeze(1).to_broadcast([128, 8, 128]),
                                                                     thrg[:].unsqueeze(2).to_broadcast([128, 8, 128]), ALU.mult),
                             reads=["thrg", "identb"], writes=["dgt%d" % tp])
                        yield
                        for ib in range(16):
                            w = ib % 2
                            for hh in range(8):
                                k3 = gi["n"] % 6
                                gi["n"] += 1
                                P.op("dve", lambda e, hh=hh, ib=ib, k3=k3: e.tensor_tensor(
                                    Pd[k3][:], E1n[:, hh, ib * 8:(ib + 1) * 8].unsqueeze(2).to_broadcast([128, 8, 128]),
                                    E[:, 2 * hh + 1, :].unsqueeze(1).to_broadcast([128, 8, 128]), ALU.mult),
                                    reads=["E", "E1n"], writes=["Pd%d" % k3])
                                if hh % 2 == 1:
                                    P.op("act", lambda e, hh=hh, k3=k3, w=w, tp=tp: e.activation(
                                        Gm[w][:, hh, :, :], Pd[k3][:], AF.Relu, bias=negthr[tp][:, hh:hh + 1]),
                                        reads=["Pd%d" % k3, "negthr%d" % tp], writes=["Gm%d_%d" % (w, hh)])
                                    P.op("act", lambda e, hh=hh, w=w: e.activation(G2[w][:, hh // 2, :, :], Gm[w][:, hh, :, :], AF.Sign),
                                         reads=["Gm%d_%d" % (w, hh)], writes=["G2%d_%d" % (w, hh)])
                                else:
                                    P.op("dve", lambda e, hh=hh, k3=k3, w=w: e.scalar_tensor_tensor(
                                        out=Gm[w][:, hh, :, :], in0=Pd[k3][:], scalar=thrg[:, hh:hh + 1], in1=Pd[k3][:], op0=ALU.is_ge, op1=ALU.mult),
                                        reads=["Pd%d" % k3, "thrg"], writes=["Gm%d_%d" % (w, hh)])
                            for q in range(2):
                                seqm = []
                                for hh in range(8):
                                    seqm.append((identb[:], Gm[w][:, hh, 4 * q:4 * q + 4, :], ["Gm%d_%d" % (w, hh), "identb"]))
                                    if hh % 2 == 1:
                                        seqm.append((dgt[tp][:, hh, :], G2[w][:, hh // 2, 4 * q:4 * q + 4, :], ["G2%d_%d" % (w, hh), "dgt%d" % tp]))
                                for mi, (lt, rt, rd) in enumerate(seqm):
                                    P.op("pe", lambda e, lt=lt, rt=rt, mi=mi, q=q, w=w, nm=len(seqm): e.matmul(
                                        ps[2 * w + q][:], lhsT=lt, rhs=rt, start=(mi == 0), stop=(mi == nm - 1)),
                                        reads=rd, writes=["ps%d" % (2 * w + q)])
                                copy_op("act", Gsb[w][:, 4 * q:4 * q + 4, :], ps[2 * w + q][:].rearrange("p (k i) -> p k i", k=4),
                                        reads=["ps%d" % (2 * w + q)], writes=["Gsb%d_%d" % (w, q)])
                            for j in range(8):
                                P.op("pe", lambda e, j=j, w=w: e.transpose(psb[w][:, j * 128:(j + 1) * 128], Gsb[w][:, j, :], identb[:]),
                                     reads=["Gsb%d_%d" % (w, j // 4), "identb"], writes=["psb%d" % w])
                            copy_op("act", gst[w][:], psb[w][:].rearrange("p (k t) -> p k t", k=8), reads=["psb%d" % w], writes=["gst%d" % w])
                            P.dma(GTP[ib * 1024:(ib + 1) * 1024, t * 128:(t + 1) * 128].rearrange("(c p) t -> p c t", p=128), gst[w][:],
                                  reads=["gst%d" % w], key="st")
                            yield

                nxt = 0
                for k, _ in enumerate(g_all()):
                    if nxt < 128:
                        phaseA(nxt)
                        nxt += 1
                while nxt < 128:
                    phaseA(nxt)
                    nxt += 1
            P.barrier()
            with ExitStack() as st6:
                acc = sbt(st6, "acc", [128, 8, D])
                pvb = [sbt(st6, "pvb%d" % i, [128, 4, D], BF16) for i in range(2)]
                gtc = [sbt(st6, "gtc%d" % i, [128, 4, 1024], BF16) for i in range(2)]
                actc = [sbt(st6, "actc%d" % i, [128, 4, 1024], BF16) for i in range(2)]
                coef = actc
                P.op("pool", lambda e: e.memset(acc[:], 0.0), writes=["acc"])
                for grp in range(32):
                    gw = grp % 2
                    r0 = grp * 512
                    P.dma(pvb[gw][:], pv[r0:r0 + 512, :].rearrange("(c p) d -> p c d", p=128), writes=["pvb%d" % gw], key=("pvb", gw), queue="pool")
                    P.dma(gtc[gw][:], GTP[r0:r0 + 512, :].rearrange("(c p) t -> p c t", p=128), writes=["gtc%d" % gw], key=("gtc", gw))
                    P.dma(actc[gw][:], ACTs[r0:r0 + 512, :].rearrange("(c p) t -> p c t", p=128), writes=["actc%d" % gw], key=("actc", gw))
                    P.op("dve", lambda e, gw=gw: e.tensor_tensor(actc[gw][:], actc[gw][:], gtc[gw][:], ALU.mult),
                         reads=["actc%d" % gw, "gtc%d" % gw], writes=["actc%d" % gw, "coef%d" % gw])
                    for t in range(8):
                        for dq in range(4):
                            for c4 in range(4):
                                P.op("pe", lambda e, gw=gw, c4=c4, t=t, dq=dq: e.matmul(
                                    ps[dq][:], lhsT=coef[gw][:, c4, t * 128:(t + 1) * 128], rhs=pvb[gw][:, c4, dq * 512:(dq + 1) * 512],
                                    start=(c4 == 0), stop=(c4 == 3)), reads=["actc%d" % gw, "pvb%d" % gw], writes=["ps%d" % dq])
                        for dq in range(4):
                            eng = "dve" if dq % 2 == 0 else "pool"
                            P.op("dve", lambda e, t=t, dq=dq: e.tensor_tensor(acc[:, t, dq * 512:(dq + 1) * 512],
                                                                              acc[:, t, dq * 512:(dq + 1) * 512], ps[dq][:], ALU.add),
                                 reads=["acc", "ps%d" % dq], writes=["acc"])
                mod5 = sbt(st6, "mod5", [128, D]); gF = sbt(st6, "gF", [128, D])
                hin = sbt(st6, "hin", [128, D]); fo = sbt(st6, "fo", [128, D]); ssf = sbt(st6, "ssf", [128, 4])
                P.dma(mod5[:], MOD[5], writes=["mod5"])
                P.dma(gF[:], gF_d.partition_broadcast(128), writes=["gF"])
                for t in range(8):
                    P.dma(hin[:], Hs[t * 128:(t + 1) * 128, :], writes=["hin"])
                    P.op("dve", lambda e, t=t: e.tensor_tensor(acc[:, t, :], acc[:, t, :], mod5[:], ALU.mult),
                         reads=["acc", "mod5"], writes=["acc"])
                    P.op("dve", lambda e, t=t: e.tensor_tensor(hin[:], hin[:], acc[:, t, :], ALU.add), reads=["hin", "acc"], writes=["hin"])
                    P.op("dve", lambda e: e.memset(ssf[:, 0:1], 0.0), writes=["ssfa"])
                    P.op("act", lambda e: e.activation(fo[:], hin[:], AF.Square, accum_out=ssf[:, 0:1]), reads=["hin", "ssfa"], writes=["fo", "ssfa"])
                    P.op("act", lambda e: e.activation(ssf[:, 1:2], ssf[:, 0:1], AF.Sqrt, bias=cst[:, 1:2], scale=1.0 / D),
                         reads=["ssfa"], writes=["ssfb"])
                    P.op("dve", lambda e: e.reciprocal(ssf[:, 2:3], ssf[:, 1:2]), reads=["ssfb"], writes=["ssfc"])
                    P.op("dve", lambda e: e.scalar_tensor_tensor(out=fo[:], in0=hin[:], scalar=ssf[:, 2:3], in1=gF[:], op0=ALU.mult, op1=ALU.mult),
                         reads=["hin", "ssfc", "gF", "fo"], writes=["fo"])
                    P.dma(out_d[t * 128:(t + 1) * 128, :], fo[:], reads=["fo"], key="out")
        P.emit()
    return nc


def _rope_tables(flip):
    pos = np.arange(9 * 128)
    if flip:
        pos = (S - 1) - pos
    row = (pos // 64).astype(np.float32)
    col = (pos % 64).astype(np.float32)
    freqs = (np.float32(10000.0) ** (-np.arange(32, dtype=np.float32) / np.float32(32))).astype(np.float32)
    ar = row[:, None] * freqs[None, :]
    ac = col[:, None] * freqs[None, :]
    ang = np.concatenate([ar, ar, ac, ac], axis=-1).astype(np.float32)
    cos = np.cos(ang).astype(np.float32)
    sin = np.sin(ang).astype(np.float32)
    sgn = np.concatenate([-np.ones(32), np.ones(32), -np.ones(32), np.ones(32)]).astype(np.float32)
    return np.ascontiguousarray(np.tile(cos, (1, 16))), np.ascontiguousarray(np.tile(sin * sgn[None, :], (1, 16)))


_NC_CACHE = {}


def make_in_maps(x, c, ctx, c_ctx, ada_w, ada_b, norm1_g, w_in, conv_w, conv_b, dt_bias, a_log, ssm_d,
                 ssm_norm_g, attn_sink, w_branch_ssm, w_branch_attn, w_out, norm2_g, peer_wq, peer_keys1,
                 peer_keys2, peer_u, peer_v, final_norm_g):
    f = lambda a: np.ascontiguousarray(np.asarray(a, dtype=np.float32))
    x = f(x); ctx = f(ctx); c = f(c); c_ctx = f(c_ctx)
    w_in0 = f(w_in)[0]
    shared = {
        "ada_w": f(ada_w)[0], "ada_b": f(ada_b)[0][None, :], "norm1_g": f(norm1_g)[0][None, :],
        "norm2_g": f(norm2_g)[0][None, :], "final_g": f(final_norm_g)[None, :], "w_in": w_in0,
        "ssm_d": f(ssm_d)[0][None, :], "ssm_norm_g": f(ssm_norm_g)[0][None, :], "attn_sink": f(attn_sink)[0][None, :],
        "w_bs": f(w_branch_ssm)[0], "w_ba": f(w_branch_attn)[0], "w_o": f(w_out)[0], "w_q": f(peer_wq)[0],
        "keys1": f(peer_keys1)[0], "keys2": f(peer_keys2)[0], "peer_u": f(peer_u)[0], "peer_v": f(peer_v)[0],
        "ident": np.eye(128, dtype=np.float32),
        "triF": np.triu(np.ones((128, 128), np.float32)),
        "triB": np.tril(np.ones((128, 128), np.float32)),
        "cc_fm": np.ascontiguousarray(c_ctx.reshape(16, 128).T),
    }
    cw = f(conv_w)[0]
    cb = f(conv_b)[0]
    convb_fm = np.ascontiguousarray(cb.reshape(48, 128).T)
    wdt = w_in0[:, C_DT:C_DT + 128]
    per_flip = {}
    for flip in (0, 1):
        cwf = cw[::-1] if flip else cw
        convw_fm = np.ascontiguousarray(cwf.T.reshape(48, 128, 5).transpose(1, 0, 2))
        wd = np.concatenate([wdt[:, 64:], wdt[:, :64]], axis=1) if flip else wdt
        dtb = f(dt_bias)[0][::-1] if flip else f(dt_bias)[0]
        al = f(a_log)[0][::-1] if flip else f(a_log)[0]
        cos_t, sin_t = _rope_tables(flip)
        per_flip[flip] = {"convw_fm": convw_fm, "convb_fm": convb_fm, "w_dt": np.ascontiguousarray(wd),
                          "dt_bias": np.ascontiguousarray(dtb.reshape(1, 128)), "a_log": np.ascontiguousarray(al.reshape(1, 128)),
                          "cos_t": cos_t, "sin_t": sin_t}
    in_maps = []
    for core in range(8):
        b, s = core // 2, core % 2
        m = dict(shared)
        m.update(per_flip[s])
        m["x_c"] = np.ascontiguousarray(x[b, ::-1] if s else x[b])
        m["ctx_c"] = np.ascontiguousarray(ctx[b, ::-1] if s else ctx[b])
        m["c_fm"] = np.ascontiguousarray(c[b].reshape(16, 128).T)
        in_maps.append(m)
    return in_maps


def kernel(**inputs):
    in_maps = make_in_maps(**inputs)
    if "nc" not in _NC_CACHE:
        _NC_CACHE["nc"] = build(False)
    nc = _NC_CACHE["nc"]
    res = run_bass_kernel_spmd(nc, in_maps, core_ids=list(range(8)))
    out = np.zeros((4, S, D), np.float32)
    for core in range(8):
        b, s = core // 2, core % 2
        o = np.asarray(res.results[core]["out"], dtype=np.float32)
        if s == 0:
            out[b, 0:1024] = o
        else:
            out[b, 1024:2048] = o[::-1]
    return out
```

```python
import math
from contextlib import ExitStack
import numpy as np
import concourse.bass as bass
import concourse.mybir as mybir
from concourse.bass_utils import run_bass_kernel_spmd

F32 = mybir.dt.float32
BF16 = mybir.dt.bfloat16
AF = mybir.ActivationFunctionType
ALU = mybir.AluOpType
AX = mybir.AxisListType

D = 2048
S = 2048
CTXL = 256
NTOK = S + CTXL
NT_ALL = 18
NT_OWN = 8
DI = 4096
NH = 64
C_K, C_V, C_XBC, C_DT, C_Q, C_Z, C_G = 0, 512, 1024, 7168, 7296, 9344, 13440
NE = 16384
EPS = 1e-6
SCALE = 128.0 ** -0.5

COMPUTE = ("pe", "act", "dve", "pool")
ALLENG = ("pe", "act", "dve", "pool", "sp")


class Prog:
    def __init__(self, nc, n_dma_sems=96):
        self.nc = nc
        self.lists = {e: [] for e in ALLENG}
        self.semnames = []
        self.semval = {}
        self.waited = {e: {} for e in ALLENG}
        self.res = {}
        for e in COMPUTE:
            self._newsem("E_" + e)
        self.n_dma_sems = n_dma_sems
        self.dma_keys = {}

    def _newsem(self, key):
        self.semnames.append(key)
        self.semval[key] = 0

    def _dma_sem(self, key):
        if key not in self.dma_keys:
            name = "D_%d" % len(self.dma_keys)
            assert len(self.dma_keys) < self.n_dma_sems, "too many dma sems"
            self.dma_keys[key] = name
            self._newsem(name)
        return self.dma_keys[key]

    def _deps(self, eng, reads, writes):
        evs = {}

        def add(ev):
            if ev is None:
                return
            s, v = ev
            if evs.get(s, 0) < v:
                evs[s] = v

        for r in reads:
            st = self.res.get(r)
            if st:
                add(st["w"])
        for w in writes:
            st = self.res.get(w)
            if st:
                add(st["w"])
                for s, v in st["r"].items():
                    add((s, v))
        out = []
        for s, v in evs.items():
            if eng == "pe" and s == "E_pe":
                continue
            if self.waited[eng].get(s, 0) >= v:
                continue
            self.waited[eng][s] = v
            out.append((s, v))
        return out

    def _commit(self, ev, reads, writes):
        s, v = ev
        for r in reads:
            st = self.res.setdefault(r, {"w": None, "r": {}})
            if st["r"].get(s, 0) < v:
                st["r"][s] = v
        for w in writes:
            self.res[w] = {"w": ev, "r": {}}

    def op(self, eng, fn, reads=(), writes=()):
        waits = self._deps(eng, reads, writes)
        s = "E_" + eng
        self.semval[s] += 1
        ev = (s, self.semval[s])
        self.lists[eng].append((waits, fn, s, 1))
        self._commit(ev, reads, writes)

    def dma(self, out, in_, reads=(), writes=(), key=None, queue="sp"):
        if key is None or key == "st":
            key = ("ld", writes[0]) if (writes and key is None) else ("st", reads[0])
        s = self._dma_sem(key)
        waits = self._deps(queue, reads, writes)
        self.semval[s] += 16
        ev = (s, self.semval[s])
        self.lists[queue].append((waits, lambda e: e.dma_start(out=out, in_=in_), s, 16))
        self._commit(ev, reads, writes)

    def barrier(self):
        for e in ALLENG:
            waits = []
            for s in self.semnames:
                v = self.semval[s]
                if v > 0 and self.waited[e].get(s, 0) < v:
                    if e == "pe" and s == "E_pe":
                        continue
                    self.waited[e][s] = v
                    waits.append((s, v))
            if waits:
                self.lists[e].append((waits, None, None, 0))
        self.res = {}

    def emit(self):
        nc = self.nc
        waits = [(s, self.semval[s]) for s in self.semnames if self.semval[s] > 0]
        self.lists["sp"].append((waits, None, None, 0))
        with ExitStack() as st:
            sems = {}
            for s in self.semnames:
                sems[s] = st.enter_context(nc.semaphore(s))
            block = st.enter_context(nc.Block())

            def mk(engname):
                def body(eng):
                    for waits, fn, s, inc in self.lists[engname]:
                        for (ws, wv) in waits:
                            eng.wait_ge(sems[ws], wv)
                        if fn is not None:
                            fn(eng).then_inc(sems[s], inc)
                return body

            block.tensor(mk("pe"))
            block.scalar(mk("act"))
            block.vector(mk("dve"))
            block.gpsimd(mk("pool"))
            block.sync(mk("sp"))


def build(debug=False):
    nc = bass.Bass("TRN2", target_bir_lowering=False)
    P = Prog(nc)
    skind = "ExternalOutput" if debug else "Internal"

    def din(name, shape, dt=F32):
        return nc.dram_tensor(name, shape, dt, kind="ExternalInput").ap()

    def dscr(name, shape, dt=F32):
        return nc.dram_tensor(name, shape, dt, kind=skind).ap()

    x_c = din("x_c", [S, D])
    ctx_c = din("ctx_c", [CTXL, D])
    c_fm = din("c_fm", [128, 16])
    cc_fm = din("cc_fm", [128, 16])
    ada_w = din("ada_w", [D, 6 * D])
    ada_b = din("ada_b", [1, 6 * D])
    g1_d = din("norm1_g", [1, D])
    g2_d = din("norm2_g", [1, D])
    gF_d = din("final_g", [1, D])
    w_in = din("w_in", [D, 17536])
    w_dt = din("w_dt", [D, 128])
    convw = din("convw_fm", [128, 48, 5])
    convb = din("convb_fm", [128, 48])
    dtb_d = din("dt_bias", [1, 128])
    alog_d = din("a_log", [1, 128])
    ssmd_d = din("ssm_d", [1, 64])
    gN_d = din("ssm_norm_g", [1, DI])
    sink_d = din("attn_sink", [1, 16])
    w_bs = din("w_bs", [DI, D])
    w_ba = din("w_ba", [D, D])
    w_o = din("w_o", [D, D])
    w_q = din("w_q", [D, D])
    keys1 = din("keys1", [128, 128])
    keys2 = din("keys2", [128, 128])
    pu = din("peer_u", [NE, D])
    pv = din("peer_v", [NE, D])
    ident_d = din("ident", [128, 128])
    triF_d = din("triF", [128, 128])
    triB_d = din("triB", [128, 128])
    cos_d = din("cos_t", [9 * 128, 2048])
    sin_d = din("sin_t", [9 * 128, 2048])
    out_d = nc.dram_tensor("out", [1024, D], F32, kind="ExternalOutput").ap()

    MOD = dscr("MOD_scr", [8, 128, D])
    KV = dscr("KV_scr", [NTOK, 1024])
    DTs = dscr("DT_scr", [NTOK, 128])
    Qs = dscr("Q_scr", [1024, 2048])
    Zs = dscr("Z_scr", [1024, DI])
    XBCT = dscr("XBCT_scr", [6144, NTOK])
    GTs = dscr("GT_scr", [4096, 1024])
    ATT = dscr("ATT_scr", [128, 16, 1024], BF16)
    XTM = dscr("XTM_scr", [NTOK, 5120], BF16)
    BCT = dscr("BCT_scr", [2048, NTOK], BF16)
    Y1 = dscr("Y1_scr", [1024, DI])
    Y2 = dscr("Y2_scr", [1024, DI])
    Hs = dscr("H_scr", [1024, D])
    GTP = dscr("GTP_scr", [NE, 1024], BF16)
    ACTs = dscr("ACT_scr", [NE, 1024], BF16)

    with ExitStack() as top:
        uid = {"n": 0}

        def sbt(st, name, shape, dt=F32):
            uid["n"] += 1
            return st.enter_context(nc.sbuf_tensor("s%d_%s" % (uid["n"], name), shape, dt))

        ps = [top.enter_context(nc.psum_tensor("ps%d" % i, [128, 512], F32)) for i in range(6)]
        psb = [top.enter_context(nc.psum_tensor("psb%d" % i, [128, 1024], BF16)) for i in range(2)]
        ident = sbt(top, "ident", [128, 128])
        identb = sbt(top, "identb", [128, 128], BF16)
        triF = sbt(top, "triF", [128, 128])
        triB = sbt(top, "triB", [128, 128])
        triFb = sbt(top, "triFb", [128, 128], BF16)
        triBb = sbt(top, "triBb", [128, 128], BF16)
        ones = sbt(top, "ones", [128, 128])
        onesb = sbt(top, "onesb", [128, 128], BF16)
        cst = sbt(top, "cst", [128, 4])
        P.dma(ident[:], ident_d, writes=["ident"])
        P.dma(triF[:], triF_d, writes=["triF"])
        P.dma(triB[:], triB_d, writes=["triB"])
        P.op("dve", lambda e: e.tensor_copy(identb[:], ident[:]), reads=["ident"], writes=["identb"])
        P.op("dve", lambda e: e.tensor_copy(triFb[:], triF[:]), reads=["triF"], writes=["triFb"])
        P.op("dve", lambda e: e.tensor_copy(triBb[:], triB[:]), reads=["triB"], writes=["triBb"])
        P.op("dve", lambda e: e.memset(ones[:], 1.0), writes=["ones"])
        P.op("dve", lambda e: e.memset(onesb[:], 1.0), writes=["onesb"])
        P.op("dve", lambda e: e.memset(cst[:, 0:1], 1.0), writes=["cst0"])
        P.op("dve", lambda e: e.memset(cst[:, 1:2], EPS), writes=["cst1"])
        P.op("dve", lambda e: e.memset(cst[:, 2:3], 0.0), writes=["cst2"])
        P.barrier()
        rr = {"n": 0}

        def evac_eng():
            rr["n"] += 1
            return "act" if rr["n"] % 2 == 0 else "dve"

        def copy_op(eng, out, in_, reads, writes):
            if eng == "act":
                P.op("act", lambda e: e.copy(out, in_), reads=reads, writes=writes)
            else:
                P.op(eng, lambda e: e.tensor_copy(out, in_), reads=reads, writes=writes)

        def load_w_block(wb, slot, Wd, KC, col0, ncols):
            for k0 in range(0, KC, 4):
                P.dma(wb[slot][:, k0:k0 + 4, 0:ncols],
                      Wd[k0 * 128:(k0 + 4) * 128, col0:col0 + ncols].rearrange("(k p) n -> p k n", p=128),
                      writes=["wb%d_%d" % (slot, k0)], key=("wb", slot), queue="pool")

        def norm_mod_T(st, src_rows, n_tiles, Atile_of, Stile_of, xT, tagp):
            xt = [sbt(st, tagp + "xt%d" % i, [128, D]) for i in range(2)]
            junk = sbt(st, tagp + "junk", [128, D])
            t1 = sbt(st, tagp + "t1", [128, D])
            ub = [sbt(st, tagp + "ub%d" % i, [128, D], BF16) for i in range(2)]
            ss = sbt(st, tagp + "ss", [128, 4])
            for t in range(n_tiles):
                sl = t % 2
                P.dma(xt[sl][:], src_rows(t), writes=[tagp + "xt%d" % sl], key=(tagp + "xt", sl))
                P.op("dve", lambda e: e.memset(ss[:, 0:1], 0.0), writes=[tagp + "ss0"])
                P.op("act", lambda e, sl=sl: e.activation(junk[:], xt[sl][:], AF.Square, accum_out=ss[:, 0:1]),
                     reads=[tagp + "xt%d" % sl, tagp + "ss0"], writes=[tagp + "junk", tagp + "ss0"])
                P.op("act", lambda e: e.activation(ss[:, 1:2], ss[:, 0:1], AF.Sqrt, bias=cst[:, 1:2], scale=1.0 / D),
                     reads=[tagp + "ss0"], writes=[tagp + "ss1"])
                P.op("dve", lambda e: e.reciprocal(ss[:, 2:3], ss[:, 1:2]), reads=[tagp + "ss1"], writes=[tagp + "ss2"])
                A, An = Atile_of(t)
                Sh, Sn = Stile_of(t)
                P.op("dve", lambda e, sl=sl, A=A: e.scalar_tensor_tensor(out=t1[:], in0=xt[sl][:], scalar=ss[:, 2:3], in1=A[:],
                                                                      op0=ALU.mult, op1=ALU.mult),
                     reads=[tagp + "xt%d" % sl, tagp + "ss2", An], writes=[tagp + "t1"])
                P.op("dve", lambda e, sl=sl, Sh=Sh: e.tensor_tensor(ub[sl][:], t1[:], Sh[:], ALU.add),
                     reads=[tagp + "t1", Sn], writes=[tagp + "ub%d" % sl])
                for half in range(2):
                    for j in range(8):
                        kc = half * 8 + j
                        P.op("pe", lambda e, sl=sl, kc=kc, half=half, j=j: e.transpose(
                            psb[half][:, j * 128:(j + 1) * 128], ub[sl][:, kc * 128:(kc + 1) * 128], identb[:]),
                            reads=[tagp + "ub%d" % sl, "identb"], writes=["psb%d" % half])
                    copy_op(evac_eng(), xT[:, half * 8:(half + 1) * 8, t * 128:(t + 1) * 128],
                            psb[half][:].rearrange("p (k t) -> p k t", k=8),
                            reads=["psb%d" % half], writes=["xT_" + tagp])

        def linear_T(xT, xTname, KC, wb, slot, ncols, tiles, consume):
            for t in tiles:
                b = rr["n"] % 4
                for kc in range(KC):
                    P.op("pe", lambda e, b=b, kc=kc, t=t: e.matmul(ps[b][:, 0:ncols], lhsT=xT[:, kc, t * 128:(t + 1) * 128],
                                                                  rhs=wb[slot][:, kc, 0:ncols], start=(kc == 0), stop=(kc == KC - 1)),
                         reads=[xTname] + ["wb%d_%d" % (slot, k0) for k0 in range(0, 16, 4)], writes=["ps%d" % b])
                consume(ps[b], "ps%d" % b, t)

        def linear_F(xT, xTname, KC, wb, slot, ncols, tokgroups, consume):
            for ch in range(ncols // 128):
                for (t0, nt) in tokgroups:
                    b = rr["n"] % 4
                    for kc in range(KC):
                        P.op("pe", lambda e, b=b, kc=kc, t0=t0, nt=nt, ch=ch: e.matmul(
                            ps[b][:, 0:nt], lhsT=wb[slot][:, kc, ch * 128:(ch + 1) * 128], rhs=xT[:, kc, t0:t0 + nt],
                            start=(kc == 0), stop=(kc == KC - 1)),
                            reads=[xTname] + ["wb%d_%d" % (slot, k0) for k0 in range(0, 16, 4)], writes=["ps%d" % b])
                    consume(ps[b], "ps%d" % b, ch, t0, nt)

        with ExitStack() as st:
            cs = sbt(st, "cs", [128, 2, 16])
            csb = sbt(st, "csb", [128, 2, 16, 128], BF16)
            adab = sbt(st, "adab", [128, 6 * D])
            g1 = sbt(st, "g1", [128, D])
            g2 = sbt(st, "g2", [128, D])
            aw = [sbt(st, "aw%d" % i, [128, 16, 512], BF16) for i in range(3)]
            res = [sbt(st, "ares%d" % i, [128, 512]) for i in range(4)]
            P.dma(cs[:, 0, :], c_fm, writes=["cs_a"], key="a0")
            P.dma(cs[:, 1, :], cc_fm, writes=["cs_b"], key="a0")
            P.dma(adab[:], ada_b.partition_broadcast(128), writes=["adab"])
            P.dma(g1[:], g1_d.partition_broadcast(128), writes=["g1"])
            P.dma(g2[:], g2_d.partition_broadcast(128), writes=["g2"])
            P.op("act", lambda e: e.activation(cs[:], cs[:], AF.Silu), reads=["cs_a", "cs_b"], writes=["cs"])
            P.op("dve", lambda e: e.tensor_copy(csb[:], cs[:].unsqueeze(3).to_broadcast([128, 2, 16, 128])),
                 reads=["cs"], writes=["csb"])
            ri = 0
            for j in range(6):
                for blk in range(4):
                    col0 = j * D + blk * 512
                    sl = (j * 4 + blk) % 3
                    for k0 in range(0, 16, 8):
                        P.dma(aw[sl][:, k0:k0 + 8, :],
                              ada_w[k0 * 128:(k0 + 8) * 128, col0:col0 + 512].rearrange("(k p) n -> p k n", p=128),
                              writes=["aw%d_%d" % (sl, k0)], key=("aw", sl), queue="pool")
                    for which in range(2 if j < 2 else 1):
                        b = ri % 4
                        r = res[ri % 4]
                        rn = "ares%d" % (ri % 4)
                        ri += 1
                        for kc in range(16):
                            P.op("pe", lambda e, b=b, kc=kc, sl=sl, which=which: e.matmul(
                                ps[b][:], lhsT=csb[:, which, kc, :], rhs=aw[sl][:, kc, :], start=(kc == 0), stop=(kc == 15)),
                                reads=["csb"] + ["aw%d_%d" % (sl, k0) for k0 in range(0, 16, 8)], writes=["ps%d" % b])
                        P.op("dve", lambda e, b=b, r=r, col0=col0: e.tensor_tensor(r[:], ps[b][:], adab[:, col0:col0 + 512], ALU.add),
                             reads=["ps%d" % b, "adab"], writes=[rn])
                        if j in (1, 4):
                            g = g1 if j == 1 else g2
                            P.op("dve", lambda e, r=r, g=g, blk=blk: e.scalar_tensor_tensor(
                                out=r[:], in0=r[:], scalar=1.0, in1=g[:, blk * 512:(blk + 1) * 512], op0=ALU.add, op1=ALU.mult),
                                reads=[rn, "g1", "g2"], writes=[rn])
                        if which == 0:
                            mi = {0: 1, 1: 0, 2: 2, 3: 4, 4: 3, 5: 5}[j]
                        else:
                            mi = {0: 7, 1: 6}[j]
                        P.dma(MOD[mi, :, blk * 512:(blk + 1) * 512], r[:], reads=[rn], writes=["MODd"], key="st")
        P.barrier()

        with ExitStack() as st:
            uT = sbt(st, "uT", [128, 16, NTOK], BF16)
            with ExitStack() as st1:
                A1 = sbt(st1, "A1", [128, D]); S1 = sbt(st1, "S1", [128, D])
                Ac = sbt(st1, "Ac", [128, D]); Sc = sbt(st1, "Sc", [128, D])
                P.dma(A1[:], MOD[0], writes=["A1"])
                P.dma(S1[:], MOD[1], writes=["S1"])
                P.dma(Ac[:], MOD[6], writes=["Ac"])
                P.dma(Sc[:], MOD[7], writes=["Sc"])
                norm_mod_T(st1,
                           lambda t: x_c[t * 128:(t + 1) * 128, :] if t < 16 else ctx_c[(t - 16) * 128:(t - 15) * 128, :],
                           NT_ALL,
                           lambda t: (A1, "A1") if t < 16 else (Ac, "Ac"),
                           lambda t: (S1, "S1") if t < 16 else (Sc, "Sc"),
                           uT, "n1")
            P.barrier()
            wb = [sbt(st, "wb%d" % i, [128, 16, 512], BF16) for i in range(2)]
            stg = [sbt(st, "stg%d" % i, [128, 512]) for i in range(4)]
            sti = {"n": 0}

            def store_T(dst_of):
                def consume(pst, psn, t):
                    i = sti["n"] % 4
                    sti["n"] += 1
                    dst = dst_of(t)
                    ncols = dst.shape[1]
                    copy_op(evac_eng(), stg[i][:, 0:ncols], pst[:, 0:ncols], reads=[psn], writes=["stg%d" % i])
                    P.dma(dst, stg[i][:, 0:ncols], reads=["stg%d" % i], key="st")
                return consume

            def store_F(dst_of):
                def consume(pst, psn, ch, t0, nt):
                    i = sti["n"] % 4
                    sti["n"] += 1
                    copy_op(evac_eng(), stg[i][:, 0:nt], pst[:, 0:nt], reads=[psn], writes=["stg%d" % i])
                    P.dma(dst_of(ch, t0, nt), stg[i][:, 0:nt], reads=["stg%d" % i], key="st")
                return consume

            ALLT = list(range(NT_ALL))
            OWNT = list(range(NT_OWN))
            KVT = list(range(9)) + [16, 17]
            ALLG = [(0, 512), (512, 512), (1024, 512), (1536, 512), (2048, 256)]
            OWNG = [(0, 512), (512, 512)]
            blocks = []
            blocks.append((w_in, C_K, 512, "T", KVT, store_T(lambda t: KV[t * 128:(t + 1) * 128, 0:512])))
            blocks.append((w_in, C_V, 512, "T", KVT, store_T(lambda t: KV[t * 128:(t + 1) * 128, 512:1024])))
            blocks.append((w_dt, 0, 128, "T", ALLT, store_T(lambda t: DTs[t * 128:(t + 1) * 128, :])))
            for bi in range(12):
                blocks.append((w_in, C_XBC + bi * 512, 512, "F", ALLG,
                               store_F(lambda ch, t0, nt, bi=bi: XBCT[bi * 512 + ch * 128: bi * 512 + (ch + 1) * 128, t0:t0 + nt])))
            for bi in range(4):
                blocks.append((w_in, C_Q + bi * 512, 512, "T", OWNT,
                               store_T(lambda t, bi=bi: Qs[t * 128:(t + 1) * 128, bi * 512:(bi + 1) * 512])))
            for bi in range(8):
                blocks.append((w_in, C_Z + bi * 512, 512, "T", OWNT,
                               store_T(lambda t, bi=bi: Zs[t * 128:(t + 1) * 128, bi * 512:(bi + 1) * 512])))
            for bi in range(8):
                blocks.append((w_in, C_G + bi * 512, 512, "F", OWNG,
                               store_F(lambda ch, t0, nt, bi=bi: GTs[bi * 512 + ch * 128: bi * 512 + (ch + 1) * 128, t0:t0 + nt])))
            for i, (Wd, col0, ncols, orient, toks, cons) in enumerate(blocks):
                sl = i % 2
                load_w_block(wb, sl, Wd, 16, col0, ncols)
                if orient == "T":
                    linear_T(uT, "xT_n1", 16, wb, sl, ncols, toks, cons)
                else:
                    linear_F(uT, "xT_n1", 16, wb, sl, ncols, toks, cons)
        P.barrier()

        with ExitStack() as st:
            kT = sbt(st, "kT", [128, 4, 11 * 128], BF16)
            Vt = sbt(st, "Vt", [128, 11, 512], BF16)
            qT = sbt(st, "qT", [128, 16, 1024], BF16)
            aT = sbt(st, "aT", [128, 16, 1024], BF16)
            esink = sbt(st, "esink", [128, 16])
            cosb = [sbt(st, "cosb%d" % i, [128, 2048]) for i in range(2)]
            sinb = [sbt(st, "sinb%d" % i, [128, 2048]) for i in range(2)]
            kin = [sbt(st, "kin%d" % i, [128, 1024]) for i in range(2)]
            qin = [sbt(st, "qin%d" % i, [128, 2048]) for i in range(2)]
            r1 = sbt(st, "r1", [128, 2048]); r2 = sbt(st, "r2", [128, 2048])
            rb = [sbt(st, "rb%d" % i, [128, 2048], BF16) for i in range(2)]
            P.dma(esink[:], sink_d.partition_broadcast(128), writes=["esink"])
            P.op("act", lambda e: e.activation(esink[:], esink[:], AF.Exp), reads=["esink"], writes=["esink"])

            def rope(src, nh, sl, out_bf, rn_src, rn_out):
                n = nh * 128
                v = lambda ap: ap.rearrange("p (g b c) -> p g b c", g=nh * 2, b=2, c=32)
                P.op("dve", lambda e: e.tensor_tensor(r1[:, 0:n], src, cosb[sl][:, 0:n], ALU.mult),
                     reads=[rn_src, "cosb%d" % sl], writes=["r1"])
                P.op("dve", lambda e: e.tensor_tensor(v(r2[:, 0:n])[:, :, 0, :], v(src)[:, :, 1, :], v(sinb[sl][:, 0:n])[:, :, 0, :], ALU.mult),
                     reads=[rn_src, "sinb%d" % sl], writes=["r2a"])
                P.op("dve", lambda e: e.tensor_tensor(v(r2[:, 0:n])[:, :, 1, :], v(src)[:, :, 0, :], v(sinb[sl][:, 0:n])[:, :, 1, :], ALU.mult),
                     reads=[rn_src, "sinb%d" % sl], writes=["r2b"])
                P.op("dve", lambda e: e.tensor_tensor(out_bf, r1[:, 0:n], r2[:, 0:n], ALU.add),
                     reads=["r1", "r2a", "r2b"], writes=[rn_out])

            for blk in range(11):
                sl = blk % 2
                row0 = blk * 128 if blk < 9 else S + (blk - 9) * 128
                P.dma(kin[sl][:], KV[row0:row0 + 128, :], writes=["kin%d" % sl], key=("kin", sl))
                copy_op("act", Vt[:, blk, :], kin[sl][:, 512:1024], reads=["kin%d" % sl], writes=["Vt"])
                if blk < 9:
                    P.dma(cosb[sl][:], cos_d[blk * 128:(blk + 1) * 128, :], writes=["cosb%d" % sl], key=None)
                    P.dma(sinb[sl][:], sin_d[blk * 128:(blk + 1) * 128, :], writes=["sinb%d" % sl], key=None)
                    rope(kin[sl][:, 0:512], 4, sl, rb[sl][:, 0:512], "kin%d" % sl, "rb%d" % sl)
                else:
                    copy_op("dve", rb[sl][:, 0:512], kin[sl][:, 0:512], reads=["kin%d" % sl], writes=["rb%d" % sl])
                for g in range(4):
                    P.op("pe", lambda e, sl=sl, g=g: e.transpose(psb[0][:, g * 128:(g + 1) * 128], rb[sl][:, g * 128:(g + 1) * 128], identb[:]),
                         reads=["rb%d" % sl, "identb"], writes=["psb0"])
                copy_op(evac_eng(), kT[:, :, blk * 128:(blk + 1) * 128], psb[0][:, 0:512].rearrange("p (g t) -> p g t", g=4),
                        reads=["psb0"], writes=["kT"])
            for t in range(8):
                sl = t % 2
                P.dma(qin[sl][:], Qs[t * 128:(t + 1) * 128, :], writes=["qin%d" % sl], key=("qin", sl))
                P.dma(cosb[sl][:], cos_d[t * 128:(t + 1) * 128, :], writes=["cosb%d" % sl], key=None)
                P.dma(sinb[sl][:], sin_d[t * 128:(t + 1) * 128, :], writes=["sinb%d" % sl], key=None)
                rope(qin[sl][:], 16, sl, rb[sl][:], "qin%d" % sl, "rb%d" % sl)
                for half in range(2):
                    for j in range(8):
                        hh = half * 8 + j
                        P.op("pe", lambda e, sl=sl, hh=hh, half=half, j=j: e.transpose(
                            psb[half][:, j * 128:(j + 1) * 128], rb[sl][:, hh * 128:(hh + 1) * 128], identb[:]),
                            reads=["rb%d" % sl, "identb"], writes=["psb%d" % half])
                    copy_op(evac_eng(), qT[:, half * 8:(half + 1) * 8, t * 128:(t + 1) * 128],
                            psb[half][:].rearrange("p (k t) -> p k t", k=8), reads=["psb%d" % half], writes=["qT"])
            pT = [sbt(st, "pT%d" % i, [128, 512], BF16) for i in range(4)]
            den = [sbt(st, "den%d" % i, [128, 512]) for i in range(2)]
            units = []
            for qb in range(8):
                for g in range(4):
                    kbs = []
                    if qb >= 1:
                        kbs.append((qb - 1, triBb, "triBb"))
                    kbs.append((qb, None, None))
                    kbs.append((qb + 1, triFb, "triFb"))
                    kbs.append((9, None, None))
                    kbs.append((10, None, None))
                    for ki, (kb, mask, mname) in enumerate(kbs):
                        units.append((qb, g, ki, len(kbs), kb, mask, mname))

            def stageS(u):
                qb, g, ki, n, kb, mask, mname = units[u]
                sb_ = u % 2
                pp = u % 4
                P.op("pe", lambda e: e.matmul(ps[sb_][:], lhsT=kT[:, g, kb * 128:(kb + 1) * 128],
                                              rhs=qT[:, 4 * g:4 * g + 4, qb * 128:(qb + 1) * 128], start=True, stop=True),
                     reads=["kT", "qT"], writes=["ps%d" % sb_])
                P.op("act", lambda e: e.activation(pT[pp][:], ps[sb_][:], AF.Exp, scale=SCALE),
                     reads=["ps%d" % sb_], writes=["pT%d" % pp])
                if mask is not None:
                    P.op("pool", lambda e: e.tensor_tensor(
                        pT[pp][:].rearrange("p (r q) -> p r q", r=4), pT[pp][:].rearrange("p (r q) -> p r q", r=4),
                        mask[:].unsqueeze(1).to_broadcast([128, 4, 128]), ALU.mult),
                        reads=["pT%d" % pp, mname], writes=["pT%d" % pp])

            def stageV(u):
                qb, g, ki, n, kb, mask, mname = units[u]
                pp = u % 4
                par = (qb * 4 + g) % 2
                pa, pb = (2, 3) if par == 0 else (4, 5)
                P.op("pe", lambda e: e.matmul(ps[pa][:], lhsT=Vt[:, kb, g * 128:(g + 1) * 128], rhs=pT[pp][:],
                                              start=(ki == 0), stop=(ki == n - 1)),
                     reads=["Vt", "pT%d" % pp], writes=["ps%d" % pa])
                P.op("pe", lambda e: e.matmul(ps[pb][:], lhsT=onesb[:], rhs=pT[pp][:], start=(ki == 0), stop=(ki == n - 1)),
                     reads=["onesb", "pT%d" % pp], writes=["ps%d" % pb])
                if ki == n - 1:
                    dn = den[par]
                    P.op("dve", lambda e: e.tensor_tensor(dn[:].rearrange("p (r q) -> p r q", r=4),
                                                          ps[pb][:].rearrange("p (r q) -> p r q", r=4),
                                                          esink[:, 4 * g:4 * g + 4].unsqueeze(2).to_broadcast([128, 4, 128]), ALU.add),
                         reads=["ps%d" % pb, "esink"], writes=["den%d" % par])
                    P.op("dve", lambda e: e.reciprocal(dn[:], dn[:]), reads=["den%d" % par], writes=["den%d" % par])
                    P.op("dve", lambda e: e.tensor_tensor(aT[:, 4 * g:4 * g + 4, qb * 128:(qb + 1) * 128],
                                                          ps[pa][:].rearrange("p (r q) -> p r q", r=4),
                                                          dn[:].rearrange("p (r q) -> p r q", r=4), ALU.mult),
                         reads=["ps%d" % pa, "den%d" % par], writes=["aT"])

            for i in range(len(units) + 2):
                if i < len(units):
                    stageS(i)
                if i >= 2:
                    stageV(i - 2)
            P.dma(ATT, aT[:], reads=["aT"], key="st")
        P.barrier()

        with ExitStack() as st:
            cw = sbt(st, "cw", [128, 48, 5]); cb = sbt(st, "cb", [128, 48])
            P.dma(cw[:], convw, writes=["cw"])
            P.dma(cb[:], convb, writes=["cb"])
            cin = [sbt(st, "cin%d" % i, [128, S + 4 + CTXL + 4], BF16) for i in range(2)]
            dg = [sbt(st, "dg%d" % i, [128, 5, 128], BF16) for i in range(2)]
            cout = sbt(st, "cout", [128, 8, NTOK], BF16)
            tstg = [sbt(st, "tstg%d" % i, [128, 1024], BF16) for i in range(2)]
            for i in range(2):
                P.op("pool", lambda e, i=i: e.memset(cin[i][:], 0.0), writes=["cin%d" % i])
            CG = [(0, 0, 512), (512, 512, 512), (1024, 1024, 512), (1536, 1536, 512), (S, S + 4, CTXL)]
            pc = 0
            for grp in range(6):
                for c8 in range(8):
                    cc = grp * 8 + c8
                    sl = cc % 2
                    P.dma(cin[sl][:, 2:2 + S], XBCT[cc * 128:(cc + 1) * 128, 0:S], writes=["cin%d_a" % sl], key=("cin", sl), queue="pool")
                    P.dma(cin[sl][:, S + 6:S + 6 + CTXL], XBCT[cc * 128:(cc + 1) * 128, S:NTOK], writes=["cin%d_b" % sl], key=("cin", sl), queue="pool")
                    for j in range(5):
                        P.op("dve", lambda e, sl=sl, cc=cc, j=j: e.tensor_scalar(dg[sl][:, j, :], identb[:], cw[:, cc, j:j + 1], None, op0=ALU.mult),
                             reads=["identb", "cw"], writes=["dg%d" % sl])
                    for (o0, i0, L) in CG:
                        bnk = pc % 4
                        pc += 1
                        for j in range(5):
                            P.op("pe", lambda e, sl=sl, j=j, i0=i0, L=L, bnk=bnk: e.matmul(
                                ps[bnk][:, 0:L], lhsT=dg[sl][:, j, :], rhs=cin[sl][:, i0 + j:i0 + j + L], start=(j == 0), stop=(j == 4)),
                                reads=["dg%d" % sl, "cin%d" % sl, "cin%d_a" % sl, "cin%d_b" % sl], writes=["ps%d" % bnk])
                        P.op("act", lambda e, cc=cc, c8=c8, o0=o0, L=L, bnk=bnk: e.activation(
                            cout[:, c8, o0:o0 + L], ps[bnk][:, 0:L], AF.Silu, bias=cb[:, cc:cc + 1]),
                            reads=["ps%d" % bnk, "cb"], writes=["cout"])
                if grp < 5:
                    for t in range(NT_ALL):
                        hb = t % 2
                        for c8 in range(8):
                            P.op("pe", lambda e, hb=hb, c8=c8, t=t: e.transpose(
                                psb[hb][:, c8 * 128:(c8 + 1) * 128], cout[:, c8, t * 128:(t + 1) * 128], identb[:]),
                                reads=["cout", "identb"], writes=["psb%d" % hb])
                        copy_op(evac_eng(), tstg[hb][:], psb[hb][:], reads=["psb%d" % hb], writes=["tstg%d" % hb])
                        P.dma(XTM[t * 128:(t + 1) * 128, grp * 1024:(grp + 1) * 1024], tstg[hb][:], reads=["tstg%d" % hb], key="st")
                if grp >= 4:
                    P.dma(BCT[(grp - 4) * 1024:(grp - 3) * 1024, :].rearrange("(c p) t -> p c t", p=128), cout[:], reads=["cout"], key="st2")
        P.barrier()

        with ExitStack() as st:
            hst = [sbt(st, "hst%d" % i, [128, DI]) for i in range(2)]
            hbf = sbt(st, "hbf", [128, DI], BF16)
            xtm = [sbt(st, "xtm%d" % i, [128, 5120], BF16) for i in range(2)]
            bct = [sbt(st, "bct%d" % i, [128, 16, 128], BF16) for i in range(2)]
            dtr = [sbt(st, "dtr%d" % i, [128, 128]) for i in range(2)]
            dtbb = sbt(st, "dtbb", [128, 128]); Ab = sbt(st, "Ab", [128, 128]); Dd = sbt(st, "Dd", [128, 64])
            dtL = [sbt(st, "dt%d" % i, [128, 64]) for i in range(2)]; aaL = [sbt(st, "aa%d" % i, [128, 64]) for i in range(2)]
            acsL = [sbt(st, "acs%d" % i, [128, 64]) for i in range(2)]; atotL = [sbt(st, "atot%d" % i, [128, 64]) for i in range(2)]
            wendL = [sbt(st, "wend%d" % i, [128, 64]) for i in range(2)]; eacsL = [sbt(st, "eacs%d" % i, [128, 64]) for i in range(2)]
            etotL = [sbt(st, "etot%d" % i, [128, 64]) for i in range(2)]
            wxL = [sbt(st, "wx%d" % i, [128, 128]) for i in range(2)]; wxeL = [sbt(st, "wxe%d" % i, [128, DI], BF16) for i in range(2)]
            TA = sbt(st, "TA", [128, 8, 128]); seg = sbt(st, "seg", [128, 8, 128])
            Mt2 = [sbt(st, "Mt%d" % i, [128, 8, 128], BF16) for i in range(2)]; CBa = sbt(st, "CBa", [128, 8, 128]); segL = [sbt(st, "segd%d" % i, [128, 8, 128]) for i in range(2)]
            yt = sbt(st, "yt", [128, DI]); ytmp = sbt(st, "ytmp", [128, 512])
            P.dma(dtbb[:], dtb_d.partition_broadcast(128), writes=["dtbb"])
            P.dma(Ab[:], alog_d.partition_broadcast(128), writes=["Ab"])
            P.dma(Dd[:], ssmd_d.partition_broadcast(128), writes=["Dd"])
            P.op("act", lambda e: e.activation(Ab[:], Ab[:], AF.Exp), reads=["Ab"], writes=["Ab"])
            P.op("dve", lambda e: e.tensor_scalar(Ab[:], Ab[:], -1.0, None, op0=ALU.mult), reads=["Ab"], writes=["Ab"])
            for i in range(2):
                P.op("pool", lambda e, i=i: e.memset(hst[i][:], 0.0), writes=["hst%d" % i])
            DIb = sbt(st, "DIb", [128, 64, 128], BF16)
            P.op("dve", lambda e: e.tensor_tensor(DIb[:], ident[:].unsqueeze(1).to_broadcast([128, 64, 128]),
                                                  Dd[:].unsqueeze(2).to_broadcast([128, 64, 128]), ALU.mult),
                 reads=["ident", "Dd"], writes=["DIb"])
            ldi = {"n": 0}

            def setup(tile, d, full, sl):
                tri = triF if d == 0 else triB
                trin = "triF" if d == 0 else "triB"
                h = hst[d]; hn = "hst%d" % d
                dt = dtL[sl]
                aa = aaL[sl]
                acs = acsL[sl]
                atot = atotL[sl]
                wend = wendL[sl]
                eacs = eacsL[sl]
                etot = etotL[sl]
                wx = wxL[sl]
                wxe = wxeL[sl]
                P.dma(xtm[sl][:], XTM[tile * 128:(tile + 1) * 128, :], writes=["xtm%d" % sl], key=None)
                yield
                P.dma(dtr[sl][:], DTs[tile * 128:(tile + 1) * 128, :], writes=["dtr%d" % sl], key=None)
                yield
                if full:
                    P.dma(bct[sl][:], BCT[:, tile * 128:(tile + 1) * 128].rearrange("(c p) t -> p c t", p=128),
                          writes=["bct%d" % sl], key=None)
                    yield
                P.op("dve", lambda e: e.tensor_tensor(dt[:], dtr[sl][:, d * 64:(d + 1) * 64], dtbb[:, d * 64:(d + 1) * 64], ALU.add),
                     reads=["dtr%d" % sl, "dtbb"], writes=["dt%d" % sl])
                yield
                P.op("act", lambda e: e.activation(dt[:], dt[:], AF.Exp), reads=["dt%d" % sl], writes=["dt%d" % sl])
                yield
                P.op("act", lambda e: e.activation(dt[:], dt[:], AF.Ln, bias=cst[:, 0:1]), reads=["dt%d" % sl, "cst0"], writes=["dt%d" % sl])
                yield
                P.op("dve", lambda e: e.tensor_tensor(aa[:], dt[:], Ab[:, d * 64:(d + 1) * 64], ALU.mult), reads=["dt%d" % sl, "Ab"], writes=["aa%d" % sl])
                yield
                P.op("pe", lambda e: e.matmul(ps[4][:, 0:64], lhsT=tri[:], rhs=aa[:], start=True, stop=True),
                     reads=[trin, "aa%d" % sl], writes=["ps4"])
                yield
                P.op("pe", lambda e: e.matmul(ps[4][:, 64:128], lhsT=ones[:], rhs=aa[:], start=True, stop=True),
                     reads=["ones", "aa%d" % sl], writes=["ps4"])
                yield
                P.op("dve", lambda e: e.tensor_copy(acs[:], ps[4][:, 0:64]), reads=["ps4"], writes=["acs%d" % sl])
                yield
                P.op("dve", lambda e: e.tensor_copy(atot[:], ps[4][:, 64:128]), reads=["ps4"], writes=["atot%d" % sl])
                yield
                P.op("dve", lambda e: e.tensor_tensor(wend[:], atot[:], acs[:], ALU.subtract), reads=["atot%d" % sl, "acs%d" % sl], writes=["wend%d" % sl])
                yield
                P.op("act", lambda e: e.activation(wend[:], wend[:], AF.Exp), reads=["wend%d" % sl], writes=["wend%d" % sl])
                yield
                P.op("act", lambda e: e.activation(etot[:], atot[:], AF.Exp), reads=["atot%d" % sl], writes=["etot%d" % sl])
                yield
                v3 = lambda ap: ap.rearrange("p (h q) -> p h q", h=64)
                bc = lambda ap: ap.unsqueeze(2).to_broadcast([128, 64, 64])
                P.op("dve", lambda e: e.tensor_tensor(wend[:], wend[:], dt[:], ALU.mult), reads=["wend%d" % sl, "dt%d" % sl], writes=["wend%d" % sl])
                yield
                P.op("dve", lambda e: e.tensor_tensor(v3(wxe[:]), v3(xtm[sl][:, 0:DI]), bc(wend[:]), ALU.mult),
                     reads=["xtm%d" % sl, "wend%d" % sl], writes=["wxe%d" % sl])
                yield
                if full:
                    P.op("act", lambda e: e.activation(wx[:, 0:64], dt[:], AF.Ln), reads=["dt%d" % sl], writes=["lndt%d" % sl])
                    yield
                    P.op("dve", lambda e: e.tensor_tensor(wx[:, 64:128], acs[:], wx[:, 0:64], ALU.subtract),
                         reads=["acs%d" % sl, "lndt%d" % sl], writes=["acs2%d" % sl])
                    yield
                if full:
                    P.op("act", lambda e: e.activation(eacs[:], acs[:], AF.Exp), reads=["acs%d" % sl], writes=["eacs%d" % sl])
                    yield

            def body(tile, d, full, sl, pump):
                tri = triF if d == 0 else triB
                trin = "triF" if d == 0 else "triB"
                h = hst[d]; hn = "hst%d" % d
                dt = dtL[sl]
                aa = aaL[sl]
                acs = acsL[sl]
                atot = atotL[sl]
                wend = wendL[sl]
                eacs = eacsL[sl]
                etot = etotL[sl]
                wx = wxL[sl]
                wxe = wxeL[sl]
                v3 = lambda ap: ap.rearrange("p (h q) -> p h q", h=64)
                bc = lambda ap: ap.unsqueeze(2).to_broadcast([128, 64, 64])
                if full:
                    P.op("dve", lambda e: e.tensor_copy(hbf[:], h[:]), reads=[hn], writes=["hbf"])
                    for hf in range(2):
                        for j in range(4):
                            g = 4 * hf + j
                            P.op("pe", lambda e, g=g, j=j: e.matmul(ps[5][:, j * 128:(j + 1) * 128], lhsT=bct[sl][:, g, :], rhs=bct[sl][:, 8 + g, :],
                                                                    start=True, stop=True), reads=["bct%d" % sl], writes=["ps5"])
                        P.op("dve", lambda e, hf=hf: e.tensor_tensor(CBa[:, 4 * hf:4 * hf + 4, :], ps[5][:].rearrange("p (j l) -> p j l", j=4),
                                                                     tri[:].unsqueeze(1).to_broadcast([128, 4, 128]), ALU.mult),
                             reads=["ps5", trin], writes=["CBa%d" % hf])

                    def s_TA(g):
                        for r in range(8):
                            hh = 8 * g + r
                            P.op("pe", lambda e, r=r, hh=hh: e.matmul(ps[r // 4][:, (r % 4) * 128:(r % 4 + 1) * 128],
                                                                       lhsT=aa[:, hh:hh + 1].to_broadcast([128, 128]), rhs=tri[:],
                                                                       start=True, stop=True),
                                 reads=[trin, "aa%d" % sl], writes=["ps%d" % (r // 4)])

                    def s_seg(g):
                        m = g % 2
                        for hf in range(2):
                            P.op("dve", lambda e, hf=hf: e.tensor_tensor(
                                segL[m][:, 4 * hf:4 * hf + 4, :], ps[hf][:].rearrange("p (r l) -> p r l", r=4),
                                wx[:, 64 + 8 * g + 4 * hf:64 + 8 * g + 4 * hf + 4].unsqueeze(2).to_broadcast([128, 4, 128]), ALU.subtract),
                                reads=["ps%d" % hf, "acs2%d" % sl], writes=["seg%d_%d" % (m, hf)])
                        P.op("act", lambda e: e.activation(segL[m][:], segL[m][:], AF.Exp),
                             reads=["seg%d_0" % m, "seg%d_1" % m], writes=["seg%d_0" % m, "seg%d_1" % m])

                    def s_M(g):
                        m = g % 2
                        P.op("dve", lambda e: e.scalar_tensor_tensor(out=Mt2[m][:], in0=segL[m][:], scalar=1e30,
                                                                     in1=CBa[:, g, :].unsqueeze(1).to_broadcast([128, 8, 128]),
                                                                     op0=ALU.min, op1=ALU.mult),
                             reads=["seg%d_0" % m, "seg%d_1" % m, "CBa%d" % (g // 4)], writes=["Mt%d" % m])
                        for r in range(8):
                            hh = 8 * g + r
                            P.op("pe", lambda e, r=r, hh=hh: e.matmul(ps[2][:, r * 64:(r + 1) * 64], lhsT=Mt2[m][:, r, :],
                                                                       rhs=xtm[sl][:, hh * 64:(hh + 1) * 64], start=True, stop=(d != 0)),
                                 reads=["Mt%d" % m, "xtm%d" % sl], writes=["ps2"])
                            if d == 0:
                                P.op("pe", lambda e, r=r, hh=hh: e.matmul(ps[2][:, r * 64:(r + 1) * 64], lhsT=DIb[:, hh, :],
                                                                           rhs=xtm[sl][:, hh * 64:(hh + 1) * 64], start=False, stop=True),
                                     reads=["DIb", "xtm%d" % sl], writes=["ps2"])
                        P.op("pe", lambda e: e.matmul(ps[3][:], lhsT=bct[sl][:, 8 + g, :], rhs=hbf[:, g * 512:(g + 1) * 512],
                                                      start=True, stop=True), reads=["bct%d" % sl, "hbf"], writes=["ps3"])

                    def s_Y(g):
                        P.op("dve", lambda e: e.tensor_tensor(ytmp[:].rearrange("p (r q) -> p r q", r=8),
                                                              ps[3][:].rearrange("p (r q) -> p r q", r=8),
                                                              eacs[:, 8 * g:8 * g + 8].unsqueeze(2).to_broadcast([128, 8, 64]), ALU.mult),
                             reads=["ps3", "eacs%d" % sl], writes=["ytmp"])
                        P.op("dve", lambda e: e.tensor_tensor(yt[:, g * 512:(g + 1) * 512], ps[2][:], ytmp[:], ALU.add),
                             reads=["ps2", "ytmp"], writes=["yt"])

                    for k in range(-2, 9):
                        if 0 <= k - 1 < 8:
                            s_Y(k - 1)
                        if 0 <= k + 1 < 8:
                            s_seg(k + 1)
                        if 0 <= k + 2 < 8:
                            s_TA(k + 2)
                        pump(1)
                        if 0 <= k < 8:
                            s_M(k)
                        pump(1)
                    P.dma((Y1 if d == 0 else Y2)[tile * 128:(tile + 1) * 128, :], yt[:], reads=["yt"], key="st")
                for half in range(2):
                    pump(4)
                    for g4 in range(4):
                        g = half * 4 + g4
                        P.op("pe", lambda e, g=g, g4=g4: e.matmul(ps[g4][:], lhsT=xtm[sl][:, DI + g * 128:DI + (g + 1) * 128],
                                                                  rhs=wxe[:, g * 512:(g + 1) * 512], start=True, stop=True),
                             reads=["xtm%d" % sl, "wxe%d" % sl], writes=["ps%d" % g4])
                    for g4 in range(4):
                        g = half * 4 + g4
                        eng = "dve" if g4 % 2 == 0 else "pool"
                        P.op("dve", lambda e, g=g: e.tensor_tensor(h[:, g * 512:(g + 1) * 512].rearrange("p (r q) -> p r q", r=8),
                                                                   h[:, g * 512:(g + 1) * 512].rearrange("p (r q) -> p r q", r=8),
                                                                   etot[:, 8 * g:8 * g + 8].unsqueeze(2).to_broadcast([128, 8, 64]), ALU.mult),
                             reads=[hn, "etot%d" % sl, "hbf"], writes=[hn])
                        P.op("dve", lambda e, g=g, g4=g4: e.tensor_tensor(h[:, g * 512:(g + 1) * 512], h[:, g * 512:(g + 1) * 512],
                                                                          ps[g4][:], ALU.add),
                             reads=[hn, "ps%d" % g4], writes=[hn])

            seq = ([(16, 0, False), (17, 0, False)] + [(t, 0, True) for t in range(8)] + [(17, 1, False), (16, 1, False)]
                   + [(t, 1, False) for t in range(15, 7, -1)] + [(t, 1, True) for t in range(7, -1, -1)])
            gens = [setup(sq[0], sq[1], sq[2], i % 2) for i, sq in enumerate(seq)]
            for _ in gens[0]:
                pass
            for i, sq in enumerate(seq):
                nxt = gens[i + 1] if i + 1 < len(seq) else None

                def pump(k, nxt=nxt):
                    if nxt is not None:
                        for _ in range(k):
                            next(nxt, None)
                body(sq[0], sq[1], sq[2], i % 2, pump)
                if nxt is not None:
                    for _ in nxt:
                        pass
        P.barrier()

        with ExitStack() as st:
            mT = sbt(st, "mT", [128, 16, 1024], BF16)
            with ExitStack() as st5:
                ynT = sbt(st5, "ynT", [128, 32, 1024], BF16)
                with ExitStack() as st5a:
                    y1L = [sbt(st5a, "y1%d" % i, [128, DI]) for i in range(2)]; y2L = [sbt(st5a, "y2%d" % i, [128, DI]) for i in range(2)]
                    zz = sbt(st5a, "zz", [128, DI])
                    gN = sbt(st5a, "gN", [128, DI]); ynb = sbt(st5a, "ynb", [128, DI], BF16)
                    ss5 = sbt(st5a, "ss5", [128, 4])
                    P.dma(gN[:], gN_d.partition_broadcast(128), writes=["gN"])
                    for t in range(8):
                        y1 = y1L[t % 2]; y2 = y2L[t % 2]; y1n = "y1%d" % (t % 2); y2n = "y2%d" % (t % 2)
                        P.dma(y1[:], Y1[t * 128:(t + 1) * 128, :], writes=[y1n])
                        P.dma(y2[:], Y2[t * 128:(t + 1) * 128, :], writes=[y2n])
                        P.dma(zz[:], Zs[t * 128:(t + 1) * 128, :], writes=["zz"])
                        P.op("dve", lambda e, y1=y1, y2=y2: e.tensor_tensor(y1[:], y1[:], y2[:], ALU.add), reads=[y1n, y2n], writes=[y1n])
                        P.op("act", lambda e: e.activation(zz[:], zz[:], AF.Silu), reads=["zz"], writes=["zz"])
                        P.op("dve", lambda e, y1=y1: e.tensor_tensor(y1[:], y1[:], zz[:], ALU.mult), reads=[y1n, "zz"], writes=[y1n])
                        P.op("dve", lambda e: e.memset(ss5[:, 0:1], 0.0), writes=["ss5a"])
                        P.op("act", lambda e, y1=y1, y2=y2: e.activation(y2[:], y1[:], AF.Square, accum_out=ss5[:, 0:1]),
                             reads=[y1n, "ss5a"], writes=[y2n, "ss5a"])
                        P.op("act", lambda e: e.activation(ss5[:, 1:2], ss5[:, 0:1], AF.Sqrt, bias=cst[:, 1:2], scale=1.0 / DI),
                             reads=["ss5a"], writes=["ss5b"])
                        P.op("dve", lambda e: e.reciprocal(ss5[:, 2:3], ss5[:, 1:2]), reads=["ss5b"], writes=["ss5c"])
                        P.op("dve", lambda e, y1=y1: e.scalar_tensor_tensor(out=ynb[:], in0=y1[:], scalar=ss5[:, 2:3], in1=gN[:],
                                                                     op0=ALU.mult, op1=ALU.mult),
                             reads=[y1n, "ss5c", "gN"], writes=["ynb"])
                        for q in range(4):
                            hb = q % 2
                            for j in range(8):
                                kc = q * 8 + j
                                P.op("pe", lambda e, hb=hb, j=j, kc=kc: e.transpose(
                                    psb[hb][:, j * 128:(j + 1) * 128], ynb[:, kc * 128:(kc + 1) * 128], identb[:]),
                                    reads=["ynb", "identb"], writes=["psb%d" % hb])
                            copy_op(evac_eng(), ynT[:, q * 8:(q + 1) * 8, t * 128:(t + 1) * 128],
                                    psb[hb][:].rearrange("p (k t) -> p k t", k=8), reads=["psb%d" % hb], writes=["ynT"])
                P.barrier()
                aTs = sbt(st5, "aTs", [128, 16, 1024], BF16)
                P.dma(aTs[:], ATT, writes=["aTs"])
                wbsL = [sbt(st5, "wbs%d" % i, [128, 32, 256], BF16) for i in range(2)]
                wbaL = [sbt(st5, "wba%d" % i, [128, 16, 256], BF16) for i in range(2)]
                gsL = [sbt(st5, "gs%d" % i, [128, 512]) for i in range(2)]; gaL = [sbt(st5, "ga%d" % i, [128, 512]) for i in range(2)]
                m1L = [sbt(st5, "m1%d" % i, [128, 512]) for i in range(2)]; m2L = [sbt(st5, "m2%d" % i, [128, 512]) for i in range(2)]
                for blk in range(8):
                    wbs = wbsL[blk % 2]; wba = wbaL[blk % 2]
                    wbsr = ["wbs%d_%d" % (blk % 2, k0) for k0 in range(0, 32, 4)]
                    wbar = ["wba%d_%d" % (blk % 2, k0) for k0 in range(0, 16, 4)]
                    for k0 in range(0, 32, 4):
                        P.dma(wbs[:, k0:k0 + 4, :], w_bs[k0 * 128:(k0 + 4) * 128, blk * 256:(blk + 1) * 256].rearrange("(k p) n -> p k n", p=128),
                              writes=["wbs%d_%d" % (blk % 2, k0)], key=("wbs", blk % 2), queue="pool")
                    for k0 in range(0, 16, 4):
                        P.dma(wba[:, k0:k0 + 4, :], w_ba[k0 * 128:(k0 + 4) * 128, blk * 256:(blk + 1) * 256].rearrange("(k p) n -> p k n", p=128),
                              writes=["wba%d_%d" % (blk % 2, k0)], key=("wba", blk % 2), queue="pool")
                    for ch in range(2):
                        cabs = blk * 2 + ch
                        for th in range(2):
                            t0 = th * 512
                            u5 = (cabs * 2 + th) % 2
                            gsu, gau, m1u, m2u = gsL[u5], gaL[u5], m1L[u5], m2L[u5]
                            pA, pB = 2 * u5, 2 * u5 + 1
                            P.dma(gsu[:], GTs[cabs * 128:(cabs + 1) * 128, t0:t0 + 512], writes=["gs%d" % u5])
                            P.dma(gau[:], GTs[2048 + cabs * 128:2048 + (cabs + 1) * 128, t0:t0 + 512], writes=["ga%d" % u5])
                            P.op("act", lambda e, gsu=gsu: e.activation(gsu[:], gsu[:], AF.Sigmoid), reads=["gs%d" % u5], writes=["gs%d" % u5])
                            P.op("act", lambda e, gau=gau: e.activation(gau[:], gau[:], AF.Sigmoid), reads=["ga%d" % u5], writes=["ga%d" % u5])
                            for kc in range(32):
                                P.op("pe", lambda e, kc=kc, ch=ch, t0=t0, wbs=wbs, pA=pA: e.matmul(ps[pA][:], lhsT=wbs[:, kc, ch * 128:(ch + 1) * 128],
                                                                                  rhs=ynT[:, kc, t0:t0 + 512], start=(kc == 0), stop=(kc == 31)),
                                     reads=wbsr + ["ynT"], writes=["ps%d" % pA])
                            for kc in range(16):
                                P.op("pe", lambda e, kc=kc, ch=ch, t0=t0, wba=wba, pB=pB: e.matmul(ps[pB][:], lhsT=wba[:, kc, ch * 128:(ch + 1) * 128],
                                                                                  rhs=aTs[:, kc, t0:t0 + 512], start=(kc == 0), stop=(kc == 15)),
                                     reads=wbar + ["aTs"], writes=["ps%d" % pB])
                            P.op("dve", lambda e, m1u=m1u, gsu=gsu, pA=pA: e.tensor_tensor(m1u[:], ps[pA][:], gsu[:], ALU.mult),
                                 reads=["ps%d" % pA, "gs%d" % u5], writes=["m1%d" % u5])
                            P.op("dve", lambda e, m2u=m2u, gau=gau, pB=pB: e.tensor_tensor(m2u[:], ps[pB][:], gau[:], ALU.mult),
                                 reads=["ps%d" % pB, "ga%d" % u5], writes=["m2%d" % u5])
                            P.op("dve", lambda e, cabs=cabs, t0=t0, m1u=m1u, m2u=m2u: e.tensor_tensor(mT[:, cabs, t0:t0 + 512], m1u[:], m2u[:], ALU.add),
                                 reads=["m1%d" % u5, "m2%d" % u5], writes=["mT"])
            P.barrier()
            wo = [sbt(st, "wo%d" % i, [128, 16, 512], BF16) for i in range(2)]
            mod2 = sbt(st, "mod2", [128, D])
            xo = [sbt(st, "xo%d" % i, [128, 512]) for i in range(2)]
            ho = [sbt(st, "ho%d" % i, [128, 512]) for i in range(2)]
            P.dma(mod2[:], MOD[2], writes=["mod2"])
            hi = 0
            for blk in range(4):
                sl = blk % 2
                load_w_block(wo, sl, w_o, 16, blk * 512, 512)
                for t in range(8):
                    i = hi % 2
                    hi += 1
                    P.dma(xo[i][:], x_c[t * 128:(t + 1) * 128, blk * 512:(blk + 1) * 512], writes=["xo%d" % i], key=("xo", i))
                    b = hi % 2
                    for kc in range(16):
                        P.op("pe", lambda e, b=b, kc=kc, t=t, sl=sl: e.matmul(ps[b][:], lhsT=mT[:, kc, t * 128:(t + 1) * 128],
                                                                              rhs=wo[sl][:, kc, :], start=(kc == 0), stop=(kc == 15)),
                             reads=["mT"] + ["wb%d_%d" % (sl, k0) for k0 in range(0, 16, 4)], writes=["ps%d" % b])
                    P.op("dve", lambda e, b=b, i=i, blk=blk: e.tensor_tensor(ho[i][:], ps[b][:], mod2[:, blk * 512:(blk + 1) * 512], ALU.mult),
                         reads=["ps%d" % b, "mod2"], writes=["ho%d" % i])
                    P.op("dve", lambda e, i=i: e.tensor_tensor(ho[i][:], ho[i][:], xo[i][:], ALU.add),
                         reads=["ho%d" % i, "xo%d" % i], writes=["ho%d" % i])
                    P.dma(Hs[t * 128:(t + 1) * 128, blk * 512:(blk + 1) * 512], ho[i][:], reads=["ho%d" % i], key="st")
        P.barrier()

        with ExitStack() as st:
            u2T = sbt(st, "u2T", [128, 16, 1024], BF16)
            with ExitStack() as st6:
                A2 = sbt(st6, "A2", [128, D]); S3 = sbt(st6, "S3", [128, D])
                P.dma(A2[:], MOD[3], writes=["A2"])
                P.dma(S3[:], MOD[4], writes=["S3"])
                norm_mod_T(st6, lambda t: Hs[t * 128:(t + 1) * 128, :], 8, lambda t: (A2, "A2"), lambda t: (S3, "S3"), u2T, "n2")
            P.barrier()
            with ExitStack() as st6:
                q2T = sbt(st6, "q2T", [128, 16, 1024], BF16)
                with ExitStack() as st6w:
                    wq = [sbt(st6w, "wq%d" % i, [128, 16, 512], BF16) for i in range(2)]
                    for blk in range(4):
                        sl = blk % 2
                        load_w_block(wq, sl, w_q, 16, blk * 512, 512)

                        def cons(pst, psn, ch, t0, nt, blk=blk):
                            copy_op(evac_eng(), q2T[:, blk * 4 + ch, t0:t0 + nt], pst[:, 0:nt], reads=[psn], writes=["q2T"])
                        linear_F(u2T, "xT_n2", 16, wq, sl, 512, [(0, 512), (512, 512)], cons)
                P.barrier()
                kin6 = sbt(st6, "kin6", [128, 2, 128])
                kT6 = sbt(st6, "kT6", [128, 2, 128], BF16)
                P.dma(kin6[:, 0, :], keys1, writes=["kin6_a"], key="s6k")
                P.dma(kin6[:, 1, :], keys2, writes=["kin6_b"], key="s6k")
                for i in range(2):
                    P.op("pe", lambda e, i=i: e.transpose(ps[4][:, i * 128:(i + 1) * 128], kin6[:, i, :], ident[:]),
                         reads=["kin6_a", "kin6_b", "ident"], writes=["ps4"])
                P.op("dve", lambda e: e.tensor_copy(kT6[:], ps[4][:, 0:256].rearrange("p (i k) -> p i k", i=2)),
                     reads=["ps4"], writes=["kT6"])
                sc = sbt(st6, "sc", [128, 16, 128]); tmp6 = sbt(st6, "tmp6", [128, 256])
                mx = sbt(st6, "mx", [128, 16, 16]); negm = sbt(st6, "negm", [128, 16])
                E = sbt(st6, "E", [128, 16, 128]); Et = sbt(st6, "Et", [128, 16, 16])
                cand = sbt(st6, "cand", [128, 8, 256]); ctop = sbt(st6, "ctop", [128, 8, 16])
                Zs6 = sbt(st6, "Zs6", [128, 8]); rZ = sbt(st6, "rZ", [128, 8])
                Pd = [sbt(st6, "Pd%d" % i, [128, 8, 128]) for i in range(3)]
                Gm = [sbt(st6, "Gm%d" % i, [128, 8, 8, 128], BF16) for i in range(2)]
                gst = [sbt(st6, "gst%d" % i, [128, 8, 128], BF16) for i in range(2)]
                E1n = sbt(st6, "E1n", [128, 8, 128]); Etn = sbt(st6, "Etn", [128, 8, 16]); ctop2 = sbt(st6, "ctop2", [128, 8, 16])
                thrg = sbt(st6, "thrg", [128, 8])
                gi = {"n": 0}
                pub = [sbt(st6, "pub%d" % i, [128, D], BF16) for i in range(2)]
                puT = [sbt(st6, "puT%d" % i, [128, 16, 128], BF16) for i in range(2)]
                actb = [sbt(st6, "actb%d" % i, [128, 1024], BF16) for i in range(2)]

                def phaseA(ch):
                    sl = ch % 2
                    P.dma(pub[sl][:], pu[ch * 128:(ch + 1) * 128, :], writes=["pub%d" % sl], key=("pub", sl), queue="pool")
                    for half in range(2):
                        for j in range(8):
                            kc = half * 8 + j
                            P.op("pe", lambda e, kc=kc, half=half, j=j: e.transpose(
                                psb[half][:, j * 128:(j + 1) * 128], pub[sl][:, kc * 128:(kc + 1) * 128], identb[:]),
                                reads=["pub%d" % sl, "identb"], writes=["psb%d" % half])
                        copy_op("act", puT[sl][:, half * 8:(half + 1) * 8, :], psb[half][:].rearrange("p (k t) -> p k t", k=8),
                                reads=["psb%d" % half], writes=["puT%d" % sl])
                    for th in range(2):
                        for kc in range(16):
                            P.op("pe", lambda e, kc=kc, th=th: e.matmul(ps[4 + th][:], lhsT=puT[sl][:, kc, :],
                                                                        rhs=u2T[:, kc, th * 512:(th + 1) * 512],
                                                                        start=(kc == 0), stop=(kc == 15)),
                                 reads=["puT%d" % sl, "xT_n2"], writes=["ps%d" % (4 + th)])
                        P.op("act", lambda e, th=th: e.activation(actb[sl][:, th * 512:(th + 1) * 512], ps[4 + th][:], AF.Gelu),
                             reads=["ps%d" % (4 + th)], writes=["actb%d_%d" % (sl, th)])
                    P.dma(ACTs[ch * 128:(ch + 1) * 128, :], actb[sl][:], reads=["actb%d_0" % sl, "actb%d_1" % sl], key=("st", "actb%d" % sl))

                def g_all():
                    for t in range(8):
                        for c in range(16):
                            P.op("pe", lambda e, c=c, t=t: e.matmul(ps[c // 4][:, (c % 4) * 128:(c % 4 + 1) * 128],
                                                                    lhsT=q2T[:, c, t * 128:(t + 1) * 128], rhs=kT6[:, c % 2, :],
                                                                    start=True, stop=True), reads=["q2T", "kT6"], writes=["ps%d" % (c // 4)])
                        for b4 in range(4):
                            copy_op(evac_eng(), sc[:, b4 * 4:(b4 + 1) * 4, :], ps[b4][:].rearrange("p (c k) -> p c k", c=4),
                                    reads=["ps%d" % b4], writes=["sc"])
                        for c in range(16):
                            P.op("dve", lambda e, c=c: e.max(out=mx[:, c, 0:8], in_=sc[:, c, :]), reads=["sc"], writes=["mx"])
                            P.op("dve", lambda e, c=c: e.match_replace(out=tmp6[:, 0:128], in_to_replace=mx[:, c, 0:8], in_values=sc[:, c, :],
                                                                       imm_value=-1e30), reads=["sc", "mx"], writes=["tmp6"])
                            P.op("dve", lambda e, c=c: e.max(out=mx[:, c, 8:16], in_=tmp6[:, 0:128]), reads=["tmp6"], writes=["mx"])
                        P.op("dve", lambda e: e.tensor_scalar(negm[:], mx[:, :, 0], -1.0, None, op0=ALU.mult), reads=["mx"], writes=["negm"])
                        for c in range(16):
                            P.op("act", lambda e, c=c: e.activation(E[:, c, :], sc[:, c, :], AF.Exp, bias=negm[:, c:c + 1]),
                                 reads=["sc", "negm"], writes=["E"])
                            P.op("act", lambda e, c=c: e.activation(Et[:, c, :], mx[:, c, :], AF.Exp, bias=negm[:, c:c + 1]),
                                 reads=["mx", "negm"], writes=["Et"])
                        Et4 = Et[:].rearrange("p (h two) a -> p h two a", two=2)
                        P.op("dve", lambda e, Et4=Et4: e.tensor_tensor(cand[:].rearrange("p h (a b) -> p h a b", a=16),
                                                                       Et4[:, :, 0, :].unsqueeze(3).to_broadcast([128, 8, 16, 16]),
                                                                       Et4[:, :, 1, :].unsqueeze(2).to_broadcast([128, 8, 16, 16]), ALU.mult),
                             reads=["Et"], writes=["cand"])
                        for hh in range(8):
                            P.op("dve", lambda e, hh=hh: e.max(out=ctop[:, hh, 0:8], in_=cand[:, hh, :]), reads=["cand"], writes=["ctop"])
                            P.op("dve", lambda e, hh=hh: e.match_replace(out=tmp6[:], in_to_replace=ctop[:, hh, 0:8], in_values=cand[:, hh, :],
                                                                         imm_value=-1e30), reads=["cand", "ctop"], writes=["tmp6"])
                            P.op("dve", lambda e, hh=hh: e.max(out=ctop[:, hh, 8:16], in_=tmp6[:]), reads=["tmp6"], writes=["ctop"])
                        P.op("dve", lambda e: e.reduce_sum(Zs6[:], ctop[:], axis=AX.X), reads=["ctop"], writes=["Zs6"])
                        P.op("dve", lambda e: e.reciprocal(rZ[:], Zs6[:]), reads=["Zs6"], writes=["rZ"])
                        E4 = E[:].rearrange("p (h two) k -> p h two k", two=2)
                        P.op("dve", lambda e, E4=E4: e.tensor_tensor(E1n[:], E4[:, :, 0, :], rZ[:].unsqueeze(2).to_broadcast([128, 8, 128]), ALU.mult),
                             reads=["E", "rZ"], writes=["E1n"])
                        P.op("dve", lambda e: e.tensor_tensor(thrg[:], ctop[:, :, 15], rZ[:], ALU.mult), reads=["ctop", "rZ"], writes=["thrg"])
                        P.op("dve", lambda e: e.tensor_scalar(thrg[:], thrg[:], 1.0 - 1e-6, None, op0=ALU.mult), reads=["thrg"], writes=["thrg"])
                        yield
                        for ib in range(16):
                            w = ib % 2
                            for hh in range(8):
                                k3 = gi["n"] % 3
                                gi["n"] += 1
                                if False:
                                    for j in range(8):
                                        P.op("act", lambda e, hh=hh, ib=ib, k3=k3, j=j: e.activation(
                                            Pd[k3][:, j, :], E[:, 2 * hh + 1, :], AF.Identity, scale=E1n[:, hh, ib * 8 + j:ib * 8 + j + 1]),
                                            reads=["E", "E1n"], writes=["Pd%d" % k3])
                                else:
                                    P.op("dve", lambda e, hh=hh, ib=ib, k3=k3: e.tensor_tensor(
                                        Pd[k3][:], E1n[:, hh, ib * 8:(ib + 1) * 8].unsqueeze(2).to_broadcast([128, 8, 128]),
                                        E[:, 2 * hh + 1, :].unsqueeze(1).to_broadcast([128, 8, 128]), ALU.mult),
                                        reads=["E", "E1n"], writes=["Pd%d" % k3])
                                P.op("dve", lambda e, hh=hh, k3=k3, w=w: e.scalar_tensor_tensor(
                                    out=Gm[w][:, hh, :, :], in0=Pd[k3][:], scalar=thrg[:, hh:hh + 1], in1=Pd[k3][:], op0=ALU.is_ge, op1=ALU.mult),
                                    reads=["Pd%d" % k3, "thrg"], writes=["Gm%d_%d" % (w, hh)])
                            for j in range(8):
                                for hh in range(8):
                                    P.op("pe", lambda e, hh=hh, j=j, w=w: e.matmul(
                                        ps[2 * w + j // 4][:, (j % 4) * 128:(j % 4 + 1) * 128], lhsT=Gm[w][:, hh, j, :], rhs=identb[:],
                                        start=(hh == 0), stop=(hh == 7)),
                                        reads=["Gm%d_%d" % (w, hh), "identb"], writes=["ps%d" % (2 * w + j // 4)])
                            for q in range(2):
                                copy_op(evac_eng() if False else "act", gst[w][:, q * 4:(q + 1) * 4, :], ps[2 * w + q][:].rearrange("p (k t) -> p k t", k=4),
                                        reads=["ps%d" % (2 * w + q)], writes=["gst%d" % w])
                            P.dma(GTP[ib * 1024:(ib + 1) * 1024, t * 128:(t + 1) * 128].rearrange("(c p) t -> p c t", p=128), gst[w][:],
                                  reads=["gst%d" % w], key="st")
                            yield

                nxt = 0
                for k, _ in enumerate(g_all()):
                    if nxt < 128:
                        phaseA(nxt)
                        nxt += 1
                while nxt < 128:
                    phaseA(nxt)
                    nxt += 1
            P.barrier()
            with ExitStack() as st6:
                acc = sbt(st6, "acc", [128, 8, D])
                pvb = [sbt(st6, "pvb%d" % i, [128, 4, D], BF16) for i in range(2)]
                gtc = [sbt(st6, "gtc%d" % i, [128, 4, 1024], BF16) for i in range(2)]
                actc = [sbt(st6, "actc%d" % i, [128, 4, 1024], BF16) for i in range(2)]
                coef = actc
                P.op("pool", lambda e: e.memset(acc[:], 0.0), writes=["acc"])
                for grp in range(32):
                    gw = grp % 2
                    r0 = grp * 512
                    P.dma(pvb[gw][:], pv[r0:r0 + 512, :].rearrange("(c p) d -> p c d", p=128), writes=["pvb%d" % gw], key=("pvb", gw), queue="pool")
                    P.dma(gtc[gw][:], GTP[r0:r0 + 512, :].rearrange("(c p) t -> p c t", p=128), writes=["gtc%d" % gw], key=("gtc", gw))
                    P.dma(actc[gw][:], ACTs[r0:r0 + 512, :].rearrange("(c p) t -> p c t", p=128), writes=["actc%d" % gw], key=("actc", gw))
                    P.op("dve", lambda e, gw=gw: e.tensor_tensor(actc[gw][:], actc[gw][:], gtc[gw][:], ALU.mult),
                         reads=["actc%d" % gw, "gtc%d" % gw], writes=["actc%d" % gw, "coef%d" % gw])
                    for t in range(8):
                        for dq in range(4):
                            for c4 in range(4):
                                P.op("pe", lambda e, gw=gw, c4=c4, t=t, dq=dq: e.matmul(
                                    ps[dq][:], lhsT=coef[gw][:, c4, t * 128:(t + 1) * 128], rhs=pvb[gw][:, c4, dq * 512:(dq + 1) * 512],
                                    start=(c4 == 0), stop=(c4 == 3)), reads=["actc%d" % gw, "pvb%d" % gw], writes=["ps%d" % dq])
                        for dq in range(4):
                            eng = "dve" if dq % 2 == 0 else "pool"
                            P.op("dve", lambda e, t=t, dq=dq: e.tensor_tensor(acc[:, t, dq * 512:(dq + 1) * 512],
                                                                              acc[:, t, dq * 512:(dq + 1) * 512], ps[dq][:], ALU.add),
                                 reads=["acc", "ps%d" % dq], writes=["acc"])
                mod5 = sbt(st6, "mod5", [128, D]); gF = sbt(st6, "gF", [128, D])
                hin = sbt(st6, "hin", [128, D]); fo = sbt(st6, "fo", [128, D]); ssf = sbt(st6, "ssf", [128, 4])
                P.dma(mod5[:], MOD[5], writes=["mod5"])
                P.dma(gF[:], gF_d.partition_broadcast(128), writes=["gF"])
                for t in range(8):
                    P.dma(hin[:], Hs[t * 128:(t + 1) * 128, :], writes=["hin"])
                    P.op("dve", lambda e, t=t: e.tensor_tensor(acc[:, t, :], acc[:, t, :], mod5[:], ALU.mult),
                         reads=["acc", "mod5"], writes=["acc"])
                    P.op("dve", lambda e, t=t: e.tensor_tensor(hin[:], hin[:], acc[:, t, :], ALU.add), reads=["hin", "acc"], writes=["hin"])
                    P.op("dve", lambda e: e.memset(ssf[:, 0:1], 0.0), writes=["ssfa"])
                    P.op("act", lambda e: e.activation(fo[:], hin[:], AF.Square, accum_out=ssf[:, 0:1]), reads=["hin", "ssfa"], writes=["fo", "ssfa"])
                    P.op("act", lambda e: e.activation(ssf[:, 1:2], ssf[:, 0:1], AF.Sqrt, bias=cst[:, 1:2], scale=1.0 / D),
                         reads=["ssfa"], writes=["ssfb"])
                    P.op("dve", lambda e: e.reciprocal(ssf[:, 2:3], ssf[:, 1:2]), reads=["ssfb"], writes=["ssfc"])
                    P.op("dve", lambda e: e.scalar_tensor_tensor(out=fo[:], in0=hin[:], scalar=ssf[:, 2:3], in1=gF[:], op0=ALU.mult, op1=ALU.mult),
                         reads=["hin", "ssfc", "gF", "fo"], writes=["fo"])
                    P.dma(out_d[t * 128:(t + 1) * 128, :], fo[:], reads=["fo"], key="out")
        P.emit()
    return nc


def _rope_tables(flip):
    pos = np.arange(9 * 128)
    if flip:
        pos = (S - 1) - pos
    row = (pos // 64).astype(np.float32)
    col = (pos % 64).astype(np.float32)
    freqs = (np.float32(10000.0) ** (-np.arange(32, dtype=np.float32) / np.float32(32))).astype(np.float32)
    ar = row[:, None] * freqs[None, :]
    ac = col[:, None] * freqs[None, :]
    ang = np.concatenate([ar, ar, ac, ac], axis=-1).astype(np.float32)
    cos = np.cos(ang).astype(np.float32)
    sin = np.sin(ang).astype(np.float32)
    sgn = np.concatenate([-np.ones(32), np.ones(32), -np.ones(32), np.ones(32)]).astype(np.float32)
    return np.ascontiguousarray(np.tile(cos, (1, 16))), np.ascontiguousarray(np.tile(sin * sgn[None, :], (1, 16)))


_NC_CACHE = {}


def make_in_maps(x, c, ctx, c_ctx, ada_w, ada_b, norm1_g, w_in, conv_w, conv_b, dt_bias, a_log, ssm_d,
                 ssm_norm_g, attn_sink, w_branch_ssm, w_branch_attn, w_out, norm2_g, peer_wq, peer_keys1,
                 peer_keys2, peer_u, peer_v, final_norm_g):
    f = lambda a: np.ascontiguousarray(np.asarray(a, dtype=np.float32))
    x = f(x); ctx = f(ctx); c = f(c); c_ctx = f(c_ctx)
    w_in0 = f(w_in)[0]
    shared = {
        "ada_w": f(ada_w)[0], "ada_b": f(ada_b)[0][None, :], "norm1_g": f(norm1_g)[0][None, :],
        "norm2_g": f(norm2_g)[0][None, :], "final_g": f(final_norm_g)[None, :], "w_in": w_in0,
        "ssm_d": f(ssm_d)[0][None, :], "ssm_norm_g": f(ssm_norm_g)[0][None, :], "attn_sink": f(attn_sink)[0][None, :],
        "w_bs": f(w_branch_ssm)[0], "w_ba": f(w_branch_attn)[0], "w_o": f(w_out)[0], "w_q": f(peer_wq)[0],
        "keys1": f(peer_keys1)[0], "keys2": f(peer_keys2)[0], "peer_u": f(peer_u)[0], "peer_v": f(peer_v)[0],
        "ident": np.eye(128, dtype=np.float32),
        "triF": np.triu(np.ones((128, 128), np.float32)),
        "triB": np.tril(np.ones((128, 128), np.float32)),
        "cc_fm": np.ascontiguousarray(c_ctx.reshape(16, 128).T),
    }
    cw = f(conv_w)[0]
    cb = f(conv_b)[0]
    convb_fm = np.ascontiguousarray(cb.reshape(48, 128).T)
    wdt = w_in0[:, C_DT:C_DT + 128]
    per_flip = {}
    for flip in (0, 1):
        cwf = cw[::-1] if flip else cw
        convw_fm = np.ascontiguousarray(cwf.T.reshape(48, 128, 5).transpose(1, 0, 2))
        wd = np.concatenate([wdt[:, 64:], wdt[:, :64]], axis=1) if flip else wdt
        dtb = f(dt_bias)[0][::-1] if flip else f(dt_bias)[0]
        al = f(a_log)[0][::-1] if flip else f(a_log)[0]
        cos_t, sin_t = _rope_tables(flip)
        per_flip[flip] = {"convw_fm": convw_fm, "convb_fm": convb_fm, "w_dt": np.ascontiguousarray(wd),
                          "dt_bias": np.ascontiguousarray(dtb.reshape(1, 128)), "a_log": np.ascontiguousarray(al.reshape(1, 128)),
                          "cos_t": cos_t, "sin_t": sin_t}
    in_maps = []
    for core in range(8):
        b, s = core // 2, core % 2
        m = dict(shared)
        m.update(per_flip[s])
        m["x_c"] = np.ascontiguousarray(x[b, ::-1] if s else x[b])
        m["ctx_c"] = np.ascontiguousarray(ctx[b, ::-1] if s else ctx[b])
        m["c_fm"] = np.ascontiguousarray(c[b].reshape(16, 128).T)
        in_maps.append(m)
    return in_maps


def kernel(**inputs):
    in_maps = make_in_maps(**inputs)
    if "nc" not in _NC_CACHE:
        _NC_CACHE["nc"] = build(False)
    nc = _NC_CACHE["nc"]
    res = run_bass_kernel_spmd(nc, in_maps, core_ids=list(range(8)))
    out = np.zeros((4, S, D), np.float32)
    for core in range(8):
        b, s = core // 2, core % 2
        o = np.asarray(res.results[core]["out"], dtype=np.float32)
        if s == 0:
            out[b, 0:1024] = o
        else:
            out[b, 1024:2048] = o[::-1]
    return out
```

```python
import math
from contextlib import ExitStack
import numpy as np
import concourse.bass as bass
import concourse.mybir as mybir
from concourse.bass_utils import run_bass_kernel_spmd

F32 = mybir.dt.float32
BF16 = mybir.dt.bfloat16
AF = mybir.ActivationFunctionType
ALU = mybir.AluOpType
AX = mybir.AxisListType

D = 2048
S = 2048
CTXL = 256
NTOK = S + CTXL
NT_ALL = 18
NT_OWN = 8
DI = 4096
NH = 64
C_K, C_V, C_XBC, C_DT, C_Q, C_Z, C_G = 0, 512, 1024, 7168, 7296, 9344, 13440
NE = 16384
EPS = 1e-6
SCALE = 128.0 ** -0.5

COMPUTE = ("pe", "act", "dve", "pool")
ALLENG = ("pe", "act", "dve", "pool", "sp")


class Prog:
    def __init__(self, nc, n_dma_sems=96):
        self.nc = nc
        self.lists = {e: [] for e in ALLENG}
        self.semnames = []
        self.semval = {}
        self.waited = {e: {} for e in ALLENG}
        self.res = {}
        for e in COMPUTE:
            self._newsem("E_" + e)
        self.n_dma_sems = n_dma_sems
        self.dma_keys = {}

    def _newsem(self, key):
        self.semnames.append(key)
        self.semval[key] = 0

    def _dma_sem(self, key):
        if key not in self.dma_keys:
            name = "D_%d" % len(self.dma_keys)
            assert len(self.dma_keys) < self.n_dma_sems, "too many dma sems"
            self.dma_keys[key] = name
            self._newsem(name)
        return self.dma_keys[key]

    def _deps(self, eng, reads, writes):
        evs = {}

        def add(ev):
            if ev is None:
                return
            s, v = ev
            if evs.get(s, 0) < v:
                evs[s] = v

        for r in reads:
            st = self.res.get(r)
            if st:
                add(st["w"])
        for w in writes:
            st = self.res.get(w)
            if st:
                add(st["w"])
                for s, v in st["r"].items():
                    add((s, v))
        out = []
        for s, v in evs.items():
            if eng == "pe" and s == "E_pe":
                continue
            if self.waited[eng].get(s, 0) >= v:
                continue
            self.waited[eng][s] = v
            out.append((s, v))
        return out

    def _commit(self, ev, reads, writes):
        s, v = ev
        for r in reads:
            st = self.res.setdefault(r, {"w": None, "r": {}})
            if st["r"].get(s, 0) < v:
                st["r"][s] = v
        for w in writes:
            self.res[w] = {"w": ev, "r": {}}

    def op(self, eng, fn, reads=(), writes=()):
        waits = self._deps(eng, reads, writes)
        s = "E_" + eng
        self.semval[s] += 1
        ev = (s, self.semval[s])
        self.lists[eng].append((waits, fn, s, 1))
        self._commit(ev, reads, writes)

    def dma(self, out, in_, reads=(), writes=(), key=None, queue="sp"):
        if key is None or key == "st":
            key = ("ld", writes[0]) if (writes and key is None) else ("st", reads[0])
        s = self._dma_sem(key)
        waits = self._deps(queue, reads, writes)
        self.semval[s] += 16
        ev = (s, self.semval[s])
        self.lists[queue].append((waits, lambda e: e.dma_start(out=out, in_=in_), s, 16))
        self._commit(ev, reads, writes)

    def barrier(self):
        for e in ALLENG:
            waits = []
            for s in self.semnames:
                v = self.semval[s]
                if v > 0 and self.waited[e].get(s, 0) < v:
                    if e == "pe" and s == "E_pe":
                        continue
                    self.waited[e][s] = v
                    waits.append((s, v))
            if waits:
                self.lists[e].append((waits, None, None, 0))
        self.res = {}

    def emit(self):
        nc = self.nc
        waits = [(s, self.semval[s]) for s in self.semnames if self.semval[s] > 0]
        self.lists["sp"].append((waits, None, None, 0))
        with ExitStack() as st:
            sems = {}
            for s in self.semnames:
                sems[s] = st.enter_context(nc.semaphore(s))
            block = st.enter_context(nc.Block())

            def mk(engname):
                def body(eng):
                    for waits, fn, s, inc in self.lists[engname]:
                        for (ws, wv) in waits:
                            eng.wait_ge(sems[ws], wv)
                        if fn is not None:
                            fn(eng).then_inc(sems[s], inc)
                return body

            block.tensor(mk("pe"))
            block.scalar(mk("act"))
            block.vector(mk("dve"))
            block.gpsimd(mk("pool"))
            block.sync(mk("sp"))


def build(debug=False):
    nc = bass.Bass("TRN2", target_bir_lowering=False)
    P = Prog(nc)
    skind = "ExternalOutput" if debug else "Internal"

    def din(name, shape, dt=F32):
        return nc.dram_tensor(name, shape, dt, kind="ExternalInput").ap()

    def dscr(name, shape, dt=F32):
        return nc.dram_tensor(name, shape, dt, kind=skind).ap()

    x_c = din("x_c", [S, D])
    ctx_c = din("ctx_c", [CTXL, D])
    c_fm = din("c_fm", [128, 16])
    cc_fm = din("cc_fm", [128, 16])
    ada_w = din("ada_w", [D, 6 * D])
    ada_b = din("ada_b", [1, 6 * D])
    g1_d = din("norm1_g", [1, D])
    g2_d = din("norm2_g", [1, D])
    gF_d = din("final_g", [1, D])
    w_in = din("w_in", [D, 17536])
    w_dt = din("w_dt", [D, 128])
    convw = din("convw_fm", [128, 48, 5])
    convb = din("convb_fm", [128, 48])
    dtb_d = din("dt_bias", [1, 128])
    alog_d = din("a_log", [1, 128])
    ssmd_d = din("ssm_d", [1, 64])
    gN_d = din("ssm_norm_g", [1, DI])
    sink_d = din("attn_sink", [1, 16])
    w_bs = din("w_bs", [DI, D])
    w_ba = din("w_ba", [D, D])
    w_o = din("w_o", [D, D])
    w_q = din("w_q", [D, D])
    keys1 = din("keys1", [128, 128])
    keys2 = din("keys2", [128, 128])
    pu = din("peer_u", [NE, D])
    pv = din("peer_v", [NE, D])
    ident_d = din("ident", [128, 128])
    triF_d = din("triF", [128, 128])
    triB_d = din("triB", [128, 128])
    cos_d = din("cos_t", [9 * 128, 2048])
    sin_d = din("sin_t", [9 * 128, 2048])
    out_d = nc.dram_tensor("out", [1024, D], F32, kind="ExternalOutput").ap()

    MOD = dscr("MOD_scr", [8, 128, D])
    KV = dscr("KV_scr", [NTOK, 1024])
    DTs = dscr("DT_scr", [NTOK, 128])
    Qs = dscr("Q_scr", [1024, 2048])
    Zs = dscr("Z_scr", [1024, DI])
    XBCT = dscr("XBCT_scr", [6144, NTOK])
    GTs = dscr("GT_scr", [4096, 1024])
    ATT = dscr("ATT_scr", [128, 16, 1024], BF16)
    XTM = dscr("XTM_scr", [NTOK, 5120], BF16)
    BCT = dscr("BCT_scr", [2048, NTOK], BF16)
    Y1 = dscr("Y1_scr", [1024, DI])
    Y2 = dscr("Y2_scr", [1024, DI])
    Hs = dscr("H_scr", [1024, D])
    GTP = dscr("GTP_scr", [NE, 1024], BF16)
    ACTs = dscr("ACT_scr", [NE, 1024], BF16)

    with ExitStack() as top:
        uid = {"n": 0}

        def sbt(st, name, shape, dt=F32):
            uid["n"] += 1
            return st.enter_context(nc.sbuf_tensor("s%d_%s" % (uid["n"], name), shape, dt))

        ps = [top.enter_context(nc.psum_tensor("ps%d" % i, [128, 512], F32)) for i in range(6)]
        psb = [top.enter_context(nc.psum_tensor("psb%d" % i, [128, 1024], BF16)) for i in range(2)]
        ident = sbt(top, "ident", [128, 128])
        identb = sbt(top, "identb", [128, 128], BF16)
        triF = sbt(top, "triF", [128, 128])
        triB = sbt(top, "triB", [128, 128])
        triFb = sbt(top, "triFb", [128, 128], BF16)
        triBb = sbt(top, "triBb", [128, 128], BF16)
        ones = sbt(top, "ones", [128, 128])
        onesb = sbt(top, "onesb", [128, 128], BF16)
        cst = sbt(top, "cst", [128, 4])
        P.dma(ident[:], ident_d, writes=["ident"])
        P.dma(triF[:], triF_d, writes=["triF"])
        P.dma(triB[:], triB_d, writes=["triB"])
        P.op("dve", lambda e: e.tensor_copy(identb[:], ident[:]), reads=["ident"], writes=["identb"])
        P.op("dve", lambda e: e.tensor_copy(triFb[:], triF[:]), reads=["triF"], writes=["triFb"])
        P.op("dve", lambda e: e.tensor_copy(triBb[:], triB[:]), reads=["triB"], writes=["triBb"])
        P.op("dve", lambda e: e.memset(ones[:], 1.0), writes=["ones"])
        P.op("dve", lambda e: e.memset(onesb[:], 1.0), writes=["onesb"])
        P.op("dve", lambda e: e.memset(cst[:, 0:1], 1.0), writes=["cst0"])
        P.op("dve", lambda e: e.memset(cst[:, 1:2], EPS), writes=["cst1"])
        P.op("dve", lambda e: e.memset(cst[:, 2:3], 0.0), writes=["cst2"])
        P.barrier()
        rr = {"n": 0}

        def evac_eng():
            rr["n"] += 1
            return "act" if rr["n"] % 2 == 0 else "dve"

        def copy_op(eng, out, in_, reads, writes):
            if eng == "act":
                P.op("act", lambda e: e.copy(out, in_), reads=reads, writes=writes)
            else:
                P.op(eng, lambda e: e.tensor_copy(out, in_), reads=reads, writes=writes)

        def load_w_block(wb, slot, Wd, KC, col0, ncols):
            for k0 in range(0, KC, 4):
                P.dma(wb[slot][:, k0:k0 + 4, 0:ncols],
                      Wd[k0 * 128:(k0 + 4) * 128, col0:col0 + ncols].rearrange("(k p) n -> p k n", p=128),
                      writes=["wb%d_%d" % (slot, k0)], key=("wb", slot), queue="pool")

        def norm_mod_T(st, src_rows, n_tiles, Atile_of, Stile_of, xT, tagp):
            xt = [sbt(st, tagp + "xt%d" % i, [128, D]) for i in range(2)]
            junk = sbt(st, tagp + "junk", [128, D])
            t1 = sbt(st, tagp + "t1", [128, D])
            ub = [sbt(st, tagp + "ub%d" % i, [128, D], BF16) for i in range(2)]
            ss = sbt(st, tagp + "ss", [128, 4])
            for t in range(n_tiles):
                sl = t % 2
                P.dma(xt[sl][:], src_rows(t), writes=[tagp + "xt%d" % sl], key=(tagp + "xt", sl))
                P.op("dve", lambda e: e.memset(ss[:, 0:1], 0.0), writes=[tagp + "ss0"])
                P.op("act", lambda e, sl=sl: e.activation(junk[:], xt[sl][:], AF.Square, accum_out=ss[:, 0:1]),
                     reads=[tagp + "xt%d" % sl, tagp + "ss0"], writes=[tagp + "junk", tagp + "ss0"])
                P.op("act", lambda e: e.activation(ss[:, 1:2], ss[:, 0:1], AF.Sqrt, bias=cst[:, 1:2], scale=1.0 / D),
                     reads=[tagp + "ss0"], writes=[tagp + "ss1"])
                P.op("dve", lambda e: e.reciprocal(ss[:, 2:3], ss[:, 1:2]), reads=[tagp + "ss1"], writes=[tagp + "ss2"])
                A, An = Atile_of(t)
                Sh, Sn = Stile_of(t)
                P.op("dve", lambda e, sl=sl, A=A: e.scalar_tensor_tensor(out=t1[:], in0=xt[sl][:], scalar=ss[:, 2:3], in1=A[:],
                                                                      op0=ALU.mult, op1=ALU.mult),
                     reads=[tagp + "xt%d" % sl, tagp + "ss2", An], writes=[tagp + "t1"])
                P.op("dve", lambda e, sl=sl, Sh=Sh: e.tensor_tensor(ub[sl][:], t1[:], Sh[:], ALU.add),
                     reads=[tagp + "t1", Sn], writes=[tagp + "ub%d" % sl])
                for half in range(2):
                    for j in range(8):
                        kc = half * 8 + j
                        P.op("pe", lambda e, sl=sl, kc=kc, half=half, j=j: e.transpose(
                            psb[half][:, j * 128:(j + 1) * 128], ub[sl][:, kc * 128:(kc + 1) * 128], identb[:]),
                            reads=[tagp + "ub%d" % sl, "identb"], writes=["psb%d" % half])
                    copy_op(evac_eng(), xT[:, half * 8:(half + 1) * 8, t * 128:(t + 1) * 128],
                            psb[half][:].rearrange("p (k t) -> p k t", k=8),
                            reads=["psb%d" % half], writes=["xT_" + tagp])

        def linear_T(xT, xTname, KC, wb, slot, ncols, tiles, consume):
            for t in tiles:
                b = rr["n"] % 4
                for kc in range(KC):
                    P.op("pe", lambda e, b=b, kc=kc, t=t: e.matmul(ps[b][:, 0:ncols], lhsT=xT[:, kc, t * 128:(t + 1) * 128],
                                                                  rhs=wb[slot][:, kc, 0:ncols], start=(kc == 0), stop=(kc == KC - 1)),
                         reads=[xTname] + ["wb%d_%d" % (slot, k0) for k0 in range(0, 16, 4)], writes=["ps%d" % b])
                consume(ps[b], "ps%d" % b, t)

        def linear_F(xT, xTname, KC, wb, slot, ncols, tokgroups, consume):
            for ch in range(ncols // 128):
                for (t0, nt) in tokgroups:
                    b = rr["n"] % 4
                    for kc in range(KC):
                        P.op("pe", lambda e, b=b, kc=kc, t0=t0, nt=nt, ch=ch: e.matmul(
                            ps[b][:, 0:nt], lhsT=wb[slot][:, kc, ch * 128:(ch + 1) * 128], rhs=xT[:, kc, t0:t0 + nt],
                            start=(kc == 0), stop=(kc == KC - 1)),
                            reads=[xTname] + ["wb%d_%d" % (slot, k0) for k0 in range(0, 16, 4)], writes=["ps%d" % b])
                    consume(ps[b], "ps%d" % b, ch, t0, nt)

        with ExitStack() as st:
            cs = sbt(st, "cs", [128, 2, 16])
            csb = sbt(st, "csb", [128, 2, 16, 128], BF16)
            adab = sbt(st, "adab", [128, 6 * D])
            g1 = sbt(st, "g1", [128, D])
            g2 = sbt(st, "g2", [128, D])
            aw = [sbt(st, "aw%d" % i, [128, 16, 512], BF16) for i in range(3)]
            res = [sbt(st, "ares%d" % i, [128, 512]) for i in range(4)]
            P.dma(cs[:, 0, :], c_fm, writes=["cs_a"], key="a0")
            P.dma(cs[:, 1, :], cc_fm, writes=["cs_b"], key="a0")
            P.dma(adab[:], ada_b.partition_broadcast(128), writes=["adab"])
            P.dma(g1[:], g1_d.partition_broadcast(128), writes=["g1"])
            P.dma(g2[:], g2_d.partition_broadcast(128), writes=["g2"])
            P.op("act", lambda e: e.activation(cs[:], cs[:], AF.Silu), reads=["cs_a", "cs_b"], writes=["cs"])
            P.op("dve", lambda e: e.tensor_copy(csb[:], cs[:].unsqueeze(3).to_broadcast([128, 2, 16, 128])),
                 reads=["cs"], writes=["csb"])
            ri = 0
            for j in range(6):
                for blk in range(4):
                    col0 = j * D + blk * 512
                    sl = (j * 4 + blk) % 3
                    for k0 in range(0, 16, 8):
                        P.dma(aw[sl][:, k0:k0 + 8, :],
                              ada_w[k0 * 128:(k0 + 8) * 128, col0:col0 + 512].rearrange("(k p) n -> p k n", p=128),
                              writes=["aw%d_%d" % (sl, k0)], key=("aw", sl), queue="pool")
                    for which in range(2 if j < 2 else 1):
                        b = ri % 4
                        r = res[ri % 4]
                        rn = "ares%d" % (ri % 4)
                        ri += 1
                        for kc in range(16):
                            P.op("pe", lambda e, b=b, kc=kc, sl=sl, which=which: e.matmul(
                                ps[b][:], lhsT=csb[:, which, kc, :], rhs=aw[sl][:, kc, :], start=(kc == 0), stop=(kc == 15)),
                                reads=["csb"] + ["aw%d_%d" % (sl, k0) for k0 in range(0, 16, 8)], writes=["ps%d" % b])
                        P.op("dve", lambda e, b=b, r=r, col0=col0: e.tensor_tensor(r[:], ps[b][:], adab[:, col0:col0 + 512], ALU.add),
                             reads=["ps%d" % b, "adab"], writes=[rn])
                        if j in (1, 4):
                            g = g1 if j == 1 else g2
                            P.op("dve", lambda e, r=r, g=g, blk=blk: e.scalar_tensor_tensor(
                                out=r[:], in0=r[:], scalar=1.0, in1=g[:, blk * 512:(blk + 1) * 512], op0=ALU.add, op1=ALU.mult),
                                reads=[rn, "g1", "g2"], writes=[rn])
                        if which == 0:
                            mi = {0: 1, 1: 0, 2: 2, 3: 4, 4: 3, 5: 5}[j]
                        else:
                            mi = {0: 7, 1: 6}[j]
                        P.dma(MOD[mi, :, blk * 512:(blk + 1) * 512], r[:], reads=[rn], writes=["MODd"], key="st")
        P.barrier()

        with ExitStack() as st:
            uT = sbt(st, "uT", [128, 16, NTOK], BF16)
            with ExitStack() as st1:
                A1 = sbt(st1, "A1", [128, D]); S1 = sbt(st1, "S1", [128, D])
                Ac = sbt(st1, "Ac", [128, D]); Sc = sbt(st1, "Sc", [128, D])
                P.dma(A1[:], MOD[0], writes=["A1"])
                P.dma(S1[:], MOD[1], writes=["S1"])
                P.dma(Ac[:], MOD[6], writes=["Ac"])
                P.dma(Sc[:], MOD[7], writes=["Sc"])
                norm_mod_T(st1,
                           lambda t: x_c[t * 128:(t + 1) * 128, :] if t < 16 else ctx_c[(t - 16) * 128:(t - 15) * 128, :],
                           NT_ALL,
                           lambda t: (A1, "A1") if t < 16 else (Ac, "Ac"),
                           lambda t: (S1, "S1") if t < 16 else (Sc, "Sc"),
                           uT, "n1")
            P.barrier()
            wb = [sbt(st, "wb%d" % i, [128, 16, 512], BF16) for i in range(2)]
            stg = [sbt(st, "stg%d" % i, [128, 512]) for i in range(4)]
            sti = {"n": 0}

            def store_T(dst_of):
                def consume(pst, psn, t):
                    i = sti["n"] % 4
                    sti["n"] += 1
                    dst = dst_of(t)
                    ncols = dst.shape[1]
                    copy_op(evac_eng(), stg[i][:, 0:ncols], pst[:, 0:ncols], reads=[psn], writes=["stg%d" % i])
                    P.dma(dst, stg[i][:, 0:ncols], reads=["stg%d" % i], key="st")
                return consume

            def store_F(dst_of):
                def consume(pst, psn, ch, t0, nt):
                    i = sti["n"] % 4
                    sti["n"] += 1
                    copy_op(evac_eng(), stg[i][:, 0:nt], pst[:, 0:nt], reads=[psn], writes=["stg%d" % i])
                    P.dma(dst_of(ch, t0, nt), stg[i][:, 0:nt], reads=["stg%d" % i], key="st")
                return consume

            ALLT = list(range(NT_ALL))
            OWNT = list(range(NT_OWN))
            KVT = list(range(9)) + [16, 17]
            ALLG = [(0, 512), (512, 512), (1024, 512), (1536, 512), (2048, 256)]
            OWNG = [(0, 512), (512, 512)]
            blocks = []
            blocks.append((w_in, C_K, 512, "T", KVT, store_T(lambda t: KV[t * 128:(t + 1) * 128, 0:512])))
            blocks.append((w_in, C_V, 512, "T", KVT, store_T(lambda t: KV[t * 128:(t + 1) * 128, 512:1024])))
            blocks.append((w_dt, 0, 128, "T", ALLT, store_T(lambda t: DTs[t * 128:(t + 1) * 128, :])))
            for bi in range(12):
                blocks.append((w_in, C_XBC + bi * 512, 512, "F", ALLG,
                               store_F(lambda ch, t0, nt, bi=bi: XBCT[bi * 512 + ch * 128: bi * 512 + (ch + 1) * 128, t0:t0 + nt])))
            for bi in range(4):
                blocks.append((w_in, C_Q + bi * 512, 512, "T", OWNT,
                               store_T(lambda t, bi=bi: Qs[t * 128:(t + 1) * 128, bi * 512:(bi + 1) * 512])))
            for bi in range(8):
                blocks.append((w_in, C_Z + bi * 512, 512, "T", OWNT,
                               store_T(lambda t, bi=bi: Zs[t * 128:(t + 1) * 128, bi * 512:(bi + 1) * 512])))
            for bi in range(8):
                blocks.append((w_in, C_G + bi * 512, 512, "F", OWNG,
                               store_F(lambda ch, t0, nt, bi=bi: GTs[bi * 512 + ch * 128: bi * 512 + (ch + 1) * 128, t0:t0 + nt])))
            for i, (Wd, col0, ncols, orient, toks, cons) in enumerate(blocks):
                sl = i % 2
                load_w_block(wb, sl, Wd, 16, col0, ncols)
                if orient == "T":
                    linear_T(uT, "xT_n1", 16, wb, sl, ncols, toks, cons)
                else:
                    linear_F(uT, "xT_n1", 16, wb, sl, ncols, toks, cons)
        P.barrier()

        with ExitStack() as st:
            kT = sbt(st, "kT", [128, 4, 11 * 128], BF16)
            Vt = sbt(st, "Vt", [128, 11, 512], BF16)
            qT = sbt(st, "qT", [128, 16, 1024], BF16)
            aT = sbt(st, "aT", [128, 16, 1024], BF16)
            esink = sbt(st, "esink", [128, 16])
            cosb = [sbt(st, "cosb%d" % i, [128, 2048]) for i in range(2)]
            sinb = [sbt(st, "sinb%d" % i, [128, 2048]) for i in range(2)]
            kin = [sbt(st, "kin%d" % i, [128, 1024]) for i in range(2)]
            qin = [sbt(st, "qin%d" % i, [128, 2048]) for i in range(2)]
            r1 = sbt(st, "r1", [128, 2048]); r2 = sbt(st, "r2", [128, 2048])
            rb = [sbt(st, "rb%d" % i, [128, 2048], BF16) for i in range(2)]
            P.dma(esink[:], sink_d.partition_broadcast(128), writes=["esink"])
            P.op("act", lambda e: e.activation(esink[:], esink[:], AF.Exp), reads=["esink"], writes=["esink"])

            def rope(src, nh, sl, out_bf, rn_src, rn_out):
                n = nh * 128
                v = lambda ap: ap.rearrange("p (g b c) -> p g b c", g=nh * 2, b=2, c=32)
                P.op("dve", lambda e: e.tensor_tensor(r1[:, 0:n], src, cosb[sl][:, 0:n], ALU.mult),
                     reads=[rn_src, "cosb%d" % sl], writes=["r1"])
                P.op("dve", lambda e: e.tensor_tensor(v(r2[:, 0:n])[:, :, 0, :], v(src)[:, :, 1, :], v(sinb[sl][:, 0:n])[:, :, 0, :], ALU.mult),
                     reads=[rn_src, "sinb%d" % sl], writes=["r2a"])
                P.op("dve", lambda e: e.tensor_tensor(v(r2[:, 0:n])[:, :, 1, :], v(src)[:, :, 0, :], v(sinb[sl][:, 0:n])[:, :, 1, :], ALU.mult),
                     reads=[rn_src, "sinb%d" % sl], writes=["r2b"])
                P.op("dve", lambda e: e.tensor_tensor(out_bf, r1[:, 0:n], r2[:, 0:n], ALU.add),
                     reads=["r1", "r2a", "r2b"], writes=[rn_out])

            for blk in range(11):
                sl = blk % 2
                row0 = blk * 128 if blk < 9 else S + (blk - 9) * 128
                P.dma(kin[sl][:], KV[row0:row0 + 128, :], writes=["kin%d" % sl], key=("kin", sl))
                copy_op("act", Vt[:, blk, :], kin[sl][:, 512:1024], reads=["kin%d" % sl], writes=["Vt"])
                if blk < 9:
                    P.dma(cosb[sl][:], cos_d[blk * 128:(blk + 1) * 128, :], writes=["cosb%d" % sl], key=None)
                    P.dma(sinb[sl][:], sin_d[blk * 128:(blk + 1) * 128, :], writes=["sinb%d" % sl], key=None)
                    rope(kin[sl][:, 0:512], 4, sl, rb[sl][:, 0:512], "kin%d" % sl, "rb%d" % sl)
                else:
                    copy_op("dve", rb[sl][:, 0:512], kin[sl][:, 0:512], reads=["kin%d" % sl], writes=["rb%d" % sl])
                for g in range(4):
                    P.op("pe", lambda e, sl=sl, g=g: e.transpose(psb[0][:, g * 128:(g + 1) * 128], rb[sl][:, g * 128:(g + 1) * 128], identb[:]),
                         reads=["rb%d" % sl, "identb"], writes=["psb0"])
                copy_op(evac_eng(), kT[:, :, blk * 128:(blk + 1) * 128], psb[0][:, 0:512].rearrange("p (g t) -> p g t", g=4),
                        reads=["psb0"], writes=["kT"])
            for t in range(8):
                sl = t % 2
                P.dma(qin[sl][:], Qs[t * 128:(t + 1) * 128, :], writes=["qin%d" % sl], key=("qin", sl))
                P.dma(cosb[sl][:], cos_d[t * 128:(t + 1) * 128, :], writes=["cosb%d" % sl], key=None)
                P.dma(sinb[sl][:], sin_d[t * 128:(t + 1) * 128, :], writes=["sinb%d" % sl], key=None)
                rope(qin[sl][:], 16, sl, rb[sl][:], "qin%d" % sl, "rb%d" % sl)
                for half in range(2):
                    for j in range(8):
                        hh = half * 8 + j
                        P.op("pe", lambda e, sl=sl, hh=hh, half=half, j=j: e.transpose(
                            psb[half][:, j * 128:(j + 1) * 128], rb[sl][:, hh * 128:(hh + 1) * 128], identb[:]),
                            reads=["rb%d" % sl, "identb"], writes=["psb%d" % half])
                    copy_op(evac_eng(), qT[:, half * 8:(half + 1) * 8, t * 128:(t + 1) * 128],
                            psb[half][:].rearrange("p (k t) -> p k t", k=8), reads=["psb%d" % half], writes=["qT"])
            pT = [sbt(st, "pT%d" % i, [128, 512], BF16) for i in range(4)]
            den = [sbt(st, "den%d" % i, [128, 512]) for i in range(2)]
            units = []
            for qb in range(8):
                for g in range(4):
                    kbs = []
                    if qb >= 1:
                        kbs.append((qb - 1, triBb, "triBb"))
                    kbs.append((qb, None, None))
                    kbs.append((qb + 1, triFb, "triFb"))
                    kbs.append((9, None, None))
                    kbs.append((10, None, None))
                    for ki, (kb, mask, mname) in enumerate(kbs):
                        units.append((qb, g, ki, len(kbs), kb, mask, mname))

            def stageS(u):
                qb, g, ki, n, kb, mask, mname = units[u]
                sb_ = u % 2
                pp = u % 4
                P.op("pe", lambda e: e.matmul(ps[sb_][:], lhsT=kT[:, g, kb * 128:(kb + 1) * 128],
                                              rhs=qT[:, 4 * g:4 * g + 4, qb * 128:(qb + 1) * 128], start=True, stop=True),
                     reads=["kT", "qT"], writes=["ps%d" % sb_])
                P.op("act", lambda e: e.activation(pT[pp][:], ps[sb_][:], AF.Exp, scale=SCALE),
                     reads=["ps%d" % sb_], writes=["pT%d" % pp])
                if mask is not None:
                    P.op("dve", lambda e: e.tensor_tensor(
                        pT[pp][:].rearrange("p (r q) -> p r q", r=4), pT[pp][:].rearrange("p (r q) -> p r q", r=4),
                        mask[:].unsqueeze(1).to_broadcast([128, 4, 128]), ALU.mult),
                        reads=["pT%d" % pp, mname], writes=["pT%d" % pp])

            def stageV(u):
                qb, g, ki, n, kb, mask, mname = units[u]
                pp = u % 4
                par = (qb * 4 + g) % 2
                pa, pb = (2, 3) if par == 0 else (4, 5)
                P.op("pe", lambda e: e.matmul(ps[pa][:], lhsT=Vt[:, kb, g * 128:(g + 1) * 128], rhs=pT[pp][:],
                                              start=(ki == 0), stop=(ki == n - 1)),
                     reads=["Vt", "pT%d" % pp], writes=["ps%d" % pa])
                P.op("pe", lambda e: e.matmul(ps[pb][:], lhsT=onesb[:], rhs=pT[pp][:], start=(ki == 0), stop=(ki == n - 1)),
                     reads=["onesb", "pT%d" % pp], writes=["ps%d" % pb])
                if ki == n - 1:
                    dn = den[par]
                    P.op("dve", lambda e: e.tensor_tensor(dn[:].rearrange("p (r q) -> p r q", r=4),
                                                          ps[pb][:].rearrange("p (r q) -> p r q", r=4),
                                                          esink[:, 4 * g:4 * g + 4].unsqueeze(2).to_broadcast([128, 4, 128]), ALU.add),
                         reads=["ps%d" % pb, "esink"], writes=["den%d" % par])
                    P.op("dve", lambda e: e.reciprocal(dn[:], dn[:]), reads=["den%d" % par], writes=["den%d" % par])
                    P.op("dve", lambda e: e.tensor_tensor(aT[:, 4 * g:4 * g + 4, qb * 128:(qb + 1) * 128],
                                                          ps[pa][:].rearrange("p (r q) -> p r q", r=4),
                                                          dn[:].rearrange("p (r q) -> p r q", r=4), ALU.mult),
                         reads=["ps%d" % pa, "den%d" % par], writes=["aT"])

            for i in range(len(units) + 2):
                if i < len(units):
                    stageS(i)
                if i >= 2:
                    stageV(i - 2)
            P.dma(ATT, aT[:], reads=["aT"], key="st")
        P.barrier()

        with ExitStack() as st:
            cw = sbt(st, "cw", [128, 48, 5]); cb = sbt(st, "cb", [128, 48])
            P.dma(cw[:], convw, writes=["cw"])
            P.dma(cb[:], convb, writes=["cb"])
            cin = [sbt(st, "cin%d" % i, [128, S + 4 + CTXL + 4], BF16) for i in range(2)]
            dg = [sbt(st, "dg%d" % i, [128, 5, 128], BF16) for i in range(2)]
            cout = sbt(st, "cout", [128, 8, NTOK], BF16)
            tstg = [sbt(st, "tstg%d" % i, [128, 1024], BF16) for i in range(2)]
            for i in range(2):
                P.op("pool", lambda e, i=i: e.memset(cin[i][:], 0.0), writes=["cin%d" % i])
            CG = [(0, 0, 512), (512, 512, 512), (1024, 1024, 512), (1536, 1536, 512), (S, S + 4, CTXL)]
            pc = 0
            for grp in range(6):
                for c8 in range(8):
                    cc = grp * 8 + c8
                    sl = cc % 2
                    P.dma(cin[sl][:, 2:2 + S], XBCT[cc * 128:(cc + 1) * 128, 0:S], writes=["cin%d_a" % sl], key=("cin", sl), queue="pool")
                    P.dma(cin[sl][:, S + 6:S + 6 + CTXL], XBCT[cc * 128:(cc + 1) * 128, S:NTOK], writes=["cin%d_b" % sl], key=("cin", sl), queue="pool")
                    for j in range(5):
                        P.op("dve", lambda e, sl=sl, cc=cc, j=j: e.tensor_scalar(dg[sl][:, j, :], identb[:], cw[:, cc, j:j + 1], None, op0=ALU.mult),
                             reads=["identb", "cw"], writes=["dg%d" % sl])
                    for (o0, i0, L) in CG:
                        bnk = pc % 4
                        pc += 1
                        for j in range(5):
                            P.op("pe", lambda e, sl=sl, j=j, i0=i0, L=L, bnk=bnk: e.matmul(
                                ps[bnk][:, 0:L], lhsT=dg[sl][:, j, :], rhs=cin[sl][:, i0 + j:i0 + j + L], start=(j == 0), stop=(j == 4)),
                                reads=["dg%d" % sl, "cin%d" % sl, "cin%d_a" % sl, "cin%d_b" % sl], writes=["ps%d" % bnk])
                        P.op("act", lambda e, cc=cc, c8=c8, o0=o0, L=L, bnk=bnk: e.activation(
                            cout[:, c8, o0:o0 + L], ps[bnk][:, 0:L], AF.Silu, bias=cb[:, cc:cc + 1]),
                            reads=["ps%d" % bnk, "cb"], writes=["cout"])
                if grp < 5:
                    for t in range(NT_ALL):
                        hb = t % 2
                        for c8 in range(8):
                            P.op("pe", lambda e, hb=hb, c8=c8, t=t: e.transpose(
                                psb[hb][:, c8 * 128:(c8 + 1) * 128], cout[:, c8, t * 128:(t + 1) * 128], identb[:]),
                                reads=["cout", "identb"], writes=["psb%d" % hb])
                        copy_op(evac_eng(), tstg[hb][:], psb[hb][:], reads=["psb%d" % hb], writes=["tstg%d" % hb])
                        P.dma(XTM[t * 128:(t + 1) * 128, grp * 1024:(grp + 1) * 1024], tstg[hb][:], reads=["tstg%d" % hb], key="st")
                if grp >= 4:
                    P.dma(BCT[(grp - 4) * 1024:(grp - 3) * 1024, :].rearrange("(c p) t -> p c t", p=128), cout[:], reads=["cout"], key="st2")
        P.barrier()

        with ExitStack() as st:
            hst = [sbt(st, "hst%d" % i, [128, DI]) for i in range(2)]
            hbf = sbt(st, "hbf", [128, DI], BF16)
            xtm = [sbt(st, "xtm%d" % i, [128, 5120], BF16) for i in range(2)]
            bct = [sbt(st, "bct%d" % i, [128, 16, 128], BF16) for i in range(2)]
            dtr = [sbt(st, "dtr%d" % i, [128, 128]) for i in range(2)]
            dtbb = sbt(st, "dtbb", [128, 128]); Ab = sbt(st, "Ab", [128, 128]); Dd = sbt(st, "Dd", [128, 64])
            dtL = [sbt(st, "dt%d" % i, [128, 64]) for i in range(2)]; aaL = [sbt(st, "aa%d" % i, [128, 64]) for i in range(2)]
            acsL = [sbt(st, "acs%d" % i, [128, 64]) for i in range(2)]; atotL = [sbt(st, "atot%d" % i, [128, 64]) for i in range(2)]
            wendL = [sbt(st, "wend%d" % i, [128, 64]) for i in range(2)]; eacsL = [sbt(st, "eacs%d" % i, [128, 64]) for i in range(2)]
            etotL = [sbt(st, "etot%d" % i, [128, 64]) for i in range(2)]
            wxL = [sbt(st, "wx%d" % i, [128, 128]) for i in range(2)]; wxeL = [sbt(st, "wxe%d" % i, [128, DI], BF16) for i in range(2)]
            TA = sbt(st, "TA", [128, 8, 128]); seg = sbt(st, "seg", [128, 8, 128])
            Mt2 = [sbt(st, "Mt%d" % i, [128, 8, 128], BF16) for i in range(2)]; CBa = sbt(st, "CBa", [128, 8, 128]); segL = [sbt(st, "segd%d" % i, [128, 8, 128]) for i in range(2)]
            yt = sbt(st, "yt", [128, DI]); ytmp = sbt(st, "ytmp", [128, 512])
            P.dma(dtbb[:], dtb_d.partition_broadcast(128), writes=["dtbb"])
            P.dma(Ab[:], alog_d.partition_broadcast(128), writes=["Ab"])
            P.dma(Dd[:], ssmd_d.partition_broadcast(128), writes=["Dd"])
            P.op("act", lambda e: e.activation(Ab[:], Ab[:], AF.Exp), reads=["Ab"], writes=["Ab"])
            P.op("dve", lambda e: e.tensor_scalar(Ab[:], Ab[:], -1.0, None, op0=ALU.mult), reads=["Ab"], writes=["Ab"])
            for i in range(2):
                P.op("pool", lambda e, i=i: e.memset(hst[i][:], 0.0), writes=["hst%d" % i])
            DIb = sbt(st, "DIb", [128, 64, 128], BF16)
            P.op("dve", lambda e: e.tensor_tensor(DIb[:], ident[:].unsqueeze(1).to_broadcast([128, 64, 128]),
                                                  Dd[:].unsqueeze(2).to_broadcast([128, 64, 128]), ALU.mult),
                 reads=["ident", "Dd"], writes=["DIb"])
            ldi = {"n": 0}

            def setup(tile, d, full, sl):
                tri = triF if d == 0 else triB
                trin = "triF" if d == 0 else "triB"
                h = hst[d]; hn = "hst%d" % d
                dt = dtL[sl]
                aa = aaL[sl]
                acs = acsL[sl]
                atot = atotL[sl]
                wend = wendL[sl]
                eacs = eacsL[sl]
                etot = etotL[sl]
                wx = wxL[sl]
                wxe = wxeL[sl]
                P.dma(xtm[sl][:], XTM[tile * 128:(tile + 1) * 128, :], writes=["xtm%d" % sl], key=None)
                yield
                P.dma(dtr[sl][:], DTs[tile * 128:(tile + 1) * 128, :], writes=["dtr%d" % sl], key=None)
                yield
                if full:
                    P.dma(bct[sl][:], BCT[:, tile * 128:(tile + 1) * 128].rearrange("(c p) t -> p c t", p=128),
                          writes=["bct%d" % sl], key=None)
                    yield
                P.op("dve", lambda e: e.tensor_tensor(dt[:], dtr[sl][:, d * 64:(d + 1) * 64], dtbb[:, d * 64:(d + 1) * 64], ALU.add),
                     reads=["dtr%d" % sl, "dtbb"], writes=["dt%d" % sl])
                yield
                P.op("act", lambda e: e.activation(dt[:], dt[:], AF.Exp), reads=["dt%d" % sl], writes=["dt%d" % sl])
                yield
                P.op("act", lambda e: e.activation(dt[:], dt[:], AF.Ln, bias=cst[:, 0:1]), reads=["dt%d" % sl, "cst0"], writes=["dt%d" % sl])
                yield
                P.op("dve", lambda e: e.tensor_tensor(aa[:], dt[:], Ab[:, d * 64:(d + 1) * 64], ALU.mult), reads=["dt%d" % sl, "Ab"], writes=["aa%d" % sl])
                yield
                P.op("pe", lambda e: e.matmul(ps[4][:, 0:64], lhsT=tri[:], rhs=aa[:], start=True, stop=True),
                     reads=[trin, "aa%d" % sl], writes=["ps4"])
                yield
                P.op("pe", lambda e: e.matmul(ps[4][:, 64:128], lhsT=ones[:], rhs=aa[:], start=True, stop=True),
                     reads=["ones", "aa%d" % sl], writes=["ps4"])
                yield
                P.op("dve", lambda e: e.tensor_copy(acs[:], ps[4][:, 0:64]), reads=["ps4"], writes=["acs%d" % sl])
                yield
                P.op("dve", lambda e: e.tensor_copy(atot[:], ps[4][:, 64:128]), reads=["ps4"], writes=["atot%d" % sl])
                yield
                P.op("dve", lambda e: e.tensor_tensor(wend[:], atot[:], acs[:], ALU.subtract), reads=["atot%d" % sl, "acs%d" % sl], writes=["wend%d" % sl])
                yield
                P.op("act", lambda e: e.activation(wend[:], wend[:], AF.Exp), reads=["wend%d" % sl], writes=["wend%d" % sl])
                yield
                P.op("act", lambda e: e.activation(etot[:], atot[:], AF.Exp), reads=["atot%d" % sl], writes=["etot%d" % sl])
                yield
                v3 = lambda ap: ap.rearrange("p (h q) -> p h q", h=64)
                bc = lambda ap: ap.unsqueeze(2).to_broadcast([128, 64, 64])
                P.op("dve", lambda e: e.tensor_tensor(wend[:], wend[:], dt[:], ALU.mult), reads=["wend%d" % sl, "dt%d" % sl], writes=["wend%d" % sl])
                yield
                P.op("dve", lambda e: e.tensor_tensor(v3(wxe[:]), v3(xtm[sl][:, 0:DI]), bc(wend[:]), ALU.mult),
                     reads=["xtm%d" % sl, "wend%d" % sl], writes=["wxe%d" % sl])
                yield
                if full:
                    P.op("act", lambda e: e.activation(wx[:, 0:64], dt[:], AF.Ln), reads=["dt%d" % sl], writes=["lndt%d" % sl])
                    yield
                    P.op("dve", lambda e: e.tensor_tensor(wx[:, 64:128], acs[:], wx[:, 0:64], ALU.subtract),
                         reads=["acs%d" % sl, "lndt%d" % sl], writes=["acs2%d" % sl])
                    yield
                if full:
                    P.op("act", lambda e: e.activation(eacs[:], acs[:], AF.Exp), reads=["acs%d" % sl], writes=["eacs%d" % sl])
                    yield

            def body(tile, d, full, sl, pump):
                tri = triF if d == 0 else triB
                trin = "triF" if d == 0 else "triB"
                h = hst[d]; hn = "hst%d" % d
                dt = dtL[sl]
                aa = aaL[sl]
                acs = acsL[sl]
                atot = atotL[sl]
                wend = wendL[sl]
                eacs = eacsL[sl]
                etot = etotL[sl]
                wx = wxL[sl]
                wxe = wxeL[sl]
                v3 = lambda ap: ap.rearrange("p (h q) -> p h q", h=64)
                bc = lambda ap: ap.unsqueeze(2).to_broadcast([128, 64, 64])
                if full:
                    P.op("act", lambda e: e.copy(hbf[:], h[:]), reads=[hn], writes=["hbf"])
                    for hf in range(2):
                        for j in range(4):
                            g = 4 * hf + j
                            P.op("pe", lambda e, g=g, j=j: e.matmul(ps[5][:, j * 128:(j + 1) * 128], lhsT=bct[sl][:, g, :], rhs=bct[sl][:, 8 + g, :],
                                                                    start=True, stop=True), reads=["bct%d" % sl], writes=["ps5"])
                        P.op("dve", lambda e, hf=hf: e.tensor_tensor(CBa[:, 4 * hf:4 * hf + 4, :], ps[5][:].rearrange("p (j l) -> p j l", j=4),
                                                                     tri[:].unsqueeze(1).to_broadcast([128, 4, 128]), ALU.mult),
                             reads=["ps5", trin], writes=["CBa%d" % hf])

                    def s_TA(g):
                        for r in range(8):
                            hh = 8 * g + r
                            P.op("pe", lambda e, r=r, hh=hh: e.matmul(ps[r // 4][:, (r % 4) * 128:(r % 4 + 1) * 128],
                                                                       lhsT=aa[:, hh:hh + 1].to_broadcast([128, 128]), rhs=tri[:],
                                                                       start=True, stop=True),
                                 reads=[trin, "aa%d" % sl], writes=["ps%d" % (r // 4)])

                    def s_seg(g):
                        m = g % 2
                        for hf in range(2):
                            P.op("dve", lambda e, hf=hf: e.tensor_tensor(
                                segL[m][:, 4 * hf:4 * hf + 4, :], ps[hf][:].rearrange("p (r l) -> p r l", r=4),
                                wx[:, 64 + 8 * g + 4 * hf:64 + 8 * g + 4 * hf + 4].unsqueeze(2).to_broadcast([128, 4, 128]), ALU.subtract),
                                reads=["ps%d" % hf, "acs2%d" % sl], writes=["seg%d_%d" % (m, hf)])
                        P.op("act", lambda e: e.activation(segL[m][:], segL[m][:], AF.Exp),
                             reads=["seg%d_0" % m, "seg%d_1" % m], writes=["seg%d_0" % m, "seg%d_1" % m])

                    def s_M(g):
                        m = g % 2
                        P.op("dve", lambda e: e.scalar_tensor_tensor(out=Mt2[m][:], in0=segL[m][:], scalar=1e30,
                                                                     in1=CBa[:, g, :].unsqueeze(1).to_broadcast([128, 8, 128]),
                                                                     op0=ALU.min, op1=ALU.mult),
                             reads=["seg%d_0" % m, "seg%d_1" % m, "CBa%d" % (g // 4)], writes=["Mt%d" % m])
                        for r in range(8):
                            hh = 8 * g + r
                            P.op("pe", lambda e, r=r, hh=hh: e.matmul(ps[2][:, r * 64:(r + 1) * 64], lhsT=Mt2[m][:, r, :],
                                                                       rhs=xtm[sl][:, hh * 64:(hh + 1) * 64], start=True, stop=(d != 0)),
                                 reads=["Mt%d" % m, "xtm%d" % sl], writes=["ps2"])
                            if d == 0:
                                P.op("pe", lambda e, r=r, hh=hh: e.matmul(ps[2][:, r * 64:(r + 1) * 64], lhsT=DIb[:, hh, :],
                                                                           rhs=xtm[sl][:, hh * 64:(hh + 1) * 64], start=False, stop=True),
                                     reads=["DIb", "xtm%d" % sl], writes=["ps2"])
                        P.op("pe", lambda e: e.matmul(ps[3][:], lhsT=bct[sl][:, 8 + g, :], rhs=hbf[:, g * 512:(g + 1) * 512],
                                                      start=True, stop=True), reads=["bct%d" % sl, "hbf"], writes=["ps3"])

                    def s_Y(g):
                        P.op("dve", lambda e: e.tensor_tensor(ytmp[:].rearrange("p (r q) -> p r q", r=8),
                                                              ps[3][:].rearrange("p (r q) -> p r q", r=8),
                                                              eacs[:, 8 * g:8 * g + 8].unsqueeze(2).to_broadcast([128, 8, 64]), ALU.mult),
                             reads=["ps3", "eacs%d" % sl], writes=["ytmp"])
                        P.op("dve", lambda e: e.tensor_tensor(yt[:, g * 512:(g + 1) * 512], ps[2][:], ytmp[:], ALU.add),
                             reads=["ps2", "ytmp"], writes=["yt"])

                    for k in range(-2, 9):
                        if 0 <= k - 1 < 8:
                            s_Y(k - 1)
                        if 0 <= k + 1 < 8:
                            s_seg(k + 1)
                        if 0 <= k + 2 < 8:
                            s_TA(k + 2)
                        pump(1)
                        if 0 <= k < 8:
                            s_M(k)
                        pump(1)
                    P.dma((Y1 if d == 0 else Y2)[tile * 128:(tile + 1) * 128, :], yt[:], reads=["yt"], key="st")
                for half in range(2):
                    pump(4)
                    for g4 in range(4):
                        g = half * 4 + g4
                        P.op("pe", lambda e, g=g, g4=g4: e.matmul(ps[g4][:], lhsT=xtm[sl][:, DI + g * 128:DI + (g + 1) * 128],
                                                                  rhs=wxe[:, g * 512:(g + 1) * 512], start=True, stop=True),
                             reads=["xtm%d" % sl, "wxe%d" % sl], writes=["ps%d" % g4])
                    for g4 in range(4):
                        g = half * 4 + g4
                        eng = "dve" if g4 % 2 == 0 else "pool"
                        P.op("dve", lambda e, g=g: e.tensor_tensor(h[:, g * 512:(g + 1) * 512].rearrange("p (r q) -> p r q", r=8),
                                                                   h[:, g * 512:(g + 1) * 512].rearrange("p (r q) -> p r q", r=8),
                                                                   etot[:, 8 * g:8 * g + 8].unsqueeze(2).to_broadcast([128, 8, 64]), ALU.mult),
                             reads=[hn, "etot%d" % sl, "hbf"], writes=[hn])
                        P.op("dve", lambda e, g=g, g4=g4: e.tensor_tensor(h[:, g * 512:(g + 1) * 512], h[:, g * 512:(g + 1) * 512],
                                                                          ps[g4][:], ALU.add),
                             reads=[hn, "ps%d" % g4], writes=[hn])

            seq = ([(16, 0, False), (17, 0, False)] + [(t, 0, True) for t in range(8)] + [(17, 1, False), (16, 1, False)]
                   + [(t, 1, False) for t in range(15, 7, -1)] + [(t, 1, True) for t in range(7, -1, -1)])
            gens = [setup(sq[0], sq[1], sq[2], i % 2) for i, sq in enumerate(seq)]
            for _ in gens[0]:
                pass
            for i, sq in enumerate(seq):
                nxt = gens[i + 1] if i + 1 < len(seq) else None

                def pump(k, nxt=nxt):
                    if nxt is not None:
                        for _ in range(k):
                            next(nxt, None)
                body(sq[0], sq[1], sq[2], i % 2, pump)
                if nxt is not None:
                    for _ in nxt:
                        pass
        P.barrier()

        with ExitStack() as st:
            mT = sbt(st, "mT", [128, 16, 1024], BF16)
            with ExitStack() as st5:
                ynT = sbt(st5, "ynT", [128, 32, 1024], BF16)
                with ExitStack() as st5a:
                    y1L = [sbt(st5a, "y1%d" % i, [128, DI]) for i in range(2)]; y2L = [sbt(st5a, "y2%d" % i, [128, DI]) for i in range(2)]
                    zz = sbt(st5a, "zz", [128, DI])
                    gN = sbt(st5a, "gN", [128, DI]); ynb = sbt(st5a, "ynb", [128, DI], BF16)
                    ss5 = sbt(st5a, "ss5", [128, 4])
                    P.dma(gN[:], gN_d.partition_broadcast(128), writes=["gN"])
                    for t in range(8):
                        y1 = y1L[t % 2]; y2 = y2L[t % 2]; y1n = "y1%d" % (t % 2); y2n = "y2%d" % (t % 2)
                        P.dma(y1[:], Y1[t * 128:(t + 1) * 128, :], writes=[y1n])
                        P.dma(y2[:], Y2[t * 128:(t + 1) * 128, :], writes=[y2n])
                        P.dma(zz[:], Zs[t * 128:(t + 1) * 128, :], writes=["zz"])
                        P.op("dve", lambda e, y1=y1, y2=y2: e.tensor_tensor(y1[:], y1[:], y2[:], ALU.add), reads=[y1n, y2n], writes=[y1n])
                        P.op("act", lambda e: e.activation(zz[:], zz[:], AF.Silu), reads=["zz"], writes=["zz"])
                        P.op("dve", lambda e, y1=y1: e.tensor_tensor(y1[:], y1[:], zz[:], ALU.mult), reads=[y1n, "zz"], writes=[y1n])
                        P.op("dve", lambda e: e.memset(ss5[:, 0:1], 0.0), writes=["ss5a"])
                        P.op("act", lambda e, y1=y1, y2=y2: e.activation(y2[:], y1[:], AF.Square, accum_out=ss5[:, 0:1]),
                             reads=[y1n, "ss5a"], writes=[y2n, "ss5a"])
                        P.op("act", lambda e: e.activation(ss5[:, 1:2], ss5[:, 0:1], AF.Sqrt, bias=cst[:, 1:2], scale=1.0 / DI),
                             reads=["ss5a"], writes=["ss5b"])
                        P.op("dve", lambda e: e.reciprocal(ss5[:, 2:3], ss5[:, 1:2]), reads=["ss5b"], writes=["ss5c"])
                        P.op("dve", lambda e, y1=y1: e.scalar_tensor_tensor(out=ynb[:], in0=y1[:], scalar=ss5[:, 2:3], in1=gN[:],
                                                                     op0=ALU.mult, op1=ALU.mult),
                             reads=[y1n, "ss5c", "gN"], writes=["ynb"])
                        for q in range(4):
                            hb = q % 2
                            for j in range(8):
                                kc = q * 8 + j
                                P.op("pe", lambda e, hb=hb, j=j, kc=kc: e.transpose(
                                    psb[hb][:, j * 128:(j + 1) * 128], ynb[:, kc * 128:(kc + 1) * 128], identb[:]),
                                    reads=["ynb", "identb"], writes=["psb%d" % hb])
                            copy_op(evac_eng(), ynT[:, q * 8:(q + 1) * 8, t * 128:(t + 1) * 128],
                                    psb[hb][:].rearrange("p (k t) -> p k t", k=8), reads=["psb%d" % hb], writes=["ynT"])
                P.barrier()
                aTs = sbt(st5, "aTs", [128, 16, 1024], BF16)
                P.dma(aTs[:], ATT, writes=["aTs"])
                wbsL = [sbt(st5, "wbs%d" % i, [128, 32, 256], BF16) for i in range(2)]
                wbaL = [sbt(st5, "wba%d" % i, [128, 16, 256], BF16) for i in range(2)]
                gsL = [sbt(st5, "gs%d" % i, [128, 512]) for i in range(2)]; gaL = [sbt(st5, "ga%d" % i, [128, 512]) for i in range(2)]
                m1L = [sbt(st5, "m1%d" % i, [128, 512]) for i in range(2)]; m2L = [sbt(st5, "m2%d" % i, [128, 512]) for i in range(2)]
                for blk in range(8):
                    wbs = wbsL[blk % 2]; wba = wbaL[blk % 2]
                    wbsr = ["wbs%d_%d" % (blk % 2, k0) for k0 in range(0, 32, 4)]
                    wbar = ["wba%d_%d" % (blk % 2, k0) for k0 in range(0, 16, 4)]
                    for k0 in range(0, 32, 4):
                        P.dma(wbs[:, k0:k0 + 4, :], w_bs[k0 * 128:(k0 + 4) * 128, blk * 256:(blk + 1) * 256].rearrange("(k p) n -> p k n", p=128),
                              writes=["wbs%d_%d" % (blk % 2, k0)], key=("wbs", blk % 2), queue="pool")
                    for k0 in range(0, 16, 4):
                        P.dma(wba[:, k0:k0 + 4, :], w_ba[k0 * 128:(k0 + 4) * 128, blk * 256:(blk + 1) * 256].rearrange("(k p) n -> p k n", p=128),
                              writes=["wba%d_%d" % (blk % 2, k0)], key=("wba", blk % 2), queue="pool")
                    for ch in range(2):
                        cabs = blk * 2 + ch
                        for th in range(2):
                            t0 = th * 512
                            u5 = (cabs * 2 + th) % 2
                            gsu, gau, m1u, m2u = gsL[u5], gaL[u5], m1L[u5], m2L[u5]
                            pA, pB = 2 * u5, 2 * u5 + 1
                            P.dma(gsu[:], GTs[cabs * 128:(cabs + 1) * 128, t0:t0 + 512], writes=["gs%d" % u5])
                            P.dma(gau[:], GTs[2048 + cabs * 128:2048 + (cabs + 1) * 128, t0:t0 + 512], writes=["ga%d" % u5])
                            P.op("act", lambda e, gsu=gsu: e.activation(gsu[:], gsu[:], AF.Sigmoid), reads=["gs%d" % u5], writes=["gs%d" % u5])
                            P.op("act", lambda e, gau=gau: e.activation(gau[:], gau[:], AF.Sigmoid), reads=["ga%d" % u5], writes=["ga%d" % u5])
                            for kc in range(32):
                                P.op("pe", lambda e, kc=kc, ch=ch, t0=t0, wbs=wbs, pA=pA: e.matmul(ps[pA][:], lhsT=wbs[:, kc, ch * 128:(ch + 1) * 128],
                                                                                  rhs=ynT[:, kc, t0:t0 + 512], start=(kc == 0), stop=(kc == 31)),
                                     reads=wbsr + ["ynT"], writes=["ps%d" % pA])
                            for kc in range(16):
                                P.op("pe", lambda e, kc=kc, ch=ch, t0=t0, wba=wba, pB=pB: e.matmul(ps[pB][:], lhsT=wba[:, kc, ch * 128:(ch + 1) * 128],
                                                                                  rhs=aTs[:, kc, t0:t0 + 512], start=(kc == 0), stop=(kc == 15)),
                                     reads=wbar + ["aTs"], writes=["ps%d" % pB])
                            P.op("dve", lambda e, m1u=m1u, gsu=gsu, pA=pA: e.tensor_tensor(m1u[:], ps[pA][:], gsu[:], ALU.mult),
                                 reads=["ps%d" % pA, "gs%d" % u5], writes=["m1%d" % u5])
                            P.op("dve", lambda e, m2u=m2u, gau=gau, pB=pB: e.tensor_tensor(m2u[:], ps[pB][:], gau[:], ALU.mult),
                                 reads=["ps%d" % pB, "ga%d" % u5], writes=["m2%d" % u5])
                            P.op("dve", lambda e, cabs=cabs, t0=t0, m1u=m1u, m2u=m2u: e.tensor_tensor(mT[:, cabs, t0:t0 + 512], m1u[:], m2u[:], ALU.add),
                                 reads=["m1%d" % u5, "m2%d" % u5], writes=["mT"])
            P.barrier()
            wo = [sbt(st, "wo%d" % i, [128, 16, 512], BF16) for i in range(2)]
            mod2 = sbt(st, "mod2", [128, D])
            xo = [sbt(st, "xo%d" % i, [128, 512]) for i in range(2)]
            ho = [sbt(st, "ho%d" % i, [128, 512]) for i in range(2)]
            P.dma(mod2[:], MOD[2], writes=["mod2"])
            hi = 0
            for blk in range(4):
                sl = blk % 2
                load_w_block(wo, sl, w_o, 16, blk * 512, 512)
                for t in range(8):
                    i = hi % 2
                    hi += 1
                    P.dma(xo[i][:], x_c[t * 128:(t + 1) * 128, blk * 512:(blk + 1) * 512], writes=["xo%d" % i], key=("xo", i))
                    b = hi % 2
                    for kc in range(16):
                        P.op("pe", lambda e, b=b, kc=kc, t=t, sl=sl: e.matmul(ps[b][:], lhsT=mT[:, kc, t * 128:(t + 1) * 128],
                                                                              rhs=wo[sl][:, kc, :], start=(kc == 0), stop=(kc == 15)),
                             reads=["mT"] + ["wb%d_%d" % (sl, k0) for k0 in range(0, 16, 4)], writes=["ps%d" % b])
                    P.op("dve", lambda e, b=b, i=i, blk=blk: e.tensor_tensor(ho[i][:], ps[b][:], mod2[:, blk * 512:(blk + 1) * 512], ALU.mult),
                         reads=["ps%d" % b, "mod2"], writes=["ho%d" % i])
                    P.op("dve", lambda e, i=i: e.tensor_tensor(ho[i][:], ho[i][:], xo[i][:], ALU.add),
                         reads=["ho%d" % i, "xo%d" % i], writes=["ho%d" % i])
                    P.dma(Hs[t * 128:(t + 1) * 128, blk * 512:(blk + 1) * 512], ho[i][:], reads=["ho%d" % i], key="st")
        P.barrier()

        with ExitStack() as st:
            u2T = sbt(st, "u2T", [128, 16, 1024], BF16)
            with ExitStack() as st6:
                A2 = sbt(st6, "A2", [128, D]); S3 = sbt(st6, "S3", [128, D])
                P.dma(A2[:], MOD[3], writes=["A2"])
                P.dma(S3[:], MOD[4], writes=["S3"])
                norm_mod_T(st6, lambda t: Hs[t * 128:(t + 1) * 128, :], 8, lambda t: (A2, "A2"), lambda t: (S3, "S3"), u2T, "n2")
            P.barrier()
            with ExitStack() as st6:
                q2T = sbt(st6, "q2T", [128, 16, 1024], BF16)
                with ExitStack() as st6w:
                    wq = [sbt(st6w, "wq%d" % i, [128, 16, 512], BF16) for i in range(2)]
                    for blk in range(4):
                        sl = blk % 2
                        load_w_block(wq, sl, w_q, 16, blk * 512, 512)

                        def cons(pst, psn, ch, t0, nt, blk=blk):
                            copy_op(evac_eng(), q2T[:, blk * 4 + ch, t0:t0 + nt], pst[:, 0:nt], reads=[psn], writes=["q2T"])
                        linear_F(u2T, "xT_n2", 16, wq, sl, 512, [(0, 512), (512, 512)], cons)
                P.barrier()
                kin6 = sbt(st6, "kin6", [128, 2, 128])
                kT6 = sbt(st6, "kT6", [128, 2, 128], BF16)
                P.dma(kin6[:, 0, :], keys1, writes=["kin6_a"], key="s6k")
                P.dma(kin6[:, 1, :], keys2, writes=["kin6_b"], key="s6k")
                for i in range(2):
                    P.op("pe", lambda e, i=i: e.transpose(ps[4][:, i * 128:(i + 1) * 128], kin6[:, i, :], ident[:]),
                         reads=["kin6_a", "kin6_b", "ident"], writes=["ps4"])
                P.op("dve", lambda e: e.tensor_copy(kT6[:], ps[4][:, 0:256].rearrange("p (i k) -> p i k", i=2)),
                     reads=["ps4"], writes=["kT6"])
                sc = sbt(st6, "sc", [128, 16, 128]); tmp6 = sbt(st6, "tmp6", [128, 256])
                mx = sbt(st6, "mx", [128, 16, 16]); negm = sbt(st6, "negm", [128, 16])
                E = sbt(st6, "E", [128, 16, 128]); Et = sbt(st6, "Et", [128, 16, 16])
                cand = sbt(st6, "cand", [128, 8, 256]); ctop = sbt(st6, "ctop", [128, 8, 16])
                Zs6 = sbt(st6, "Zs6", [128, 8]); rZ = sbt(st6, "rZ", [128, 8])
                Pd = [sbt(st6, "Pd%d" % i, [128, 8, 128]) for i in range(3)]
                Gm = [sbt(st6, "Gm%d" % i, [128, 8, 8, 128], BF16) for i in range(2)]
                gst = [sbt(st6, "gst%d" % i, [128, 8, 128], BF16) for i in range(2)]
                E1n = sbt(st6, "E1n", [128, 8, 128]); Etn = sbt(st6, "Etn", [128, 8, 16]); ctop2 = sbt(st6, "ctop2", [128, 8, 16])
                thrg = sbt(st6, "thrg", [128, 8])
                gi = {"n": 0}
                pub = [sbt(st6, "pub%d" % i, [128, D], BF16) for i in range(2)]
                puT = [sbt(st6, "puT%d" % i, [128, 16, 128], BF16) for i in range(2)]
                actb = [sbt(st6, "actb%d" % i, [128, 1024], BF16) for i in range(2)]

                def phaseA(ch):
                    sl = ch % 2
                    P.dma(pub[sl][:], pu[ch * 128:(ch + 1) * 128, :], writes=["pub%d" % sl], key=("pub", sl), queue="pool")
                    for half in range(2):
                        for j in range(8):
                            kc = half * 8 + j
                            P.op("pe", lambda e, kc=kc, half=half, j=j: e.transpose(
                                psb[half][:, j * 128:(j + 1) * 128], pub[sl][:, kc * 128:(kc + 1) * 128], identb[:]),
                                reads=["pub%d" % sl, "identb"], writes=["psb%d" % half])
                        copy_op("act", puT[sl][:, half * 8:(half + 1) * 8, :], psb[half][:].rearrange("p (k t) -> p k t", k=8),
                                reads=["psb%d" % half], writes=["puT%d" % sl])
                    for th in range(2):
                        for kc in range(16):
                            P.op("pe", lambda e, kc=kc, th=th: e.matmul(ps[4 + th][:], lhsT=puT[sl][:, kc, :],
                                                                        rhs=u2T[:, kc, th * 512:(th + 1) * 512],
                                                                        start=(kc == 0), stop=(kc == 15)),
                                 reads=["puT%d" % sl, "xT_n2"], writes=["ps%d" % (4 + th)])
                        P.op("act", lambda e, th=th: e.activation(actb[sl][:, th * 512:(th + 1) * 512], ps[4 + th][:], AF.Gelu),
                             reads=["ps%d" % (4 + th)], writes=["actb%d_%d" % (sl, th)])
                    P.dma(ACTs[ch * 128:(ch + 1) * 128, :], actb[sl][:], reads=["actb%d_0" % sl, "actb%d_1" % sl], key=("st", "actb%d" % sl))

                def g_all():
                    for t in range(8):
                        for c in range(16):
                            P.op("pe", lambda e, c=c, t=t: e.matmul(ps[c // 4][:, (c % 4) * 128:(c % 4 + 1) * 128],
                                                                    lhsT=q2T[:, c, t * 128:(t + 1) * 128], rhs=kT6[:, c % 2, :],
                                                                    start=True, stop=True), reads=["q2T", "kT6"], writes=["ps%d" % (c // 4)])
                        for b4 in range(4):
                            copy_op(evac_eng(), sc[:, b4 * 4:(b4 + 1) * 4, :], ps[b4][:].rearrange("p (c k) -> p c k", c=4),
                                    reads=["ps%d" % b4], writes=["sc"])
                        for c in range(16):
                            P.op("dve", lambda e, c=c: e.max(out=mx[:, c, 0:8], in_=sc[:, c, :]), reads=["sc"], writes=["mx"])
                            P.op("dve", lambda e, c=c: e.match_replace(out=tmp6[:, 0:128], in_to_replace=mx[:, c, 0:8], in_values=sc[:, c, :],
                                                                       imm_value=-1e30), reads=["sc", "mx"], writes=["tmp6"])
                            P.op("dve", lambda e, c=c: e.max(out=mx[:, c, 8:16], in_=tmp6[:, 0:128]), reads=["tmp6"], writes=["mx"])
                        P.op("dve", lambda e: e.tensor_scalar(negm[:], mx[:, :, 0], -1.0, None, op0=ALU.mult), reads=["mx"], writes=["negm"])
                        for c in range(16):
                            P.op("act", lambda e, c=c: e.activation(E[:, c, :], sc[:, c, :], AF.Exp, bias=negm[:, c:c + 1]),
                                 reads=["sc", "negm"], writes=["E"])
                            P.op("act", lambda e, c=c: e.activation(Et[:, c, :], mx[:, c, :], AF.Exp, bias=negm[:, c:c + 1]),
                                 reads=["mx", "negm"], writes=["Et"])
                        Et4 = Et[:].rearrange("p (h two) a -> p h two a", two=2)
                        P.op("dve", lambda e, Et4=Et4: e.tensor_tensor(cand[:].rearrange("p h (a b) -> p h a b", a=16),
                                                                       Et4[:, :, 0, :].unsqueeze(3).to_broadcast([128, 8, 16, 16]),
                                                                       Et4[:, :, 1, :].unsqueeze(2).to_broadcast([128, 8, 16, 16]), ALU.mult),
                             reads=["Et"], writes=["cand"])
                        for hh in range(8):
                            P.op("dve", lambda e, hh=hh: e.max(out=ctop[:, hh, 0:8], in_=cand[:, hh, :]), reads=["cand"], writes=["ctop"])
                            P.op("dve", lambda e, hh=hh: e.match_replace(out=tmp6[:], in_to_replace=ctop[:, hh, 0:8], in_values=cand[:, hh, :],
                                                                         imm_value=-1e30), reads=["cand", "ctop"], writes=["tmp6"])
                            P.op("dve", lambda e, hh=hh: e.max(out=ctop[:, hh, 8:16], in_=tmp6[:]), reads=["tmp6"], writes=["ctop"])
                        P.op("dve", lambda e: e.reduce_sum(Zs6[:], ctop[:], axis=AX.X), reads=["ctop"], writes=["Zs6"])
                        P.op("dve", lambda e: e.reciprocal(rZ[:], Zs6[:]), reads=["Zs6"], writes=["rZ"])
                        E4 = E[:].rearrange("p (h two) k -> p h two k", two=2)
                        P.op("dve", lambda e, E4=E4: e.tensor_tensor(E1n[:], E4[:, :, 0, :], rZ[:].unsqueeze(2).to_broadcast([128, 8, 128]), ALU.mult),
                             reads=["E", "rZ"], writes=["E1n"])
                        P.op("dve", lambda e: e.tensor_tensor(thrg[:], ctop[:, :, 15], rZ[:], ALU.mult), reads=["ctop", "rZ"], writes=["thrg"])
                        P.op("dve", lambda e: e.tensor_scalar(thrg[:], thrg[:], 1.0 - 1e-6, None, op0=ALU.mult), reads=["thrg"], writes=["thrg"])
                        yield
                        for ib in range(16):
                            w = ib % 2
                            for hh in range(8):
                                k3 = gi["n"] % 3
                                gi["n"] += 1
                                if False:
                                    for j in range(8):
                                        P.op("act", lambda e, hh=hh, ib=ib, k3=k3, j=j: e.activation(
                                            Pd[k3][:, j, :], E[:, 2 * hh + 1, :], AF.Identity, scale=E1n[:, hh, ib * 8 + j:ib * 8 + j + 1]),
                                            reads=["E", "E1n"], writes=["Pd%d" % k3])
                                else:
                                    P.op("dve", lambda e, hh=hh, ib=ib, k3=k3: e.tensor_tensor(
                                        Pd[k3][:], E1n[:, hh, ib * 8:(ib + 1) * 8].unsqueeze(2).to_broadcast([128, 8, 128]),
                                        E[:, 2 * hh + 1, :].unsqueeze(1).to_broadcast([128, 8, 128]), ALU.mult),
                                        reads=["E", "E1n"], writes=["Pd%d" % k3])
                                P.op("dve", lambda e, hh=hh, k3=k3, w=w: e.scalar_tensor_tensor(
                                    out=Gm[w][:, hh, :, :], in0=Pd[k3][:], scalar=thrg[:, hh:hh + 1], in1=Pd[k3][:], op0=ALU.is_ge, op1=ALU.mult),
                                    reads=["Pd%d" % k3, "thrg"], writes=["Gm%d_%d" % (w, hh)])
                            for j in range(8):
                                for hh in range(8):
                                    P.op("pe", lambda e, hh=hh, j=j, w=w: e.matmul(
                                        ps[2 * w + j // 4][:, (j % 4) * 128:(j % 4 + 1) * 128], lhsT=Gm[w][:, hh, j, :], rhs=identb[:],
                                        start=(hh == 0), stop=(hh == 7)),
                                        reads=["Gm%d_%d" % (w, hh), "identb"], writes=["ps%d" % (2 * w + j // 4)])
                            for q in range(2):
                                copy_op(evac_eng() if False else "act", gst[w][:, q * 4:(q + 1) * 4, :], ps[2 * w + q][:].rearrange("p (k t) -> p k t", k=4),
                                        reads=["ps%d" % (2 * w + q)], writes=["gst%d" % w])
                            P.dma(GTP[ib * 1024:(ib + 1) * 1024, t * 128:(t + 1) * 128].rearrange("(c p) t -> p c t", p=128), gst[w][:],
                                  reads=["gst%d" % w], key="st")
                            yield

                nxt = 0
                for k, _ in enumerate(g_all()):
                    if nxt < 128:
                        phaseA(nxt)
                        nxt += 1
                while nxt < 128:
                    phaseA(nxt)
                    nxt += 1
            P.barrier()
            with ExitStack() as st6:
                acc = sbt(st6, "acc", [128, 8, D])
                pvb = [sbt(st6, "pvb%d" % i, [128, 4, D], BF16) for i in range(2)]
                gtc = [sbt(st6, "gtc%d" % i, [128, 4, 1024], BF16) for i in range(2)]
                actc = [sbt(st6, "actc%d" % i, [128, 4, 1024], BF16) for i in range(2)]
                coef = actc
                P.op("pool", lambda e: e.memset(acc[:], 0.0), writes=["acc"])
                for grp in range(32):
                    gw = grp % 2
                    r0 = grp * 512
                    P.dma(pvb[gw][:], pv[r0:r0 + 512, :].rearrange("(c p) d -> p c d", p=128), writes=["pvb%d" % gw], key=("pvb", gw), queue="pool")
                    P.dma(gtc[gw][:], GTP[r0:r0 + 512, :].rearrange("(c p) t -> p c t", p=128), writes=["gtc%d" % gw], key=("gtc", gw))
                    P.dma(actc[gw][:], ACTs[r0:r0 + 512, :].rearrange("(c p) t -> p c t", p=128), writes=["actc%d" % gw], key=("actc", gw))
                    P.op("dve", lambda e, gw=gw: e.tensor_tensor(actc[gw][:], actc[gw][:], gtc[gw][:], ALU.mult),
                         reads=["actc%d" % gw, "gtc%d" % gw], writes=["actc%d" % gw, "coef%d" % gw])
                    for t in range(8):
                        for dq in range(4):
                            for c4 in range(4):
                                P.op("pe", lambda e, gw=gw, c4=c4, t=t, dq=dq: e.matmul(
                                    ps[dq][:], lhsT=coef[gw][:, c4, t * 128:(t + 1) * 128], rhs=pvb[gw][:, c4, dq * 512:(dq + 1) * 512],
                                    start=(c4 == 0), stop=(c4 == 3)), reads=["actc%d" % gw, "pvb%d" % gw], writes=["ps%d" % dq])
                        for dq in range(4):
                            eng = "dve" if dq % 2 == 0 else "pool"
                            P.op("dve", lambda e, t=t, dq=dq: e.tensor_tensor(acc[:, t, dq * 512:(dq + 1) * 512],
                                                                              acc[:, t, dq * 512:(dq + 1) * 512], ps[dq][:], ALU.add),
                                 reads=["acc", "ps%d" % dq], writes=["acc"])
                mod5 = sbt(st6, "mod5", [128, D]); gF = sbt(st6, "gF", [128, D])
                hin = sbt(st6, "hin", [128, D]); fo = sbt(st6, "fo", [128, D]); ssf = sbt(st6, "ssf", [128, 4])
                P.dma(mod5[:], MOD[5], writes=["mod5"])
                P.dma(gF[:], gF_d.partition_broadcast(128), writes=["gF"])
                for t in range(8):
                    P.dma(hin[:], Hs[t * 128:(t + 1) * 128, :], writes=["hin"])
                    P.op("dve", lambda e, t=t: e.tensor_tensor(acc[:, t, :], acc[:, t, :], mod5[:], ALU.mult),
                         reads=["acc", "mod5"], writes=["acc"])
                    P.op("dve", lambda e, t=t: e.tensor_tensor(hin[:], hin[:], acc[:, t, :], ALU.add), reads=["hin", "acc"], writes=["hin"])
                    P.op("dve", lambda e: e.memset(ssf[:, 0:1], 0.0), writes=["ssfa"])
                    P.op("act", lambda e: e.activation(fo[:], hin[:], AF.Square, accum_out=ssf[:, 0:1]), reads=["hin", "ssfa"], writes=["fo", "ssfa"])
                    P.op("act", lambda e: e.activation(ssf[:, 1:2], ssf[:, 0:1], AF.Sqrt, bias=cst[:, 1:2], scale=1.0 / D),
                         reads=["ssfa"], writes=["ssfb"])
                    P.op("dve", lambda e: e.reciprocal(ssf[:, 2:3], ssf[:, 1:2]), reads=["ssfb"], writes=["ssfc"])
                    P.op("dve", lambda e: e.scalar_tensor_tensor(out=fo[:], in0=hin[:], scalar=ssf[:, 2:3], in1=gF[:], op0=ALU.mult, op1=ALU.mult),
                         reads=["hin", "ssfc", "gF", "fo"], writes=["fo"])
                    P.dma(out_d[t * 128:(t + 1) * 128, :], fo[:], reads=["fo"], key="out")
        P.emit()
    return nc


def _rope_tables(flip):
    pos = np.arange(9 * 128)
    if flip:
        pos = (S - 1) - pos
    row = (pos // 64).astype(np.float32)
    col = (pos % 64).astype(np.float32)
    freqs = (np.float32(10000.0) ** (-np.arange(32, dtype=np.float32) / np.float32(32))).astype(np.float32)
    ar = row[:, None] * freqs[None, :]
    ac = col[:, None] * freqs[None, :]
    ang = np.concatenate([ar, ar, ac, ac], axis=-1).astype(np.float32)
    cos = np.cos(ang).astype(np.float32)
    sin = np.sin(ang).astype(np.float32)
    sgn = np.concatenate([-np.ones(32), np.ones(32), -np.ones(32), np.ones(32)]).astype(np.float32)
    return np.ascontiguousarray(np.tile(cos, (1, 16))), np.ascontiguousarray(np.tile(sin * sgn[None, :], (1, 16)))


_NC_CACHE = {}


def make_in_maps(x, c, ctx, c_ctx, ada_w, ada_b, norm1_g, w_in, conv_w, conv_b, dt_bias, a_log, ssm_d,
                 ssm_norm_g, attn_sink, w_branch_ssm, w_branch_attn, w_out, norm2_g, peer_wq, peer_keys1,
                 peer_keys2, peer_u, peer_v, final_norm_g):
    f = lambda a: np.ascontiguousarray(np.asarray(a, dtype=np.float32))
    x = f(x); ctx = f(ctx); c = f(c); c_ctx = f(c_ctx)
    w_in0 = f(w_in)[0]
    shared = {
        "ada_w": f(ada_w)[0], "ada_b": f(ada_b)[0][None, :], "norm1_g": f(norm1_g)[0][None, :],
        "norm2_g": f(norm2_g)[0][None, :], "final_g": f(final_norm_g)[None, :], "w_in": w_in0,
        "ssm_d": f(ssm_d)[0][None, :], "ssm_norm_g": f(ssm_norm_g)[0][None, :], "attn_sink": f(attn_sink)[0][None, :],
        "w_bs": f(w_branch_ssm)[0], "w_ba": f(w_branch_attn)[0], "w_o": f(w_out)[0], "w_q": f(peer_wq)[0],
        "keys1": f(peer_keys1)[0], "keys2": f(peer_keys2)[0], "peer_u": f(peer_u)[0], "peer_v": f(peer_v)[0],
        "ident": np.eye(128, dtype=np.float32),
        "triF": np.triu(np.ones((128, 128), np.float32)),
        "triB": np.tril(np.ones((128, 128), np.float32)),
        "cc_fm": np.ascontiguousarray(c_ctx.reshape(16, 128).T),
    }
    cw = f(conv_w)[0]
    cb = f(conv_b)[0]
    convb_fm = np.ascontiguousarray(cb.reshape(48, 128).T)
    wdt = w_in0[:, C_DT:C_DT + 128]
    per_flip = {}
    for flip in (0, 1):
        cwf = cw[::-1] if flip else cw
        convw_fm = np.ascontiguousarray(cwf.T.reshape(48, 128, 5).transpose(1, 0, 2))
        wd = np.concatenate([wdt[:, 64:], wdt[:, :64]], axis=1) if flip else wdt
        dtb = f(dt_bias)[0][::-1] if flip else f(dt_bias)[0]
        al = f(a_log)[0][::-1] if flip else f(a_log)[0]
        cos_t, sin_t = _rope_tables(flip)
        per_flip[flip] = {"convw_fm": convw_fm, "convb_fm": convb_fm, "w_dt": np.ascontiguousarray(wd),
                          "dt_bias": np.ascontiguousarray(dtb.reshape(1, 128)), "a_log": np.ascontiguousarray(al.reshape(1, 128)),
                          "cos_t": cos_t, "sin_t": sin_t}
    in_maps = []
    for core in range(8):
        b, s = core // 2, core % 2
        m = dict(shared)
        m.update(per_flip[s])
        m["x_c"] = np.ascontiguousarray(x[b, ::-1] if s else x[b])
        m["ctx_c"] = np.ascontiguousarray(ctx[b, ::-1] if s else ctx[b])
        m["c_fm"] = np.ascontiguousarray(c[b].reshape(16, 128).T)
        in_maps.append(m)
    return in_maps


def kernel(**inputs):
    in_maps = make_in_maps(**inputs)
    if "nc" not in _NC_CACHE:
        _NC_CACHE["nc"] = build(False)
    nc = _NC_CACHE["nc"]
    res = run_bass_kernel_spmd(nc, in_maps, core_ids=list(range(8)))
    out = np.zeros((4, S, D), np.float32)
    for core in range(8):
        b, s = core // 2, core % 2
        o = np.asarray(res.results[core]["out"], dtype=np.float32)
        if s == 0:
            out[b, 0:1024] = o
        else:
            out[b, 1024:2048] = o[::-1]
    return out
```

```python
import math
from contextlib import ExitStack
import numpy as np
import concourse.bass as bass
import concourse.mybir as mybir
from concourse.bass_utils import run_bass_kernel_spmd

F32 = mybir.dt.float32
BF16 = mybir.dt.bfloat16
AF = mybir.ActivationFunctionType
ALU = mybir.AluOpType
AX = mybir.AxisListType

D = 2048
S = 2048
CTXL = 256
NTOK = S + CTXL
NT_ALL = 18
NT_OWN = 8
DI = 4096
NH = 64
C_K, C_V, C_XBC, C_DT, C_Q, C_Z, C_G = 0, 512, 1024, 7168, 7296, 9344, 13440
NE = 16384
EPS = 1e-6
SCALE = 128.0 ** -0.5

COMPUTE = ("pe", "act", "dve", "pool")
ALLENG = ("pe", "act", "dve", "pool", "sp")


class Prog:
    def __init__(self, nc, n_dma_sems=96):
        self.nc = nc
        self.lists = {e: [] for e in ALLENG}
        self.semnames = []
        self.semval = {}
        self.waited = {e: {} for e in ALLENG}
        self.res = {}
        for e in COMPUTE:
            self._newsem("E_" + e)
        self.n_dma_sems = n_dma_sems
        self.dma_keys = {}

    def _newsem(self, key):
        self.semnames.append(key)
        self.semval[key] = 0

    def _dma_sem(self, key):
        if key not in self.dma_keys:
            name = "D_%d" % len(self.dma_keys)
            assert len(self.dma_keys) < self.n_dma_sems, "too many dma sems"
            self.dma_keys[key] = name
            self._newsem(name)
        return self.dma_keys[key]

    def _deps(self, eng, reads, writes):
        evs = {}

        def add(ev):
            if ev is None:
                return
            s, v = ev
            if evs.get(s, 0) < v:
                evs[s] = v

        for r in reads:
            st = self.res.get(r)
            if st:
                add(st["w"])
        for w in writes:
            st = self.res.get(w)
            if st:
                add(st["w"])
                for s, v in st["r"].items():
                    add((s, v))
        out = []
        for s, v in evs.items():
            if eng == "pe" and s == "E_pe":
                continue
            if self.waited[eng].get(s, 0) >= v:
                continue
            self.waited[eng][s] = v
            out.append((s, v))
        return out

    def _commit(self, ev, reads, writes):
        s, v = ev
        for r in reads:
            st = self.res.setdefault(r, {"w": None, "r": {}})
            if st["r"].get(s, 0) < v:
                st["r"][s] = v
        for w in writes:
            self.res[w] = {"w": ev, "r": {}}

    def op(self, eng, fn, reads=(), writes=()):
        waits = self._deps(eng, reads, writes)
        s = "E_" + eng
        self.semval[s] += 1
        ev = (s, self.semval[s])
        self.lists[eng].append((waits, fn, s, 1))
        self._commit(ev, reads, writes)

    def dma(self, out, in_, reads=(), writes=(), key=None, queue="sp"):
        if key is None or key == "st":
            key = ("ld", writes[0]) if (writes and key is None) else ("st", reads[0])
        s = self._dma_sem(key)
        waits = self._deps(queue, reads, writes)
        self.semval[s] += 16
        ev = (s, self.semval[s])
        self.lists[queue].append((waits, lambda e: e.dma_start(out=out, in_=in_), s, 16))
        self._commit(ev, reads, writes)

    def barrier(self):
        for e in ALLENG:
            waits = []
            for s in self.semnames:
                v = self.semval[s]
                if v > 0 and self.waited[e].get(s, 0) < v:
                    if e == "pe" and s == "E_pe":
                        continue
                    self.waited[e][s] = v
                    waits.append((s, v))
            if waits:
                self.lists[e].append((waits, None, None, 0))
        self.res = {}

    def emit(self):
        nc = self.nc
        waits = [(s, self.semval[s]) for s in self.semnames if self.semval[s] > 0]
        self.lists["sp"].append((waits, None, None, 0))
        with ExitStack() as st:
            sems = {}
            for s in self.semnames:
                sems[s] = st.enter_context(nc.semaphore(s))
            block = st.enter_context(nc.Block())

            def mk(engname):
                def body(eng):
                    for waits, fn, s, inc in self.lists[engname]:
                        for (ws, wv) in waits:
                            eng.wait_ge(sems[ws], wv)
                        if fn is not None:
                            fn(eng).then_inc(sems[s], inc)
                return body

            block.tensor(mk("pe"))
            block.scalar(mk("act"))
            block.vector(mk("dve"))
            block.gpsimd(mk("pool"))
            block.sync(mk("sp"))


def build(debug=False):
    nc = bass.Bass("TRN2", target_bir_lowering=False)
    P = Prog(nc)
    skind = "ExternalOutput" if debug else "Internal"

    def din(name, shape, dt=F32):
        return nc.dram_tensor(name, shape, dt, kind="ExternalInput").ap()

    def dscr(name, shape, dt=F32):
        return nc.dram_tensor(name, shape, dt, kind=skind).ap()

    x_c = din("x_c", [S, D])
    ctx_c = din("ctx_c", [CTXL, D])
    c_fm = din("c_fm", [128, 16])
    cc_fm = din("cc_fm", [128, 16])
    ada_w = din("ada_w", [D, 6 * D])
    ada_b = din("ada_b", [1, 6 * D])
    g1_d = din("norm1_g", [1, D])
    g2_d = din("norm2_g", [1, D])
    gF_d = din("final_g", [1, D])
    w_in = din("w_in", [D, 17536])
    w_dt = din("w_dt", [D, 128])
    convw = din("convw_fm", [128, 48, 5])
    convb = din("convb_fm", [128, 48])
    dtb_d = din("dt_bias", [1, 128])
    alog_d = din("a_log", [1, 128])
    ssmd_d = din("ssm_d", [1, 64])
    gN_d = din("ssm_norm_g", [1, DI])
    sink_d = din("attn_sink", [1, 16])
    w_bs = din("w_bs", [DI, D])
    w_ba = din("w_ba", [D, D])
    w_o = din("w_o", [D, D])
    w_q = din("w_q", [D, D])
    keys1 = din("keys1", [128, 128])
    keys2 = din("keys2", [128, 128])
    pu = din("peer_u", [NE, D])
    pv = din("peer_v", [NE, D])
    ident_d = din("ident", [128, 128])
    triF_d = din("triF", [128, 128])
    triB_d = din("triB", [128, 128])
    cos_d = din("cos_t", [9 * 128, 2048])
    sin_d = din("sin_t", [9 * 128, 2048])
    out_d = nc.dram_tensor("out", [1024, D], F32, kind="ExternalOutput").ap()

    MOD = dscr("MOD_scr", [8, 128, D])
    KV = dscr("KV_scr", [NTOK, 1024])
    DTs = dscr("DT_scr", [NTOK, 128])
    Qs = dscr("Q_scr", [1024, 2048])
    Zs = dscr("Z_scr", [1024, DI])
    XBCT = dscr("XBCT_scr", [6144, NTOK])
    GTs = dscr("GT_scr", [4096, 1024])
    ATT = dscr("ATT_scr", [128, 16, 1024], BF16)
    XTM = dscr("XTM_scr", [NTOK, 5120], BF16)
    BCT = dscr("BCT_scr", [2048, NTOK], BF16)
    Y1 = dscr("Y1_scr", [1024, DI])
    Y2 = dscr("Y2_scr", [1024, DI])
    Hs = dscr("H_scr", [1024, D])
    GTP = dscr("GTP_scr", [NE, 1024], BF16)
    ACTs = dscr("ACT_scr", [NE, 1024], BF16)

    with ExitStack() as top:
        uid = {"n": 0}

        def sbt(st, name, shape, dt=F32):
            uid["n"] += 1
            return st.enter_context(nc.sbuf_tensor("s%d_%s" % (uid["n"], name), shape, dt))

        ps = [top.enter_context(nc.psum_tensor("ps%d" % i, [128, 512], F32)) for i in range(6)]
        psb = [top.enter_context(nc.psum_tensor("psb%d" % i, [128, 1024], BF16)) for i in range(2)]
        ident = sbt(top, "ident", [128, 128])
        identb = sbt(top, "identb", [128, 128], BF16)
        triF = sbt(top, "triF", [128, 128])
        triB = sbt(top, "triB", [128, 128])
        triFb = sbt(top, "triFb", [128, 128], BF16)
        triBb = sbt(top, "triBb", [128, 128], BF16)
        ones = sbt(top, "ones", [128, 128])
        onesb = sbt(top, "onesb", [128, 128], BF16)
        cst = sbt(top, "cst", [128, 4])
        P.dma(ident[:], ident_d, writes=["ident"])
        P.dma(triF[:], triF_d, writes=["triF"])
        P.dma(triB[:], triB_d, writes=["triB"])
        P.op("dve", lambda e: e.tensor_copy(identb[:], ident[:]), reads=["ident"], writes=["identb"])
        P.op("dve", lambda e: e.tensor_copy(triFb[:], triF[:]), reads=["triF"], writes=["triFb"])
        P.op("dve", lambda e: e.tensor_copy(triBb[:], triB[:]), reads=["triB"], writes=["triBb"])
        P.op("dve", lambda e: e.memset(ones[:], 1.0), writes=["ones"])
        P.op("dve", lambda e: e.memset(onesb[:], 1.0), writes=["onesb"])
        P.op("dve", lambda e: e.memset(cst[:, 0:1], 1.0), writes=["cst0"])
        P.op("dve", lambda e: e.memset(cst[:, 1:2], EPS), writes=["cst1"])
        P.op("dve", lambda e: e.memset(cst[:, 2:3], 0.0), writes=["cst2"])
        P.barrier()
        rr = {"n": 0}

        def evac_eng():
            rr["n"] += 1
            return "act" if rr["n"] % 2 == 0 else "dve"

        def copy_op(eng, out, in_, reads, writes):
            if eng == "act":
                P.op("act", lambda e: e.copy(out, in_), reads=reads, writes=writes)
            else:
                P.op(eng, lambda e: e.tensor_copy(out, in_), reads=reads, writes=writes)

        def load_w_block(wb, slot, Wd, KC, col0, ncols):
            for k0 in range(0, KC, 4):
                P.dma(wb[slot][:, k0:k0 + 4, 0:ncols],
                      Wd[k0 * 128:(k0 + 4) * 128, col0:col0 + ncols].rearrange("(k p) n -> p k n", p=128),
                      writes=["wb%d_%d" % (slot, k0)], key=("wb", slot), queue="pool")

        def norm_mod_T(st, src_rows, n_tiles, Atile_of, Stile_of, xT, tagp):
            xt = [sbt(st, tagp + "xt%d" % i, [128, D]) for i in range(2)]
            junk = sbt(st, tagp + "junk", [128, D])
            t1 = sbt(st, tagp + "t1", [128, D])
            ub = [sbt(st, tagp + "ub%d" % i, [128, D], BF16) for i in range(2)]
            ss = sbt(st, tagp + "ss", [128, 4])
            for t in range(n_tiles):
                sl = t % 2
                P.dma(xt[sl][:], src_rows(t), writes=[tagp + "xt%d" % sl], key=(tagp + "xt", sl))
                P.op("dve", lambda e: e.memset(ss[:, 0:1], 0.0), writes=[tagp + "ss0"])
                P.op("act", lambda e, sl=sl: e.activation(junk[:], xt[sl][:], AF.Square, accum_out=ss[:, 0:1]),
                     reads=[tagp + "xt%d" % sl, tagp + "ss0"], writes=[tagp + "junk", tagp + "ss0"])
                P.op("act", lambda e: e.activation(ss[:, 1:2], ss[:, 0:1], AF.Sqrt, bias=cst[:, 1:2], scale=1.0 / D),
                     reads=[tagp + "ss0"], writes=[tagp + "ss1"])
                P.op("dve", lambda e: e.reciprocal(ss[:, 2:3], ss[:, 1:2]), reads=[tagp + "ss1"], writes=[tagp + "ss2"])
                A, An = Atile_of(t)
                Sh, Sn = Stile_of(t)
                P.op("dve", lambda e, sl=sl, A=A: e.scalar_tensor_tensor(out=t1[:], in0=xt[sl][:], scalar=ss[:, 2:3], in1=A[:],
                                                                      op0=ALU.mult, op1=ALU.mult),
                     reads=[tagp + "xt%d" % sl, tagp + "ss2", An], writes=[tagp + "t1"])
                P.op("dve", lambda e, sl=sl, Sh=Sh: e.tensor_tensor(ub[sl][:], t1[:], Sh[:], ALU.add),
                     reads=[tagp + "t1", Sn], writes=[tagp + "ub%d" % sl])
                for half in range(2):
                    for j in range(8):
                        kc = half * 8 + j
                        P.op("pe", lambda e, sl=sl, kc=kc, half=half, j=j: e.transpose(
                            psb[half][:, j * 128:(j + 1) * 128], ub[sl][:, kc * 128:(kc + 1) * 128], identb[:]),
                            reads=[tagp + "ub%d" % sl, "identb"], writes=["psb%d" % half])
                    copy_op(evac_eng(), xT[:, half * 8:(half + 1) * 8, t * 128:(t + 1) * 128],
                            psb[half][:].rearrange("p (k t) -> p k t", k=8),
                            reads=["psb%d" % half], writes=["xT_" + tagp])

        def linear_T(xT, xTname, KC, wb, slot, ncols, tiles, consume):
            for t in tiles:
                b = rr["n"] % 4
                for kc in range(KC):
                    P.op("pe", lambda e, b=b, kc=kc, t=t: e.matmul(ps[b][:, 0:ncols], lhsT=xT[:, kc, t * 128:(t + 1) * 128],
                                                                  rhs=wb[slot][:, kc, 0:ncols], start=(kc == 0), stop=(kc == KC - 1)),
                         reads=[xTname] + ["wb%d_%d" % (slot, k0) for k0 in range(0, 16, 4)], writes=["ps%d" % b])
                consume(ps[b], "ps%d" % b, t)

        def linear_F(xT, xTname, KC, wb, slot, ncols, tokgroups, consume):
            for ch in range(ncols // 128):
                for (t0, nt) in tokgroups:
                    b = rr["n"] % 4
                    for kc in range(KC):
                        P.op("pe", lambda e, b=b, kc=kc, t0=t0, nt=nt, ch=ch: e.matmul(
                            ps[b][:, 0:nt], lhsT=wb[slot][:, kc, ch * 128:(ch + 1) * 128], rhs=xT[:, kc, t0:t0 + nt],
                            start=(kc == 0), stop=(kc == KC - 1)),
                            reads=[xTname] + ["wb%d_%d" % (slot, k0) for k0 in range(0, 16, 4)], writes=["ps%d" % b])
                    consume(ps[b], "ps%d" % b, ch, t0, nt)

        with ExitStack() as st:
            cs = sbt(st, "cs", [128, 2, 16])
            csb = sbt(st, "csb", [128, 2, 16, 128], BF16)
            adab = sbt(st, "adab", [128, 6 * D])
            g1 = sbt(st, "g1", [128, D])
            g2 = sbt(st, "g2", [128, D])
            aw = [sbt(st, "aw%d" % i, [128, 16, 512], BF16) for i in range(3)]
            res = [sbt(st, "ares%d" % i, [128, 512]) for i in range(4)]
            P.dma(cs[:, 0, :], c_fm, writes=["cs_a"], key="a0")
            P.dma(cs[:, 1, :], cc_fm, writes=["cs_b"], key="a0")
            P.dma(adab[:], ada_b.partition_broadcast(128), writes=["adab"])
            P.dma(g1[:], g1_d.partition_broadcast(128), writes=["g1"])
            P.dma(g2[:], g2_d.partition_broadcast(128), writes=["g2"])
            P.op("act", lambda e: e.activation(cs[:], cs[:], AF.Silu), reads=["cs_a", "cs_b"], writes=["cs"])
            P.op("dve", lambda e: e.tensor_copy(csb[:], cs[:].unsqueeze(3).to_broadcast([128, 2, 16, 128])),
                 reads=["cs"], writes=["csb"])
            ri = 0
            for j in range(6):
                for blk in range(4):
                    col0 = j * D + blk * 512
                    sl = (j * 4 + blk) % 3
                    for k0 in range(0, 16, 8):
                        P.dma(aw[sl][:, k0:k0 + 8, :],
                              ada_w[k0 * 128:(k0 + 8) * 128, col0:col0 + 512].rearrange("(k p) n -> p k n", p=128),
                              writes=["aw%d_%d" % (sl, k0)], key=("aw", sl), queue="pool")
                    for which in range(2 if j < 2 else 1):
                        b = ri % 4
                        r = res[ri % 4]
                        rn = "ares%d" % (ri % 4)
                        ri += 1
                        for kc in range(16):
                            P.op("pe", lambda e, b=b, kc=kc, sl=sl, which=which: e.matmul(
                                ps[b][:], lhsT=csb[:, which, kc, :], rhs=aw[sl][:, kc, :], start=(kc == 0), stop=(kc == 15)),
                                reads=["csb"] + ["aw%d_%d" % (sl, k0) for k0 in range(0, 16, 8)], writes=["ps%d" % b])
                        P.op("dve", lambda e, b=b, r=r, col0=col0: e.tensor_tensor(r[:], ps[b][:], adab[:, col0:col0 + 512], ALU.add),
                             reads=["ps%d" % b, "adab"], writes=[rn])
                        if j in (1, 4):
                            g = g1 if j == 1 else g2
                            P.op("dve", lambda e, r=r, g=g, blk=blk: e.scalar_tensor_tensor(
                                out=r[:], in0=r[:], scalar=1.0, in1=g[:, blk * 512:(blk + 1) * 512], op0=ALU.add, op1=ALU.mult),
                                reads=[rn, "g1", "g2"], writes=[rn])
                        if which == 0:
                            mi = {0: 1, 1: 0, 2: 2, 3: 4, 4: 3, 5: 5}[j]
                        else:
                            mi = {0: 7, 1: 6}[j]
                        P.dma(MOD[mi, :, blk * 512:(blk + 1) * 512], r[:], reads=[rn], writes=["MODd"], key="st")
        P.barrier()

        with ExitStack() as st:
            uT = sbt(st, "uT", [128, 16, NTOK], BF16)
            with ExitStack() as st1:
                A1 = sbt(st1, "A1", [128, D]); S1 = sbt(st1, "S1", [128, D])
                Ac = sbt(st1, "Ac", [128, D]); Sc = sbt(st1, "Sc", [128, D])
                P.dma(A1[:], MOD[0], writes=["A1"])
                P.dma(S1[:], MOD[1], writes=["S1"])
                P.dma(Ac[:], MOD[6], writes=["Ac"])
                P.dma(Sc[:], MOD[7], writes=["Sc"])
                norm_mod_T(st1,
                           lambda t: x_c[t * 128:(t + 1) * 128, :] if t < 16 else ctx_c[(t - 16) * 128:(t - 15) * 128, :],
                           NT_ALL,
                           lambda t: (A1, "A1") if t < 16 else (Ac, "Ac"),
                           lambda t: (S1, "S1") if t < 16 else (Sc, "Sc"),
                           uT, "n1")
            P.barrier()
            wb = [sbt(st, "wb%d" % i, [128, 16, 512], BF16) for i in range(2)]
            stg = [sbt(st, "stg%d" % i, [128, 512]) for i in range(4)]
            sti = {"n": 0}

            def store_T(dst_of):
                def consume(pst, psn, t):
                    i = sti["n"] % 4
                    sti["n"] += 1
                    dst = dst_of(t)
                    ncols = dst.shape[1]
                    copy_op(evac_eng(), stg[i][:, 0:ncols], pst[:, 0:ncols], reads=[psn], writes=["stg%d" % i])
                    P.dma(dst, stg[i][:, 0:ncols], reads=["stg%d" % i], key="st")
                return consume

            def store_F(dst_of):
                def consume(pst, psn, ch, t0, nt):
                    i = sti["n"] % 4
                    sti["n"] += 1
                    copy_op(evac_eng(), stg[i][:, 0:nt], pst[:, 0:nt], reads=[psn], writes=["stg%d" % i])
                    P.dma(dst_of(ch, t0, nt), stg[i][:, 0:nt], reads=["stg%d" % i], key="st")
                return consume

            ALLT = list(range(NT_ALL))
            OWNT = list(range(NT_OWN))
            KVT = list(range(9)) + [16, 17]
            ALLG = [(0, 512), (512, 512), (1024, 512), (1536, 512), (2048, 256)]
            OWNG = [(0, 512), (512, 512)]
            blocks = []
            blocks.append((w_in, C_K, 512, "T", KVT, store_T(lambda t: KV[t * 128:(t + 1) * 128, 0:512])))
            blocks.append((w_in, C_V, 512, "T", KVT, store_T(lambda t: KV[t * 128:(t + 1) * 128, 512:1024])))
            blocks.append((w_dt, 0, 128, "T", ALLT, store_T(lambda t: DTs[t * 128:(t + 1) * 128, :])))
            for bi in range(12):
                blocks.append((w_in, C_XBC + bi * 512, 512, "F", ALLG,
                               store_F(lambda ch, t0, nt, bi=bi: XBCT[bi * 512 + ch * 128: bi * 512 + (ch + 1) * 128, t0:t0 + nt])))
            for bi in range(4):
                blocks.append((w_in, C_Q + bi * 512, 512, "T", OWNT,
                               store_T(lambda t, bi=bi: Qs[t * 128:(t + 1) * 128, bi * 512:(bi + 1) * 512])))
            for bi in range(8):
                blocks.append((w_in, C_Z + bi * 512, 512, "T", OWNT,
                               store_T(lambda t, bi=bi: Zs[t * 128:(t + 1) * 128, bi * 512:(bi + 1) * 512])))
            for bi in range(8):
                blocks.append((w_in, C_G + bi * 512, 512, "F", OWNG,
                               store_F(lambda ch, t0, nt, bi=bi: GTs[bi * 512 + ch * 128: bi * 512 + (ch + 1) * 128, t0:t0 + nt])))
            for i, (Wd, col0, ncols, orient, toks, cons) in enumerate(blocks):
                sl = i % 2
                load_w_block(wb, sl, Wd, 16, col0, ncols)
                if orient == "T":
                    linear_T(uT, "xT_n1", 16, wb, sl, ncols, toks, cons)
                else:
                    linear_F(uT, "xT_n1", 16, wb, sl, ncols, toks, cons)
        P.barrier()

        with ExitStack() as st:
            kT = sbt(st, "kT", [128, 4, 11 * 128], BF16)
            Vt = sbt(st, "Vt", [128, 11, 512], BF16)
            qT = sbt(st, "qT", [128, 16, 1024], BF16)
            aT = sbt(st, "aT", [128, 16, 1024], BF16)
            esink = sbt(st, "esink", [128, 16])
            cosb = [sbt(st, "cosb%d" % i, [128, 2048]) for i in range(2)]
            sinb = [sbt(st, "sinb%d" % i, [128, 2048]) for i in range(2)]
            kin = [sbt(st, "kin%d" % i, [128, 1024]) for i in range(2)]
            qin = [sbt(st, "qin%d" % i, [128, 2048]) for i in range(2)]
            r1 = sbt(st, "r1", [128, 2048]); r2 = sbt(st, "r2", [128, 2048])
            rb = [sbt(st, "rb%d" % i, [128, 2048], BF16) for i in range(2)]
            P.dma(esink[:], sink_d.partition_broadcast(128), writes=["esink"])
            P.op("act", lambda e: e.activation(esink[:], esink[:], AF.Exp), reads=["esink"], writes=["esink"])

            def rope(src, nh, sl, out_bf, rn_src, rn_out):
                n = nh * 128
                v = lambda ap: ap.rearrange("p (g b c) -> p g b c", g=nh * 2, b=2, c=32)
                P.op("dve", lambda e: e.tensor_tensor(r1[:, 0:n], src, cosb[sl][:, 0:n], ALU.mult),
                     reads=[rn_src, "cosb%d" % sl], writes=["r1"])
                P.op("dve", lambda e: e.tensor_tensor(v(r2[:, 0:n])[:, :, 0, :], v(src)[:, :, 1, :], v(sinb[sl][:, 0:n])[:, :, 0, :], ALU.mult),
                     reads=[rn_src, "sinb%d" % sl], writes=["r2a"])
                P.op("dve", lambda e: e.tensor_tensor(v(r2[:, 0:n])[:, :, 1, :], v(src)[:, :, 0, :], v(sinb[sl][:, 0:n])[:, :, 1, :], ALU.mult),
                     reads=[rn_src, "sinb%d" % sl], writes=["r2b"])
                P.op("dve", lambda e: e.tensor_tensor(out_bf, r1[:, 0:n], r2[:, 0:n], ALU.add),
                     reads=["r1", "r2a", "r2b"], writes=[rn_out])

            for blk in range(11):
                sl = blk % 2
                row0 = blk * 128 if blk < 9 else S + (blk - 9) * 128
                P.dma(kin[sl][:], KV[row0:row0 + 128, :], writes=["kin%d" % sl], key=("kin", sl))
                copy_op("act", Vt[:, blk, :], kin[sl][:, 512:1024], reads=["kin%d" % sl], writes=["Vt"])
                if blk < 9:
                    P.dma(cosb[sl][:], cos_d[blk * 128:(blk + 1) * 128, :], writes=["cosb%d" % sl], key=None)
                    P.dma(sinb[sl][:], sin_d[blk * 128:(blk + 1) * 128, :], writes=["sinb%d" % sl], key=None)
                    rope(kin[sl][:, 0:512], 4, sl, rb[sl][:, 0:512], "kin%d" % sl, "rb%d" % sl)
                else:
                    copy_op("dve", rb[sl][:, 0:512], kin[sl][:, 0:512], reads=["kin%d" % sl], writes=["rb%d" % sl])
                for g in range(4):
                    P.op("pe", lambda e, sl=sl, g=g: e.transpose(psb[0][:, g * 128:(g + 1) * 128], rb[sl][:, g * 128:(g + 1) * 128], identb[:]),
                         reads=["rb%d" % sl, "identb"], writes=["psb0"])
                copy_op(evac_eng(), kT[:, :, blk * 128:(blk + 1) * 128], psb[0][:, 0:512].rearrange("p (g t) -> p g t", g=4),
                        reads=["psb0"], writes=["kT"])
            for t in range(8):
                sl = t % 2
                P.dma(qin[sl][:], Qs[t * 128:(t + 1) * 128, :], writes=["qin%d" % sl], key=("qin", sl))
                P.dma(cosb[sl][:], cos_d[t * 128:(t + 1) * 128, :], writes=["cosb%d" % sl], key=None)
                P.dma(sinb[sl][:], sin_d[t * 128:(t + 1) * 128, :], writes=["sinb%d" % sl], key=None)
                rope(qin[sl][:], 16, sl, rb[sl][:], "qin%d" % sl, "rb%d" % sl)
                for half in range(2):
                    for j in range(8):
                        hh = half * 8 + j
                        P.op("pe", lambda e, sl=sl, hh=hh, half=half, j=j: e.transpose(
                            psb[half][:, j * 128:(j + 1) * 128], rb[sl][:, hh * 128:(hh + 1) * 128], identb[:]),
                            reads=["rb%d" % sl, "identb"], writes=["psb%d" % half])
                    copy_op(evac_eng(), qT[:, half * 8:(half + 1) * 8, t * 128:(t + 1) * 128],
                            psb[half][:].rearrange("p (k t) -> p k t", k=8), reads=["psb%d" % half], writes=["qT"])
            pT = [sbt(st, "pT%d" % i, [128, 512], BF16) for i in range(4)]
            den = [sbt(st, "den%d" % i, [128, 512]) for i in range(2)]
            units = []
            for qb in range(8):
                for g in range(4):
                    kbs = []
                    if qb >= 1:
                        kbs.append((qb - 1, triBb, "triBb"))
                    kbs.append((qb, None, None))
                    kbs.append((qb + 1, triFb, "triFb"))
                    kbs.append((9, None, None))
                    kbs.append((10, None, None))
                    for ki, (kb, mask, mname) in enumerate(kbs):
                        units.append((qb, g, ki, len(kbs), kb, mask, mname))

            def stageS(u):
                qb, g, ki, n, kb, mask, mname = units[u]
                sb_ = u % 2
                pp = u % 4
                P.op("pe", lambda e: e.matmul(ps[sb_][:], lhsT=kT[:, g, kb * 128:(kb + 1) * 128],
                                              rhs=qT[:, 4 * g:4 * g + 4, qb * 128:(qb + 1) * 128], start=True, stop=True),
                     reads=["kT", "qT"], writes=["ps%d" % sb_])
                P.op("act", lambda e: e.activation(pT[pp][:], ps[sb_][:], AF.Exp, scale=SCALE),
                     reads=["ps%d" % sb_], writes=["pT%d" % pp])
                if mask is not None:
                    P.op("pool", lambda e: e.tensor_tensor(
                        pT[pp][:].rearrange("p (r q) -> p r q", r=4), pT[pp][:].rearrange("p (r q) -> p r q", r=4),
                        mask[:].unsqueeze(1).to_broadcast([128, 4, 128]), ALU.mult),
                        reads=["pT%d" % pp, mname], writes=["pT%d" % pp])

            def stageV(u):
                qb, g, ki, n, kb, mask, mname = units[u]
                pp = u % 4
                par = (qb * 4 + g) % 2
                pa, pb = (2, 3) if par == 0 else (4, 5)
                P.op("pe", lambda e: e.matmul(ps[pa][:], lhsT=Vt[:, kb, g * 128:(g + 1) * 128], rhs=pT[pp][:],
                                              start=(ki == 0), stop=(ki == n - 1)),
                     reads=["Vt", "pT%d" % pp], writes=["ps%d" % pa])
                P.op("pe", lambda e: e.matmul(ps[pb][:], lhsT=onesb[:], rhs=pT[pp][:], start=(ki == 0), stop=(ki == n - 1)),
                     reads=["onesb", "pT%d" % pp], writes=["ps%d" % pb])
                if ki == n - 1:
                    dn = den[par]
                    P.op("dve", lambda e: e.tensor_tensor(dn[:].rearrange("p (r q) -> p r q", r=4),
                                                          ps[pb][:].rearrange("p (r q) -> p r q", r=4),
                                                          esink[:, 4 * g:4 * g + 4].unsqueeze(2).to_broadcast([128, 4, 128]), ALU.add),
                         reads=["ps%d" % pb, "esink"], writes=["den%d" % par])
                    P.op("dve", lambda e: e.reciprocal(dn[:], dn[:]), reads=["den%d" % par], writes=["den%d" % par])
                    P.op("dve", lambda e: e.tensor_tensor(aT[:, 4 * g:4 * g + 4, qb * 128:(qb + 1) * 128],
                                                          ps[pa][:].rearrange("p (r q) -> p r q", r=4),
                                                          dn[:].rearrange("p (r q) -> p r q", r=4), ALU.mult),
                         reads=["ps%d" % pa, "den%d" % par], writes=["aT"])

            for i in range(len(units) + 2):
                if i < len(units):
                    stageS(i)
                if i >= 2:
                    stageV(i - 2)
            P.dma(ATT, aT[:], reads=["aT"], key="st")
        P.barrier()

        with ExitStack() as st:
            cw = sbt(st, "cw", [128, 48, 5]); cb = sbt(st, "cb", [128, 48])
            P.dma(cw[:], convw, writes=["cw"])
            P.dma(cb[:], convb, writes=["cb"])
            cin = [sbt(st, "cin%d" % i, [128, S + 4 + CTXL + 4], BF16) for i in range(2)]
            dg = [sbt(st, "dg%d" % i, [128, 5, 128], BF16) for i in range(2)]
            cout = sbt(st, "cout", [128, 8, NTOK], BF16)
            tstg = [sbt(st, "tstg%d" % i, [128, 1024], BF16) for i in range(2)]
            for i in range(2):
                P.op("pool", lambda e, i=i: e.memset(cin[i][:], 0.0), writes=["cin%d" % i])
            CG = [(0, 0, 512), (512, 512, 512), (1024, 1024, 512), (1536, 1536, 512), (S, S + 4, CTXL)]
            pc = 0
            for grp in range(6):
                for c8 in range(8):
                    cc = grp * 8 + c8
                    sl = cc % 2
                    P.dma(cin[sl][:, 2:2 + S], XBCT[cc * 128:(cc + 1) * 128, 0:S], writes=["cin%d_a" % sl], key=("cin", sl), queue="pool")
                    P.dma(cin[sl][:, S + 6:S + 6 + CTXL], XBCT[cc * 128:(cc + 1) * 128, S:NTOK], writes=["cin%d_b" % sl], key=("cin", sl), queue="pool")
                    for j in range(5):
                        P.op("dve", lambda e, sl=sl, cc=cc, j=j: e.tensor_scalar(dg[sl][:, j, :], identb[:], cw[:, cc, j:j + 1], None, op0=ALU.mult),
                             reads=["identb", "cw"], writes=["dg%d" % sl])
                    for (o0, i0, L) in CG:
                        bnk = pc % 4
                        pc += 1
                        for j in range(5):
                            P.op("pe", lambda e, sl=sl, j=j, i0=i0, L=L, bnk=bnk: e.matmul(
                                ps[bnk][:, 0:L], lhsT=dg[sl][:, j, :], rhs=cin[sl][:, i0 + j:i0 + j + L], start=(j == 0), stop=(j == 4)),
                                reads=["dg%d" % sl, "cin%d" % sl, "cin%d_a" % sl, "cin%d_b" % sl], writes=["ps%d" % bnk])
                        P.op("act", lambda e, cc=cc, c8=c8, o0=o0, L=L, bnk=bnk: e.activation(
                            cout[:, c8, o0:o0 + L], ps[bnk][:, 0:L], AF.Silu, bias=cb[:, cc:cc + 1]),
                            reads=["ps%d" % bnk, "cb"], writes=["cout"])
                if grp < 5:
                    for t in range(NT_ALL):
                        hb = t % 2
                        for c8 in range(8):
                            P.op("pe", lambda e, hb=hb, c8=c8, t=t: e.transpose(
                                psb[hb][:, c8 * 128:(c8 + 1) * 128], cout[:, c8, t * 128:(t + 1) * 128], identb[:]),
                                reads=["cout", "identb"], writes=["psb%d" % hb])
                        copy_op(evac_eng(), tstg[hb][:], psb[hb][:], reads=["psb%d" % hb], writes=["tstg%d" % hb])
                        P.dma(XTM[t * 128:(t + 1) * 128, grp * 1024:(grp + 1) * 1024], tstg[hb][:], reads=["tstg%d" % hb], key="st")
                if grp >= 4:
                    P.dma(BCT[(grp - 4) * 1024:(grp - 3) * 1024, :].rearrange("(c p) t -> p c t", p=128), cout[:], reads=["cout"], key="st2")
        P.barrier()

        with ExitStack() as st:
            hst = [sbt(st, "hst%d" % i, [128, DI]) for i in range(2)]
            hbf = sbt(st, "hbf", [128, DI], BF16)
            xtm = [sbt(st, "xtm%d" % i, [128, 5120], BF16) for i in range(2)]
            bct = [sbt(st, "bct%d" % i, [128, 16, 128], BF16) for i in range(2)]
            dtr = [sbt(st, "dtr%d" % i, [128, 128]) for i in range(2)]
            dtbb = sbt(st, "dtbb", [128, 128]); Ab = sbt(st, "Ab", [128, 128]); Dd = sbt(st, "Dd", [128, 64])
            dtL = [sbt(st, "dt%d" % i, [128, 64]) for i in range(2)]; aaL = [sbt(st, "aa%d" % i, [128, 64]) for i in range(2)]
            acsL = [sbt(st, "acs%d" % i, [128, 64]) for i in range(2)]; atotL = [sbt(st, "atot%d" % i, [128, 64]) for i in range(2)]
            wendL = [sbt(st, "wend%d" % i, [128, 64]) for i in range(2)]; eacsL = [sbt(st, "eacs%d" % i, [128, 64]) for i in range(2)]
            etotL = [sbt(st, "etot%d" % i, [128, 64]) for i in range(2)]
            wxL = [sbt(st, "wx%d" % i, [128, 128]) for i in range(2)]; wxeL = [sbt(st, "wxe%d" % i, [128, DI], BF16) for i in range(2)]
            TA = sbt(st, "TA", [128, 8, 128]); seg = sbt(st, "seg", [128, 8, 128])
            Mt2 = [sbt(st, "Mt%d" % i, [128, 8, 128], BF16) for i in range(2)]; CBa = sbt(st, "CBa", [128, 8, 128]); segL = [sbt(st, "segd%d" % i, [128, 8, 128]) for i in range(2)]
            yt = sbt(st, "yt", [128, DI]); ytmp = sbt(st, "ytmp", [128, 512])
            P.dma(dtbb[:], dtb_d.partition_broadcast(128), writes=["dtbb"])
            P.dma(Ab[:], alog_d.partition_broadcast(128), writes=["Ab"])
            P.dma(Dd[:], ssmd_d.partition_broadcast(128), writes=["Dd"])
            P.op("act", lambda e: e.activation(Ab[:], Ab[:], AF.Exp), reads=["Ab"], writes=["Ab"])
            P.op("dve", lambda e: e.tensor_scalar(Ab[:], Ab[:], -1.0, None, op0=ALU.mult), reads=["Ab"], writes=["Ab"])
            for i in range(2):
                P.op("pool", lambda e, i=i: e.memset(hst[i][:], 0.0), writes=["hst%d" % i])
            DIb = sbt(st, "DIb", [128, 64, 128], BF16)
            P.op("dve", lambda e: e.tensor_tensor(DIb[:], ident[:].unsqueeze(1).to_broadcast([128, 64, 128]),
                                                  Dd[:].unsqueeze(2).to_broadcast([128, 64, 128]), ALU.mult),
                 reads=["ident", "Dd"], writes=["DIb"])
            ldi = {"n": 0}

            def setup(tile, d, full, sl):
                tri = triF if d == 0 else triB
                trin = "triF" if d == 0 else "triB"
                h = hst[d]; hn = "hst%d" % d
                dt = dtL[sl]
                aa = aaL[sl]
                acs = acsL[sl]
                atot = atotL[sl]
                wend = wendL[sl]
                eacs = eacsL[sl]
                etot = etotL[sl]
                wx = wxL[sl]
                wxe = wxeL[sl]
                P.dma(xtm[sl][:], XTM[tile * 128:(tile + 1) * 128, :], writes=["xtm%d" % sl], key=None)
                yield
                P.dma(dtr[sl][:], DTs[tile * 128:(tile + 1) * 128, :], writes=["dtr%d" % sl], key=None)
                yield
                if full:
                    P.dma(bct[sl][:], BCT[:, tile * 128:(tile + 1) * 128].rearrange("(c p) t -> p c t", p=128),
                          writes=["bct%d" % sl], key=None)
                    yield
                P.op("dve", lambda e: e.tensor_tensor(dt[:], dtr[sl][:, d * 64:(d + 1) * 64], dtbb[:, d * 64:(d + 1) * 64], ALU.add),
                     reads=["dtr%d" % sl, "dtbb"], writes=["dt%d" % sl])
                yield
                P.op("act", lambda e: e.activation(dt[:], dt[:], AF.Exp), reads=["dt%d" % sl], writes=["dt%d" % sl])
                yield
                P.op("act", lambda e: e.activation(dt[:], dt[:], AF.Ln, bias=cst[:, 0:1]), reads=["dt%d" % sl, "cst0"], writes=["dt%d" % sl])
                yield
                P.op("dve", lambda e: e.tensor_tensor(aa[:], dt[:], Ab[:, d * 64:(d + 1) * 64], ALU.mult), reads=["dt%d" % sl, "Ab"], writes=["aa%d" % sl])
                yield
                P.op("pe", lambda e: e.matmul(ps[4][:, 0:64], lhsT=tri[:], rhs=aa[:], start=True, stop=True),
                     reads=[trin, "aa%d" % sl], writes=["ps4"])
                yield
                P.op("pe", lambda e: e.matmul(ps[4][:, 64:128], lhsT=ones[:], rhs=aa[:], start=True, stop=True),
                     reads=["ones", "aa%d" % sl], writes=["ps4"])
                yield
                P.op("dve", lambda e: e.tensor_copy(acs[:], ps[4][:, 0:64]), reads=["ps4"], writes=["acs%d" % sl])
                yield
                P.op("dve", lambda e: e.tensor_copy(atot[:], ps[4][:, 64:128]), reads=["ps4"], writes=["atot%d" % sl])
                yield
                P.op("dve", lambda e: e.tensor_tensor(wend[:], atot[:], acs[:], ALU.subtract), reads=["atot%d" % sl, "acs%d" % sl], writes=["wend%d" % sl])
                yield
                P.op("act", lambda e: e.activation(wend[:], wend[:], AF.Exp), reads=["wend%d" % sl], writes=["wend%d" % sl])
                yield
                P.op("act", lambda e: e.activation(etot[:], atot[:], AF.Exp), reads=["atot%d" % sl], writes=["etot%d" % sl])
                yield
                v3 = lambda ap: ap.rearrange("p (h q) -> p h q", h=64)
                bc = lambda ap: ap.unsqueeze(2).to_broadcast([128, 64, 64])
                P.op("dve", lambda e: e.tensor_tensor(wend[:], wend[:], dt[:], ALU.mult), reads=["wend%d" % sl, "dt%d" % sl], writes=["wend%d" % sl])
                yield
                P.op("dve", lambda e: e.tensor_tensor(v3(wxe[:]), v3(xtm[sl][:, 0:DI]), bc(wend[:]), ALU.mult),
                     reads=["xtm%d" % sl, "wend%d" % sl], writes=["wxe%d" % sl])
                yield
                if full:
                    P.op("act", lambda e: e.activation(wx[:, 0:64], dt[:], AF.Ln), reads=["dt%d" % sl], writes=["lndt%d" % sl])
                    yield
                    P.op("dve", lambda e: e.tensor_tensor(wx[:, 64:128], acs[:], wx[:, 0:64], ALU.subtract),
                         reads=["acs%d" % sl, "lndt%d" % sl], writes=["acs2%d" % sl])
                    yield
                    P.op("dve", lambda e: e.tensor_scalar(wx[:, 0:64], wx[:, 64:128], -1.0, None, op0=ALU.mult),
                         reads=["acs2%d" % sl], writes=["lndt%d" % sl, "nacs2%d" % sl])
                    yield
                if full:
                    P.op("act", lambda e: e.activation(eacs[:], acs[:], AF.Exp), reads=["acs%d" % sl], writes=["eacs%d" % sl])
                    yield

            def body(tile, d, full, sl, pump):
                tri = triF if d == 0 else triB
                trin = "triF" if d == 0 else "triB"
                h = hst[d]; hn = "hst%d" % d
                dt = dtL[sl]
                aa = aaL[sl]
                acs = acsL[sl]
                atot = atotL[sl]
                wend = wendL[sl]
                eacs = eacsL[sl]
                etot = etotL[sl]
                wx = wxL[sl]
                wxe = wxeL[sl]
                v3 = lambda ap: ap.rearrange("p (h q) -> p h q", h=64)
                bc = lambda ap: ap.unsqueeze(2).to_broadcast([128, 64, 64])
                if full:
                    P.op("dve", lambda e: e.tensor_copy(hbf[:], h[:]), reads=[hn], writes=["hbf"])
                    for hf in range(2):
                        for j in range(4):
                            g = 4 * hf + j
                            P.op("pe", lambda e, g=g, j=j: e.matmul(ps[5][:, j * 128:(j + 1) * 128], lhsT=bct[sl][:, g, :], rhs=bct[sl][:, 8 + g, :],
                                                                    start=True, stop=True), reads=["bct%d" % sl], writes=["ps5"])
                        P.op("dve", lambda e, hf=hf: e.tensor_tensor(CBa[:, 4 * hf:4 * hf + 4, :], ps[5][:].rearrange("p (j l) -> p j l", j=4),
                                                                     tri[:].unsqueeze(1).to_broadcast([128, 4, 128]), ALU.mult),
                             reads=["ps5", trin], writes=["CBa%d" % hf])

                    def s_TA(g):
                        for r in range(8):
                            hh = 8 * g + r
                            P.op("pe", lambda e, r=r, hh=hh: e.matmul(ps[r // 4][:, (r % 4) * 128:(r % 4 + 1) * 128],
                                                                       lhsT=aa[:, hh:hh + 1].to_broadcast([128, 128]), rhs=tri[:],
                                                                       start=True, stop=True),
                                 reads=[trin, "aa%d" % sl], writes=["ps%d" % (r // 4)])

                    def s_seg(g):
                        m = g % 2
                        for r in range(8):
                            hh = 8 * g + r
                            P.op("act", lambda e, r=r, hh=hh: e.activation(segL[m][:, r, :], ps[r // 4][:, (r % 4) * 128:(r % 4 + 1) * 128],
                                                                          AF.Exp, bias=wx[:, hh:hh + 1]),
                                 reads=["ps%d" % (r // 4), "nacs2%d" % sl], writes=["seg%d_%d" % (m, r // 4)])

                    def s_M(g):
                        m = g % 2
                        P.op("dve", lambda e: e.scalar_tensor_tensor(out=Mt2[m][:], in0=segL[m][:], scalar=1e30,
                                                                     in1=CBa[:, g, :].unsqueeze(1).to_broadcast([128, 8, 128]),
                                                                     op0=ALU.min, op1=ALU.mult),
                             reads=["seg%d_0" % m, "seg%d_1" % m, "CBa%d" % (g // 4)], writes=["Mt%d" % m])
                        for r in range(8):
                            hh = 8 * g + r
                            P.op("pe", lambda e, r=r, hh=hh: e.matmul(ps[2][:, r * 64:(r + 1) * 64], lhsT=Mt2[m][:, r, :],
                                                                       rhs=xtm[sl][:, hh * 64:(hh + 1) * 64], start=True, stop=(d != 0)),
                                 reads=["Mt%d" % m, "xtm%d" % sl], writes=["ps2"])
                            if d == 0:
                                P.op("pe", lambda e, r=r, hh=hh: e.matmul(ps[2][:, r * 64:(r + 1) * 64], lhsT=DIb[:, hh, :],
                                                                           rhs=xtm[sl][:, hh * 64:(hh + 1) * 64], start=False, stop=True),
                                     reads=["DIb", "xtm%d" % sl], writes=["ps2"])
                        P.op("pe", lambda e: e.matmul(ps[3][:], lhsT=bct[sl][:, 8 + g, :], rhs=hbf[:, g * 512:(g + 1) * 512],
                                                      start=True, stop=True), reads=["bct%d" % sl, "hbf"], writes=["ps3"])

                    def s_Y(g):
                        P.op("dve", lambda e: e.tensor_tensor(ytmp[:].rearrange("p (r q) -> p r q", r=8),
                                                              ps[3][:].rearrange("p (r q) -> p r q", r=8),
                                                              eacs[:, 8 * g:8 * g + 8].unsqueeze(2).to_broadcast([128, 8, 64]), ALU.mult),
                             reads=["ps3", "eacs%d" % sl], writes=["ytmp"])
                        P.op("dve", lambda e: e.tensor_tensor(yt[:, g * 512:(g + 1) * 512], ps[2][:], ytmp[:], ALU.add),
                             reads=["ps2", "ytmp"], writes=["yt"])

                    for k in range(-2, 9):
                        if 0 <= k - 1 < 8:
                            s_Y(k - 1)
                        if 0 <= k + 1 < 8:
                            s_seg(k + 1)
                        if 0 <= k + 2 < 8:
                            s_TA(k + 2)
                        pump(1)
                        if 0 <= k < 8:
                            s_M(k)
                        pump(1)
                    P.dma((Y1 if d == 0 else Y2)[tile * 128:(tile + 1) * 128, :], yt[:], reads=["yt"], key="st")
                for half in range(2):
                    pump(4)
                    for g4 in range(4):
                        g = half * 4 + g4
                        P.op("pe", lambda e, g=g, g4=g4: e.matmul(ps[g4][:], lhsT=xtm[sl][:, DI + g * 128:DI + (g + 1) * 128],
                                                                  rhs=wxe[:, g * 512:(g + 1) * 512], start=True, stop=True),
                             reads=["xtm%d" % sl, "wxe%d" % sl], writes=["ps%d" % g4])
                    for g4 in range(4):
                        g = half * 4 + g4
                        eng = "dve" if g4 % 2 == 0 else "pool"
                        P.op("dve", lambda e, g=g: e.tensor_tensor(h[:, g * 512:(g + 1) * 512].rearrange("p (r q) -> p r q", r=8),
                                                                   h[:, g * 512:(g + 1) * 512].rearrange("p (r q) -> p r q", r=8),
                                                                   etot[:, 8 * g:8 * g + 8].unsqueeze(2).to_broadcast([128, 8, 64]), ALU.mult),
                             reads=[hn, "etot%d" % sl, "hbf"], writes=[hn])
                        P.op("dve", lambda e, g=g, g4=g4: e.tensor_tensor(h[:, g * 512:(g + 1) * 512], h[:, g * 512:(g + 1) * 512],
                                                                          ps[g4][:], ALU.add),
                             reads=[hn, "ps%d" % g4], writes=[hn])

            seq = ([(16, 0, False), (17, 0, False)] + [(t, 0, True) for t in range(8)] + [(17, 1, False), (16, 1, False)]
                   + [(t, 1, False) for t in range(15, 7, -1)] + [(t, 1, True) for t in range(7, -1, -1)])
            gens = [setup(sq[0], sq[1], sq[2], i % 2) for i, sq in enumerate(seq)]
            for _ in gens[0]:
                pass
            for i, sq in enumerate(seq):
                nxt = gens[i + 1] if i + 1 < len(seq) else None

                def pump(k, nxt=nxt):
                    if nxt is not None:
                        for _ in range(k):
                            next(nxt, None)
                body(sq[0], sq[1], sq[2], i % 2, pump)
                if nxt is not None:
                    for _ in nxt:
                        pass
        P.barrier()

        with ExitStack() as st:
            mT = sbt(st, "mT", [128, 16, 1024], BF16)
            with ExitStack() as st5:
                ynT = sbt(st5, "ynT", [128, 32, 1024], BF16)
                with ExitStack() as st5a:
                    y1L = [sbt(st5a, "y1%d" % i, [128, DI]) for i in range(2)]; y2L = [sbt(st5a, "y2%d" % i, [128, DI]) for i in range(2)]
                    zz = sbt(st5a, "zz", [128, DI])
                    gN = sbt(st5a, "gN", [128, DI]); ynb = sbt(st5a, "ynb", [128, DI], BF16)
                    ss5 = sbt(st5a, "ss5", [128, 4])
                    P.dma(gN[:], gN_d.partition_broadcast(128), writes=["gN"])
                    for t in range(8):
                        y1 = y1L[t % 2]; y2 = y2L[t % 2]; y1n = "y1%d" % (t % 2); y2n = "y2%d" % (t % 2)
                        P.dma(y1[:], Y1[t * 128:(t + 1) * 128, :], writes=[y1n])
                        P.dma(y2[:], Y2[t * 128:(t + 1) * 128, :], writes=[y2n])
                        P.dma(zz[:], Zs[t * 128:(t + 1) * 128, :], writes=["zz"])
                        P.op("dve", lambda e, y1=y1, y2=y2: e.tensor_tensor(y1[:], y1[:], y2[:], ALU.add), reads=[y1n, y2n], writes=[y1n])
                        P.op("act", lambda e: e.activation(zz[:], zz[:], AF.Silu), reads=["zz"], writes=["zz"])
                        P.op("dve", lambda e, y1=y1: e.tensor_tensor(y1[:], y1[:], zz[:], ALU.mult), reads=[y1n, "zz"], writes=[y1n])
                        P.op("dve", lambda e: e.memset(ss5[:, 0:1], 0.0), writes=["ss5a"])
                        P.op("act", lambda e, y1=y1, y2=y2: e.activation(y2[:], y1[:], AF.Square, accum_out=ss5[:, 0:1]),
                             reads=[y1n, "ss5a"], writes=[y2n, "ss5a"])
                        P.op("act", lambda e: e.activation(ss5[:, 1:2], ss5[:, 0:1], AF.Sqrt, bias=cst[:, 1:2], scale=1.0 / DI),
                             reads=["ss5a"], writes=["ss5b"])
                        P.op("dve", lambda e: e.reciprocal(ss5[:, 2:3], ss5[:, 1:2]), reads=["ss5b"], writes=["ss5c"])
                        P.op("dve", lambda e, y1=y1: e.scalar_tensor_tensor(out=ynb[:], in0=y1[:], scalar=ss5[:, 2:3], in1=gN[:],
                                                                     op0=ALU.mult, op1=ALU.mult),
                             reads=[y1n, "ss5c", "gN"], writes=["ynb"])
                        for q in range(4):
                            hb = q % 2
                            for j in range(8):
                                kc = q * 8 + j
                                P.op("pe", lambda e, hb=hb, j=j, kc=kc: e.transpose(
                                    psb[hb][:, j * 128:(j + 1) * 128], ynb[:, kc * 128:(kc + 1) * 128], identb[:]),
                                    reads=["ynb", "identb"], writes=["psb%d" % hb])
                            copy_op(evac_eng(), ynT[:, q * 8:(q + 1) * 8, t * 128:(t + 1) * 128],
                                    psb[hb][:].rearrange("p (k t) -> p k t", k=8), reads=["psb%d" % hb], writes=["ynT"])
                P.barrier()
                aTs = sbt(st5, "aTs", [128, 16, 1024], BF16)
                P.dma(aTs[:], ATT, writes=["aTs"])
                wbsL = [sbt(st5, "wbs%d" % i, [128, 32, 256], BF16) for i in range(2)]
                wbaL = [sbt(st5, "wba%d" % i, [128, 16, 256], BF16) for i in range(2)]
                gsL = [sbt(st5, "gs%d" % i, [128, 512]) for i in range(2)]; gaL = [sbt(st5, "ga%d" % i, [128, 512]) for i in range(2)]
                m1L = [sbt(st5, "m1%d" % i, [128, 512]) for i in range(2)]; m2L = [sbt(st5, "m2%d" % i, [128, 512]) for i in range(2)]
                for blk in range(8):
                    wbs = wbsL[blk % 2]; wba = wbaL[blk % 2]
                    wbsr = ["wbs%d_%d" % (blk % 2, k0) for k0 in range(0, 32, 4)]
                    wbar = ["wba%d_%d" % (blk % 2, k0) for k0 in range(0, 16, 4)]
                    for k0 in range(0, 32, 4):
                        P.dma(wbs[:, k0:k0 + 4, :], w_bs[k0 * 128:(k0 + 4) * 128, blk * 256:(blk + 1) * 256].rearrange("(k p) n -> p k n", p=128),
                              writes=["wbs%d_%d" % (blk % 2, k0)], key=("wbs", blk % 2), queue="pool")
                    for k0 in range(0, 16, 4):
                        P.dma(wba[:, k0:k0 + 4, :], w_ba[k0 * 128:(k0 + 4) * 128, blk * 256:(blk + 1) * 256].rearrange("(k p) n -> p k n", p=128),
                              writes=["wba%d_%d" % (blk % 2, k0)], key=("wba", blk % 2), queue="pool")
                    for ch in range(2):
                        cabs = blk * 2 + ch
                        for th in range(2):
                            t0 = th * 512
                            u5 = (cabs * 2 + th) % 2
                            gsu, gau, m1u, m2u = gsL[u5], gaL[u5], m1L[u5], m2L[u5]
                            pA, pB = 2 * u5, 2 * u5 + 1
                            P.dma(gsu[:], GTs[cabs * 128:(cabs + 1) * 128, t0:t0 + 512], writes=["gs%d" % u5])
                            P.dma(gau[:], GTs[2048 + cabs * 128:2048 + (cabs + 1) * 128, t0:t0 + 512], writes=["ga%d" % u5])
                            P.op("act", lambda e, gsu=gsu: e.activation(gsu[:], gsu[:], AF.Sigmoid), reads=["gs%d" % u5], writes=["gs%d" % u5])
                            P.op("act", lambda e, gau=gau: e.activation(gau[:], gau[:], AF.Sigmoid), reads=["ga%d" % u5], writes=["ga%d" % u5])
                            for kc in range(32):
                                P.op("pe", lambda e, kc=kc, ch=ch, t0=t0, wbs=wbs, pA=pA: e.matmul(ps[pA][:], lhsT=wbs[:, kc, ch * 128:(ch + 1) * 128],
                                                                                  rhs=ynT[:, kc, t0:t0 + 512], start=(kc == 0), stop=(kc == 31)),
                                     reads=wbsr + ["ynT"], writes=["ps%d" % pA])
                            for kc in range(16):
                                P.op("pe", lambda e, kc=kc, ch=ch, t0=t0, wba=wba, pB=pB: e.matmul(ps[pB][:], lhsT=wba[:, kc, ch * 128:(ch + 1) * 128],
                                                                                  rhs=aTs[:, kc, t0:t0 + 512], start=(kc == 0), stop=(kc == 15)),
                                     reads=wbar + ["aTs"], writes=["ps%d" % pB])
                            P.op("dve", lambda e, m1u=m1u, gsu=gsu, pA=pA: e.tensor_tensor(m1u[:], ps[pA][:], gsu[:], ALU.mult),
                                 reads=["ps%d" % pA, "gs%d" % u5], writes=["m1%d" % u5])
                            P.op("dve", lambda e, m2u=m2u, gau=gau, pB=pB: e.tensor_tensor(m2u[:], ps[pB][:], gau[:], ALU.mult),
                                 reads=["ps%d" % pB, "ga%d" % u5], writes=["m2%d" % u5])
                            P.op("dve", lambda e, cabs=cabs, t0=t0, m1u=m1u, m2u=m2u: e.tensor_tensor(mT[:, cabs, t0:t0 + 512], m1u[:], m2u[:], ALU.add),
                                 reads=["m1%d" % u5, "m2%d" % u5], writes=["mT"])
            P.barrier()
            wo = [sbt(st, "wo%d" % i, [128, 16, 512], BF16) for i in range(2)]
            mod2 = sbt(st, "mod2", [128, D])
            xo = [sbt(st, "xo%d" % i, [128, 512]) for i in range(2)]
            ho = [sbt(st, "ho%d" % i, [128, 512]) for i in range(2)]
            P.dma(mod2[:], MOD[2], writes=["mod2"])
            hi = 0
            for blk in range(4):
                sl = blk % 2
                load_w_block(wo, sl, w_o, 16, blk * 512, 512)
                for t in range(8):
                    i = hi % 2
                    hi += 1
                    P.dma(xo[i][:], x_c[t * 128:(t + 1) * 128, blk * 512:(blk + 1) * 512], writes=["xo%d" % i], key=("xo", i))
                    b = hi % 2
                    for kc in range(16):
                        P.op("pe", lambda e, b=b, kc=kc, t=t, sl=sl: e.matmul(ps[b][:], lhsT=mT[:, kc, t * 128:(t + 1) * 128],
                                                                              rhs=wo[sl][:, kc, :], start=(kc == 0), stop=(kc == 15)),
                             reads=["mT"] + ["wb%d_%d" % (sl, k0) for k0 in range(0, 16, 4)], writes=["ps%d" % b])
                    P.op("dve", lambda e, b=b, i=i, blk=blk: e.tensor_tensor(ho[i][:], ps[b][:], mod2[:, blk * 512:(blk + 1) * 512], ALU.mult),
                         reads=["ps%d" % b, "mod2"], writes=["ho%d" % i])
                    P.op("dve", lambda e, i=i: e.tensor_tensor(ho[i][:], ho[i][:], xo[i][:], ALU.add),
                         reads=["ho%d" % i, "xo%d" % i], writes=["ho%d" % i])
                    P.dma(Hs[t * 128:(t + 1) * 128, blk * 512:(blk + 1) * 512], ho[i][:], reads=["ho%d" % i], key="st")
        P.barrier()

        with ExitStack() as st:
            u2T = sbt(st, "u2T", [128, 16, 1024], BF16)
            with ExitStack() as st6:
                A2 = sbt(st6, "A2", [128, D]); S3 = sbt(st6, "S3", [128, D])
                P.dma(A2[:], MOD[3], writes=["A2"])
                P.dma(S3[:], MOD[4], writes=["S3"])
                norm_mod_T(st6, lambda t: Hs[t * 128:(t + 1) * 128, :], 8, lambda t: (A2, "A2"), lambda t: (S3, "S3"), u2T, "n2")
            P.barrier()
            with ExitStack() as st6:
                q2T = sbt(st6, "q2T", [128, 16, 1024], BF16)
                with ExitStack() as st6w:
                    wq = [sbt(st6w, "wq%d" % i, [128, 16, 512], BF16) for i in range(2)]
                    for blk in range(4):
                        sl = blk % 2
                        load_w_block(wq, sl, w_q, 16, blk * 512, 512)

                        def cons(pst, psn, ch, t0, nt, blk=blk):
                            copy_op(evac_eng(), q2T[:, blk * 4 + ch, t0:t0 + nt], pst[:, 0:nt], reads=[psn], writes=["q2T"])
                        linear_F(u2T, "xT_n2", 16, wq, sl, 512, [(0, 512), (512, 512)], cons)
                P.barrier()
                kin6 = sbt(st6, "kin6", [128, 2, 128])
                kT6 = sbt(st6, "kT6", [128, 2, 128], BF16)
                P.dma(kin6[:, 0, :], keys1, writes=["kin6_a"], key="s6k")
                P.dma(kin6[:, 1, :], keys2, writes=["kin6_b"], key="s6k")
                for i in range(2):
                    P.op("pe", lambda e, i=i: e.transpose(ps[4][:, i * 128:(i + 1) * 128], kin6[:, i, :], ident[:]),
                         reads=["kin6_a", "kin6_b", "ident"], writes=["ps4"])
                P.op("dve", lambda e: e.tensor_copy(kT6[:], ps[4][:, 0:256].rearrange("p (i k) -> p i k", i=2)),
                     reads=["ps4"], writes=["kT6"])
                sc = sbt(st6, "sc", [128, 16, 128]); tmp6 = sbt(st6, "tmp6", [128, 256])
                mx = sbt(st6, "mx", [128, 16, 16]); negm = sbt(st6, "negm", [128, 16])
                E = sbt(st6, "E", [128, 16, 128]); Et = sbt(st6, "Et", [128, 16, 16])
                cand = sbt(st6, "cand", [128, 8, 256]); ctop = sbt(st6, "ctop", [128, 8, 16])
                Zs6 = sbt(st6, "Zs6", [128, 8]); rZ = sbt(st6, "rZ", [128, 8])
                Pd = [sbt(st6, "Pd%d" % i, [128, 8, 128]) for i in range(3)]
                Gm = [sbt(st6, "Gm%d" % i, [128, 8, 8, 128], BF16) for i in range(2)]
                gst = [sbt(st6, "gst%d" % i, [128, 8, 128], BF16) for i in range(2)]
                E1n = sbt(st6, "E1n", [128, 8, 128]); Etn = sbt(st6, "Etn", [128, 8, 16]); ctop2 = sbt(st6, "ctop2", [128, 8, 16])
                thrg = sbt(st6, "thrg", [128, 8])
                gi = {"n": 0}
                pub = [sbt(st6, "pub%d" % i, [128, D], BF16) for i in range(2)]
                puT = [sbt(st6, "puT%d" % i, [128, 16, 128], BF16) for i in range(2)]
                actb = [sbt(st6, "actb%d" % i, [128, 1024], BF16) for i in range(2)]

                def phaseA(ch):
                    sl = ch % 2
                    P.dma(pub[sl][:], pu[ch * 128:(ch + 1) * 128, :], writes=["pub%d" % sl], key=("pub", sl), queue="pool")
                    for half in range(2):
                        for j in range(8):
                            kc = half * 8 + j
                            P.op("pe", lambda e, kc=kc, half=half, j=j: e.transpose(
                                psb[half][:, j * 128:(j + 1) * 128], pub[sl][:, kc * 128:(kc + 1) * 128], identb[:]),
                                reads=["pub%d" % sl, "identb"], writes=["psb%d" % half])
                        copy_op("act", puT[sl][:, half * 8:(half + 1) * 8, :], psb[half][:].rearrange("p (k t) -> p k t", k=8),
                                reads=["psb%d" % half], writes=["puT%d" % sl])
                    for th in range(2):
                        for kc in range(16):
                            P.op("pe", lambda e, kc=kc, th=th: e.matmul(ps[4 + th][:], lhsT=puT[sl][:, kc, :],
                                                                        rhs=u2T[:, kc, th * 512:(th + 1) * 512],
                                                                        start=(kc == 0), stop=(kc == 15)),
                                 reads=["puT%d" % sl, "xT_n2"], writes=["ps%d" % (4 + th)])
                        P.op("act", lambda e, th=th: e.activation(actb[sl][:, th * 512:(th + 1) * 512], ps[4 + th][:], AF.Gelu),
                             reads=["ps%d" % (4 + th)], writes=["actb%d_%d" % (sl, th)])
                    P.dma(ACTs[ch * 128:(ch + 1) * 128, :], actb[sl][:], reads=["actb%d_0" % sl, "actb%d_1" % sl], key=("st", "actb%d" % sl))

                def g_all():
                    for t in range(8):
                        for c in range(16):
                            P.op("pe", lambda e, c=c, t=t: e.matmul(ps[c // 4][:, (c % 4) * 128:(c % 4 + 1) * 128],
                                                                    lhsT=q2T[:, c, t * 128:(t + 1) * 128], rhs=kT6[:, c % 2, :],
                                                                    start=True, stop=True), reads=["q2T", "kT6"], writes=["ps%d" % (c // 4)])
                        for b4 in range(4):
                            copy_op(evac_eng(), sc[:, b4 * 4:(b4 + 1) * 4, :], ps[b4][:].rearrange("p (c k) -> p c k", c=4),
                                    reads=["ps%d" % b4], writes=["sc"])
                        for c in range(16):
                            P.op("dve", lambda e, c=c: e.max(out=mx[:, c, 0:8], in_=sc[:, c, :]), reads=["sc"], writes=["mx"])
                            P.op("dve", lambda e, c=c: e.match_replace(out=tmp6[:, 0:128], in_to_replace=mx[:, c, 0:8], in_values=sc[:, c, :],
                                                                       imm_value=-1e30), reads=["sc", "mx"], writes=["tmp6"])
                            P.op("dve", lambda e, c=c: e.max(out=mx[:, c, 8:16], in_=tmp6[:, 0:128]), reads=["tmp6"], writes=["mx"])
                        P.op("dve", lambda e: e.tensor_scalar(negm[:], mx[:, :, 0], -1.0, None, op0=ALU.mult), reads=["mx"], writes=["negm"])
                        for c in range(16):
                            P.op("act", lambda e, c=c: e.activation(E[:, c, :], sc[:, c, :], AF.Exp, bias=negm[:, c:c + 1]),
                                 reads=["sc", "negm"], writes=["E"])
                            P.op("act", lambda e, c=c: e.activation(Et[:, c, :], mx[:, c, :], AF.Exp, bias=negm[:, c:c + 1]),
                                 reads=["mx", "negm"], writes=["Et"])
                        Et4 = Et[:].rearrange("p (h two) a -> p h two a", two=2)
                        P.op("dve", lambda e, Et4=Et4: e.tensor_tensor(cand[:].rearrange("p h (a b) -> p h a b", a=16),
                                                                       Et4[:, :, 0, :].unsqueeze(3).to_broadcast([128, 8, 16, 16]),
                                                                       Et4[:, :, 1, :].unsqueeze(2).to_broadcast([128, 8, 16, 16]), ALU.mult),
                             reads=["Et"], writes=["cand"])
                        for hh in range(8):
                            P.op("dve", lambda e, hh=hh: e.max(out=ctop[:, hh, 0:8], in_=cand[:, hh, :]), reads=["cand"], writes=["ctop"])
                            P.op("dve", lambda e, hh=hh: e.match_replace(out=tmp6[:], in_to_replace=ctop[:, hh, 0:8], in_values=cand[:, hh, :],
                                                                         imm_value=-1e30), reads=["cand", "ctop"], writes=["tmp6"])
                            P.op("dve", lambda e, hh=hh: e.max(out=ctop[:, hh, 8:16], in_=tmp6[:]), reads=["tmp6"], writes=["ctop"])
                        P.op("dve", lambda e: e.reduce_sum(Zs6[:], ctop[:], axis=AX.X), reads=["ctop"], writes=["Zs6"])
                        P.op("dve", lambda e: e.reciprocal(rZ[:], Zs6[:]), reads=["Zs6"], writes=["rZ"])
                        E4 = E[:].rearrange("p (h two) k -> p h two k", two=2)
                        P.op("dve", lambda e, E4=E4: e.tensor_tensor(E1n[:], E4[:, :, 0, :], rZ[:].unsqueeze(2).to_broadcast([128, 8, 128]), ALU.mult),
                             reads=["E", "rZ"], writes=["E1n"])
                        P.op("dve", lambda e: e.tensor_tensor(thrg[:], ctop[:, :, 15], rZ[:], ALU.mult), reads=["ctop", "rZ"], writes=["thrg"])
                        P.op("dve", lambda e: e.tensor_scalar(thrg[:], thrg[:], 1.0 - 1e-6, None, op0=ALU.mult), reads=["thrg"], writes=["thrg"])
                        yield
                        for ib in range(16):
                            w = ib % 2
                            for hh in range(8):
                                k3 = gi["n"] % 3
                                gi["n"] += 1
                                if False:
                                    for j in range(8):
                                        P.op("act", lambda e, hh=hh, ib=ib, k3=k3, j=j: e.activation(
                                            Pd[k3][:, j, :], E[:, 2 * hh + 1, :], AF.Identity, scale=E1n[:, hh, ib * 8 + j:ib * 8 + j + 1]),
                                            reads=["E", "E1n"], writes=["Pd%d" % k3])
                                else:
                                    P.op("dve", lambda e, hh=hh, ib=ib, k3=k3: e.tensor_tensor(
                                        Pd[k3][:], E1n[:, hh, ib * 8:(ib + 1) * 8].unsqueeze(2).to_broadcast([128, 8, 128]),
                                        E[:, 2 * hh + 1, :].unsqueeze(1).to_broadcast([128, 8, 128]), ALU.mult),
                                        reads=["E", "E1n"], writes=["Pd%d" % k3])
                                P.op("dve", lambda e, hh=hh, k3=k3, w=w: e.scalar_tensor_tensor(
                                    out=Gm[w][:, hh, :, :], in0=Pd[k3][:], scalar=thrg[:, hh:hh + 1], in1=Pd[k3][:], op0=ALU.is_ge, op1=ALU.mult),
                                    reads=["Pd%d" % k3, "thrg"], writes=["Gm%d_%d" % (w, hh)])
                            for j in range(8):
                                for hh in range(8):
                                    P.op("pe", lambda e, hh=hh, j=j, w=w: e.matmul(
                                        ps[2 * w + j // 4][:, (j % 4) * 128:(j % 4 + 1) * 128], lhsT=Gm[w][:, hh, j, :], rhs=identb[:],
                                        start=(hh == 0), stop=(hh == 7)),
                                        reads=["Gm%d_%d" % (w, hh), "identb"], writes=["ps%d" % (2 * w + j // 4)])
                            for q in range(2):
                                copy_op(evac_eng() if False else "act", gst[w][:, q * 4:(q + 1) * 4, :], ps[2 * w + q][:].rearrange("p (k t) -> p k t", k=4),
                                        reads=["ps%d" % (2 * w + q)], writes=["gst%d" % w])
                            P.dma(GTP[ib * 1024:(ib + 1) * 1024, t * 128:(t + 1) * 128].rearrange("(c p) t -> p c t", p=128), gst[w][:],
                                  reads=["gst%d" % w], key="st")
                            yield

                nxt = 0
                for k, _ in enumerate(g_all()):
                    if nxt < 128:
                        phaseA(nxt)
                        nxt += 1
                while nxt < 128:
                    phaseA(nxt)
                    nxt += 1
            P.barrier()
            with ExitStack() as st6:
                acc = sbt(st6, "acc", [128, 8, D])
                pvb = [sbt(st6, "pvb%d" % i, [128, 4, D], BF16) for i in range(2)]
                gtc = [sbt(st6, "gtc%d" % i, [128, 4, 1024], BF16) for i in range(2)]
                actc = [sbt(st6, "actc%d" % i, [128, 4, 1024], BF16) for i in range(2)]
                coef = actc
                P.op("pool", lambda e: e.memset(acc[:], 0.0), writes=["acc"])
                for grp in range(32):
                    gw = grp % 2
                    r0 = grp * 512
                    P.dma(pvb[gw][:], pv[r0:r0 + 512, :].rearrange("(c p) d -> p c d", p=128), writes=["pvb%d" % gw], key=("pvb", gw), queue="pool")
                    P.dma(gtc[gw][:], GTP[r0:r0 + 512, :].rearrange("(c p) t -> p c t", p=128), writes=["gtc%d" % gw], key=("gtc", gw))
                    P.dma(actc[gw][:], ACTs[r0:r0 + 512, :].rearrange("(c p) t -> p c t", p=128), writes=["actc%d" % gw], key=("actc", gw))
                    P.op("dve", lambda e, gw=gw: e.tensor_tensor(actc[gw][:], actc[gw][:], gtc[gw][:], ALU.mult),
                         reads=["actc%d" % gw, "gtc%d" % gw], writes=["actc%d" % gw, "coef%d" % gw])
                    for t in range(8):
                        for dq in range(4):
                            for c4 in range(4):
                                P.op("pe", lambda e, gw=gw, c4=c4, t=t, dq=dq: e.matmul(
                                    ps[dq][:], lhsT=coef[gw][:, c4, t * 128:(t + 1) * 128], rhs=pvb[gw][:, c4, dq * 512:(dq + 1) * 512],
                                    start=(c4 == 0), stop=(c4 == 3)), reads=["actc%d" % gw, "pvb%d" % gw], writes=["ps%d" % dq])
                        for dq in range(4):
                            eng = "dve" if dq % 2 == 0 else "pool"
                            P.op("dve", lambda e, t=t, dq=dq: e.tensor_tensor(acc[:, t, dq * 512:(dq + 1) * 512],
                                                                              acc[:, t, dq * 512:(dq + 1) * 512], ps[dq][:], ALU.add),
                                 reads=["acc", "ps%d" % dq], writes=["acc"])
                mod5 = sbt(st6, "mod5", [128, D]); gF = sbt(st6, "gF", [128, D])
                hin = sbt(st6, "hin", [128, D]); fo = sbt(st6, "fo", [128, D]); ssf = sbt(st6, "ssf", [128, 4])
                P.dma(mod5[:], MOD[5], writes=["mod5"])
                P.dma(gF[:], gF_d.partition_broadcast(128), writes=["gF"])
                for t in range(8):
                    P.dma(hin[:], Hs[t * 128:(t + 1) * 128, :], writes=["hin"])
                    P.op("dve", lambda e, t=t: e.tensor_tensor(acc[:, t, :], acc[:, t, :], mod5[:], ALU.mult),
                         reads=["acc", "mod5"], writes=["acc"])
                    P.op("dve", lambda e, t=t: e.tensor_tensor(hin[:], hin[:], acc[:, t, :], ALU.add), reads=["hin", "acc"], writes=["hin"])
                    P.op("dve", lambda e: e.memset(ssf[:, 0:1], 0.0), writes=["ssfa"])
                    P.op("act", lambda e: e.activation(fo[:], hin[:], AF.Square, accum_out=ssf[:, 0:1]), reads=["hin", "ssfa"], writes=["fo", "ssfa"])
                    P.op("act", lambda e: e.activation(ssf[:, 1:2], ssf[:, 0:1], AF.Sqrt, bias=cst[:, 1:2], scale=1.0 / D),
                         reads=["ssfa"], writes=["ssfb"])
                    P.op("dve", lambda e: e.reciprocal(ssf[:, 2:3], ssf[:, 1:2]), reads=["ssfb"], writes=["ssfc"])
                    P.op("dve", lambda e: e.scalar_tensor_tensor(out=fo[:], in0=hin[:], scalar=ssf[:, 2:3], in1=gF[:], op0=ALU.mult, op1=ALU.mult),
                         reads=["hin", "ssfc", "gF", "fo"], writes=["fo"])
                    P.dma(out_d[t * 128:(t + 1) * 128, :], fo[:], reads=["fo"], key="out")
        P.emit()
    return nc


def _rope_tables(flip):
    pos = np.arange(9 * 128)
    if flip:
        pos = (S - 1) - pos
    row = (pos // 64).astype(np.float32)
    col = (pos % 64).astype(np.float32)
    freqs = (np.float32(10000.0) ** (-np.arange(32, dtype=np.float32) / np.float32(32))).astype(np.float32)
    ar = row[:, None] * freqs[None, :]
    ac = col[:, None] * freqs[None, :]
    ang = np.concatenate([ar, ar, ac, ac], axis=-1).astype(np.float32)
    cos = np.cos(ang).astype(np.float32)
    sin = np.sin(ang).astype(np.float32)
    sgn = np.concatenate([-np.ones(32), np.ones(32), -np.ones(32), np.ones(32)]).astype(np.float32)
    return np.ascontiguousarray(np.tile(cos, (1, 16))), np.ascontiguousarray(np.tile(sin * sgn[None, :], (1, 16)))


_NC_CACHE = {}


def make_in_maps(x, c, ctx, c_ctx, ada_w, ada_b, norm1_g, w_in, conv_w, conv_b, dt_bias, a_log, ssm_d,
                 ssm_norm_g, attn_sink, w_branch_ssm, w_branch_attn, w_out, norm2_g, peer_wq, peer_keys1,
                 peer_keys2, peer_u, peer_v, final_norm_g):
    f = lambda a: np.ascontiguousarray(np.asarray(a, dtype=np.float32))
    x = f(x); ctx = f(ctx); c = f(c); c_ctx = f(c_ctx)
    w_in0 = f(w_in)[0]
    shared = {
        "ada_w": f(ada_w)[0], "ada_b": f(ada_b)[0][None, :], "norm1_g": f(norm1_g)[0][None, :],
        "norm2_g": f(norm2_g)[0][None, :], "final_g": f(final_norm_g)[None, :], "w_in": w_in0,
        "ssm_d": f(ssm_d)[0][None, :], "ssm_norm_g": f(ssm_norm_g)[0][None, :], "attn_sink": f(attn_sink)[0][None, :],
        "w_bs": f(w_branch_ssm)[0], "w_ba": f(w_branch_attn)[0], "w_o": f(w_out)[0], "w_q": f(peer_wq)[0],
        "keys1": f(peer_keys1)[0], "keys2": f(peer_keys2)[0], "peer_u": f(peer_u)[0], "peer_v": f(peer_v)[0],
        "ident": np.eye(128, dtype=np.float32),
        "triF": np.triu(np.ones((128, 128), np.float32)),
        "triB": np.tril(np.ones((128, 128), np.float32)),
        "cc_fm": np.ascontiguousarray(c_ctx.reshape(16, 128).T),
    }
    cw = f(conv_w)[0]
    cb = f(conv_b)[0]
    convb_fm = np.ascontiguousarray(cb.reshape(48, 128).T)
    wdt = w_in0[:, C_DT:C_DT + 128]
    per_flip = {}
    for flip in (0, 1):
        cwf = cw[::-1] if flip else cw
        convw_fm = np.ascontiguousarray(cwf.T.reshape(48, 128, 5).transpose(1, 0, 2))
        wd = np.concatenate([wdt[:, 64:], wdt[:, :64]], axis=1) if flip else wdt
        dtb = f(dt_bias)[0][::-1] if flip else f(dt_bias)[0]
        al = f(a_log)[0][::-1] if flip else f(a_log)[0]
        cos_t, sin_t = _rope_tables(flip)
        per_flip[flip] = {"convw_fm": convw_fm, "convb_fm": convb_fm, "w_dt": np.ascontiguousarray(wd),
                          "dt_bias": np.ascontiguousarray(dtb.reshape(1, 128)), "a_log": np.ascontiguousarray(al.reshape(1, 128)),
                          "cos_t": cos_t, "sin_t": sin_t}
    in_maps = []
    for core in range(8):
        b, s = core // 2, core % 2
        m = dict(shared)
        m.update(per_flip[s])
        m["x_c"] = np.ascontiguousarray(x[b, ::-1] if s else x[b])
        m["ctx_c"] = np.ascontiguousarray(ctx[b, ::-1] if s else ctx[b])
        m["c_fm"] = np.ascontiguousarray(c[b].reshape(16, 128).T)
        in_maps.append(m)
    return in_maps


def kernel(**inputs):
    in_maps = make_in_maps(**inputs)
    if "nc" not in _NC_CACHE:
        _NC_CACHE["nc"] = build(False)
    nc = _NC_CACHE["nc"]
    res = run_bass_kernel_spmd(nc, in_maps, core_ids=list(range(8)))
    out = np.zeros((4, S, D), np.float32)
    for core in range(8):
        b, s = core // 2, core % 2
        o = np.asarray(res.results[core]["out"], dtype=np.float32)
        if s == 0:
            out[b, 0:1024] = o
        else:
            out[b, 1024:2048] = o[::-1]
    return out
```
